# Optimizing a Trainium2 kernel written in Bass

```python
import jax, jax.numpy as jnp
from jax import lax
import numpy as np

D_MODEL = 1024
BATCH = 8
SEQ = 4096
DEPTH = 2

GRID_W = 64
CTX_LEN = 256
D_MIX = D_MODEL
N_MIXERS = 4
W_GROUP = D_MIX // N_MIXERS
N_SUB = 4
D_SUB = W_GROUP // N_SUB
OFF_A_X = 0
OFF_A_G = W_GROUP
OFF_B = 2 * W_GROUP
OFF_C = 3 * W_GROUP
OFF_D = 4 * W_GROUP
D_IN = 6 * W_GROUP
RG_CONV = 4
RG_C = 8.0
POOL_WINDOWS = (2, 4, 8, 16)
CONF_KERNEL = 31
N_EXPERTS = 32
TOP_K = 4
D_FF = D_MODEL
SWIGLU_LIMIT = 7.0
SWIGLU_ALPHA = 1.702
MOE_BLOCK = 256
EPS = 1e-6

kernel_name = "hybrid_rglru_pool_fourier_conformer_moe_dit"


def rmsnorm(x, g):
    xf = x.astype(jnp.float32)
    y = xf * lax.rsqrt(jnp.mean(xf * xf, axis=-1, keepdims=True) + EPS)
    return (y * g.astype(jnp.float32)).astype(x.dtype)


def modulate(h, shift, scale):
    return h * (1 + scale) + shift


def pos_embed_2d(n_tokens, dtype):
    rows_n = n_tokens // GRID_W
    row = jnp.repeat(jnp.arange(rows_n), GRID_W).astype(jnp.float32)
    col = jnp.tile(jnp.arange(GRID_W), rows_n).astype(jnp.float32)
    q = D_MODEL // 4
    omega = 1.0 / (10000.0 ** (jnp.arange(q, dtype=jnp.float32) / q))

    def emb(p):
        ang = p[:, None] * omega[None, :]
        return jnp.concatenate([jnp.sin(ang), jnp.cos(ang)], axis=-1)

    return jnp.concatenate([emb(row), emb(col)], axis=-1).astype(dtype)


def depthwise_conv(x, w, b, pad):
    y = lax.conv_general_dilated(x, w[:, None, :].astype(x.dtype), window_strides=(1,), padding=[pad],
                                 dimension_numbers=('NWC', 'WIO', 'NWC'), feature_group_count=x.shape[-1])
    return y + b


def linear_scan(a, b, h0):
    b = b.at[:, 0].add(a[:, 0] * h0)

    def comb(l, r):
        return (l[0] * r[0], r[0] * l[1] + r[1])

    _, h = lax.associative_scan(comb, (a, b), axis=1)
    return h


def rglru_direction(xa, conv_w, conv_b, w_r, b_r, w_i, b_i, lam, h0):
    bn, L, _ = xa.shape
    xc = depthwise_conv(xa, conv_w, conv_b, (RG_CONV - 1, 0))
    xh = xc.reshape(bn, L, N_SUB, D_SUB)
    r = jax.nn.sigmoid(jnp.einsum('blhc,hcd->blhd', xh, w_r) + b_r).reshape(bn, L, W_GROUP)
    i = jax.nn.sigmoid(jnp.einsum('blhc,hcd->blhd', xh, w_i) + b_i).reshape(bn, L, W_GROUP)
    log_a = -RG_C * r.astype(jnp.float32) * jax.nn.softplus(-lam.astype(jnp.float32))
    a = jnp.exp(log_a)
    bterm = jnp.sqrt(-jnp.expm1(2.0 * log_a)) * (i * xc).astype(jnp.float32)
    h = linear_scan(a, bterm, h0)
    return h.astype(xa.dtype), h[:, -1]


def pool_mixer(xb, w_pool, b_pool, pool_scale):
    bn, L, _ = xb.shape
    xg = xb.reshape(bn, L, N_SUB, D_SUB)
    cs = jnp.concatenate([jnp.zeros((bn, 1, N_SUB, D_SUB), jnp.float32),
                          jnp.cumsum(xg.astype(jnp.float32), axis=1)], axis=1)
    t = jnp.arange(L)
    outs = []
    for g, w in enumerate(POOL_WINDOWS):
        lo = jnp.clip(t - w // 2, 0, L)
        hi = jnp.clip(t + w - w // 2, 0, L)
        s = cs[:, hi, g] - cs[:, lo, g]
        outs.append(s / (hi - lo).astype(jnp.float32)[None, :, None])
    pooled = jnp.stack(outs, axis=2).astype(xb.dtype) - xg
    y = jnp.einsum('blgc,gcd->blgd', pooled, w_pool) + b_pool
    return y.reshape(bn, L, W_GROUP) * pool_scale


def fourier_mixer(xc, w_four, b_four):
    bn, L, _ = xc.shape
    xg = xc.reshape(bn, L, N_SUB, D_SUB).astype(jnp.float32)
    f = jnp.fft.fft2(xg, axes=(1, 3), norm='ortho').real.astype(xc.dtype)
    y = jnp.einsum('blgc,gcd->blgd', f, w_four) + b_four
    return y.reshape(bn, L, W_GROUP)


def conformer_conv(xd, conv_w, conv_b, ln_g, ln_b, w_pw, b_pw):
    bn, L, _ = xd.shape
    v = xd[..., :W_GROUP] * jax.nn.sigmoid(xd[..., W_GROUP:])
    v = depthwise_conv(v, conv_w, conv_b, (CONF_KERNEL // 2, CONF_KERNEL // 2))
    vh = v.reshape(bn, L, N_SUB, D_SUB).astype(jnp.float32)
    mu = jnp.mean(vh, axis=-1, keepdims=True)
    var = jnp.mean(jnp.square(vh - mu), axis=-1, keepdims=True)
    vh = (vh - mu) * lax.rsqrt(var + EPS)
    v = (vh.reshape(bn, L, W_GROUP) * ln_g.astype(jnp.float32) + ln_b.astype(jnp.float32)).astype(xd.dtype)
    return jax.nn.silu(v) @ w_pw + b_pw


def token_mixers(u, h0_f, h0_b, w_in, b_in, conv_a_w, conv_a_b, w_rg_r, b_rg_r, w_rg_i, b_rg_i, rg_lambda,
                 w_pool, b_pool, pool_scale, w_four, b_four, conv_d_w, conv_d_b, ln_d_g, ln_d_b, w_pw, b_pw,
                 w_out, b_out):
    proj = u @ w_in + b_in
    xa = proj[..., OFF_A_X:OFF_A_G]
    ga = proj[..., OFF_A_G:OFF_B]
    xb = proj[..., OFF_B:OFF_C]
    xc = proj[..., OFF_C:OFF_D]
    xd = proj[..., OFF_D:]
    y_f, hf = rglru_direction(xa, conv_a_w[0], conv_a_b[0], w_rg_r[0], b_rg_r[0], w_rg_i[0], b_rg_i[0],
                              rg_lambda[0], h0_f)
    y_b, hb = rglru_direction(jnp.flip(xa, axis=1), conv_a_w[1], conv_a_b[1], w_rg_r[1], b_rg_r[1],
                              w_rg_i[1], b_rg_i[1], rg_lambda[1], h0_b)
    ya = (y_f + jnp.flip(y_b, axis=1)) * jax.nn.gelu(ga)
    yb = pool_mixer(xb, w_pool, b_pool, pool_scale)
    yc = fourier_mixer(xc, w_four, b_four)
    yd = conformer_conv(xd, conv_d_w, conv_d_b, ln_d_g, ln_d_b, w_pw, b_pw)
    y = jnp.concatenate([ya, yb, yc, yd], axis=-1) @ w_out + b_out
    return (y, hf, hb)


def moe(h, w_router, b_router, w_gu, b_gu, w_down, b_down):
    T, D = h.shape
    logits = (h @ w_router + b_router).astype(jnp.float32)
    top_vals, top_idx = lax.top_k(logits, TOP_K)
    gates = jax.nn.softmax(top_vals, axis=-1)
    A = T * TOP_K
    e_flat = top_idx.reshape(A)
    g_flat = gates.reshape(A)
    tok_flat = jnp.arange(A, dtype=jnp.int32) // TOP_K
    order = jnp.argsort(e_flat)
    e_s, tok_s, g_s = e_flat[order], tok_flat[order], g_flat[order]
    counts = jnp.bincount(e_flat, length=N_EXPERTS)
    padded = ((counts + MOE_BLOCK - 1) // MOE_BLOCK) * MOE_BLOCK
    start = jnp.cumsum(counts) - counts
    pend = jnp.cumsum(padded)
    pstart = pend - padded
    dest = pstart[e_s] + (jnp.arange(A, dtype=jnp.int32) - start[e_s])
    n_blocks = -(-A // MOE_BLOCK) + N_EXPERTS
    P = n_blocks * MOE_BLOCK
    xs = jnp.zeros((P, D), h.dtype).at[dest].set(h[tok_s])
    block_e = jnp.minimum(jnp.searchsorted(pend, jnp.arange(n_blocks) * MOE_BLOCK, side='right'),
                          N_EXPERTS - 1)

    def expert_block(args):
        xb, e = args
        gu = xb @ w_gu[e] + b_gu[e]
        gt = jnp.minimum(gu[:, :D_FF], SWIGLU_LIMIT)
        up = jnp.clip(gu[:, D_FF:], -SWIGLU_LIMIT, SWIGLU_LIMIT)
        act = (up + 1) * (gt * jax.nn.sigmoid(SWIGLU_ALPHA * gt))
        return act @ w_down[e] + b_down[e]

    ys = lax.map(expert_block, (xs.reshape(n_blocks, MOE_BLOCK, D), block_e)).reshape(P, D)
    return jnp.zeros((T, D), h.dtype).at[tok_s].add(ys[dest] * g_s[:, None].astype(h.dtype))


def setup_inputs(seed: int = 0) -> dict:
    key = jax.random.key(seed)
    keys = jax.random.split(key, 40)
    f32 = jnp.float32
    D = D_MODEL

    def nrm(i, shape, scale):
        return scale * jax.random.normal(keys[i], shape, f32)

    def gain(i, shape):
        return 1.0 + nrm(i, shape, 0.02)

    u = jax.random.uniform(keys[39], (DEPTH, 2, W_GROUP), f32, 0.9, 0.999)
    s = u ** (1.0 / RG_C)
    rg_lambda = jnp.log(s) - jnp.log1p(-s)
    return {
        "x": nrm(0, (BATCH, SEQ, D), 1.0),
        "c": nrm(1, (BATCH, D), 1.0),
        "ctx": nrm(2, (BATCH, CTX_LEN, D), 1.0),
        "c_ctx": nrm(3, (D,), 1.0),
        "w_mod": nrm(4, (DEPTH, D, 6 * D), 0.5 * D ** -0.5),
        "b_mod": nrm(5, (DEPTH, 6 * D), 0.01),
        "norm1_g": gain(6, (DEPTH, D)),
        "norm2_g": gain(7, (DEPTH, D)),
        "w_in": nrm(8, (DEPTH, D, D_IN), D ** -0.5),
        "b_in": nrm(9, (DEPTH, D_IN), 0.01),
        "conv_a_w": nrm(10, (DEPTH, 2, RG_CONV, W_GROUP), RG_CONV ** -0.5),
        "conv_a_b": nrm(11, (DEPTH, 2, W_GROUP), 0.01),
        "w_rg_r": nrm(12, (DEPTH, 2, N_SUB, D_SUB, D_SUB), D_SUB ** -0.5),
        "b_rg_r": nrm(13, (DEPTH, 2, N_SUB, D_SUB), 0.01),
        "w_rg_i": nrm(14, (DEPTH, 2, N_SUB, D_SUB, D_SUB), D_SUB ** -0.5),
        "b_rg_i": nrm(15, (DEPTH, 2, N_SUB, D_SUB), 0.01),
        "rg_lambda": rg_lambda,
        "w_pool": nrm(16, (DEPTH, N_SUB, D_SUB, D_SUB), D_SUB ** -0.5),
        "b_pool": nrm(17, (DEPTH, N_SUB, D_SUB), 0.01),
        "pool_scale": gain(18, (DEPTH, W_GROUP)),
        "w_four": nrm(19, (DEPTH, N_SUB, D_SUB, D_SUB), D_SUB ** -0.5),
        "b_four": nrm(20, (DEPTH, N_SUB, D_SUB), 0.01),
        "conv_d_w": nrm(21, (DEPTH, CONF_KERNEL, W_GROUP), CONF_KERNEL ** -0.5),
        "conv_d_b": nrm(22, (DEPTH, W_GROUP), 0.01),
        "ln_d_g": gain(23, (DEPTH, W_GROUP)),
        "ln_d_b": nrm(24, (DEPTH, W_GROUP), 0.01),
        "w_pw": nrm(25, (DEPTH, W_GROUP, W_GROUP), W_GROUP ** -0.5),
        "b_pw": nrm(26, (DEPTH, W_GROUP), 0.01),
        "w_out": nrm(27, (DEPTH, D_MIX, D), D_MIX ** -0.5),
        "b_out": nrm(28, (DEPTH, D), 0.01),
        "w_router": nrm(29, (DEPTH, D, N_EXPERTS), D ** -0.5),
        "b_router": nrm(30, (DEPTH, N_EXPERTS), 0.01),
        "w_gu": nrm(31, (DEPTH, N_EXPERTS, D, 2 * D_FF), D ** -0.5),
        "b_gu": nrm(32, (DEPTH, N_EXPERTS, 2 * D_FF), 0.01),
        "w_down": nrm(33, (DEPTH, N_EXPERTS, D_FF, D), D_FF ** -0.5),
        "b_down": nrm(34, (DEPTH, N_EXPERTS, D), 0.01),
        "final_norm_g": gain(35, (D,)),
    }


def reference(x, c, ctx, c_ctx, w_mod, b_mod, norm1_g, norm2_g, w_in, b_in, conv_a_w, conv_a_b, w_rg_r, b_rg_r,
              w_rg_i, b_rg_i, rg_lambda, w_pool, b_pool, pool_scale, w_four, b_four, conv_d_w, conv_d_b, ln_d_g,
              ln_d_b, w_pw, b_pw, w_out, b_out, w_router, b_router, w_gu, b_gu, w_down, b_down, final_norm_g):
    bn, L, D = x.shape
    Lc = ctx.shape[1]
    x = x + pos_embed_2d(L, x.dtype)[None]
    sc = jax.nn.silu(c)
    scc = jax.nn.silu(c_ctx)
    for l in range(DEPTH):
        last = l == DEPTH - 1
        mod = sc @ w_mod[l] + b_mod[l]
        mod_c = scc @ w_mod[l] + b_mod[l]
        sh1, sc1, g1, sh2, sc2, g2 = jnp.split(mod[:, None, :], 6, axis=-1)
        csh1, csc1, cg1, csh2, csc2, cg2 = jnp.split(mod_c, 6, axis=-1)
        mix_p = (w_in[l], b_in[l], conv_a_w[l], conv_a_b[l], w_rg_r[l], b_rg_r[l], w_rg_i[l], b_rg_i[l],
                 rg_lambda[l], w_pool[l], b_pool[l], pool_scale[l], w_four[l], b_four[l], conv_d_w[l],
                 conv_d_b[l], ln_d_g[l], ln_d_b[l], w_pw[l], b_pw[l], w_out[l], b_out[l])
        moe_p = (w_router[l], b_router[l], w_gu[l], b_gu[l], w_down[l], b_down[l])
        h0 = jnp.zeros((bn, W_GROUP), jnp.float32)
        u_ctx = modulate(rmsnorm(ctx, norm1_g[l]), csh1, csc1)
        y_ctx, hf, hb = token_mixers(u_ctx, h0, h0, *mix_p)
        u_x = modulate(rmsnorm(x, norm1_g[l]), sh1, sc1)
        y_x, _, _ = token_mixers(u_x, hf, hb, *mix_p)
        x = x + g1 * y_x
        h_x = modulate(rmsnorm(x, norm2_g[l]), sh2, sc2)
        if not last:
            ctx = ctx + cg1 * y_ctx
            h_c = modulate(rmsnorm(ctx, norm2_g[l]), csh2, csc2)
            y_all = moe(jnp.concatenate([h_c.reshape(-1, D), h_x.reshape(-1, D)], axis=0), *moe_p)
            ctx = ctx + cg2 * y_all[:bn * Lc].reshape(bn, Lc, D)
            x = x + g2 * y_all[bn * Lc:].reshape(bn, L, D)
        else:
            x = x + g2 * moe(h_x.reshape(-1, D), *moe_p).reshape(bn, L, D)
    return rmsnorm(x, final_norm_g)
```

```python
import contextlib
import numpy as np
import ml_dtypes
import concourse.bass as bass
import concourse.mybir as mybir
from concourse.bass_utils import run_bass_kernel_spmd

F32 = mybir.dt.float32
BF16 = mybir.dt.bfloat16
AF = mybir.ActivationFunctionType
ALU = mybir.AluOpType
AX = mybir.AxisListType

D = 1024
LC = 256
LX = 4096
S = LC + LX
PADW = 16
SP = S + 3 * PADW
NE = 32
DEPTH = 2
EPS = 1e-6
BLK = 256
NBLK = 100
NSLOT = NBLK * BLK
U32 = mybir.dt.uint32
I32 = mybir.dt.int32
CH = [(0, 256)] + [(256 + 512 * i, 512) for i in range(8)]


def pcol(s):
    return s + PADW if s < LC else s + 2 * PADW


PV = {}
_o = 0
for _n, _k in [("b_mod", 48), ("n1g", 8), ("n2g", 8), ("b_in", 12), ("caw", 16), ("cab", 4), ("brr", 4), ("bri", 4),
               ("lam", 4), ("bpool", 2), ("pscale", 2), ("bfour", 2), ("cdw", 62), ("cdb", 2), ("lng", 2), ("lnb", 2),
               ("bpw", 2), ("b_out", 8), ("b_gu", 512), ("fng", 8)]:
    PV[_n] = _o
    _o += _k
NPV = _o


def _cols(v):
    v = np.asarray(v, np.float32).reshape(-1)
    return v.reshape(-1, 128).T


def pack_pv(inp, l):
    pv = np.zeros((128, NPV), np.float32)

    def put(name, v, off=0):
        c = _cols(v)
        pv[:, PV[name] + off:PV[name] + off + c.shape[1]] = c

    put("b_mod", inp["b_mod"][l]); put("n1g", inp["norm1_g"][l]); put("n2g", inp["norm2_g"][l]); put("b_in", inp["b_in"][l])
    for d in range(2):
        for j in range(4):
            put("caw", inp["conv_a_w"][l, d, j], (d * 4 + j) * 2)
        put("cab", inp["conv_a_b"][l, d], d * 2)
        put("brr", inp["b_rg_r"][l, d], d * 2)
        put("bri", inp["b_rg_i"][l, d], d * 2)
        put("lam", inp["rg_lambda"][l, d], d * 2)
    put("bpool", inp["b_pool"][l]); put("pscale", inp["pool_scale"][l]); put("bfour", inp["b_four"][l])
    for j in range(31):
        put("cdw", inp["conv_d_w"][l, j], j * 2)
    put("cdb", inp["conv_d_b"][l]); put("lng", inp["ln_d_g"][l]); put("lnb", inp["ln_d_b"][l]); put("bpw", inp["b_pw"][l])
    put("b_out", inp["b_out"][l])
    for e in range(NE):
        put("b_gu", inp["b_gu"][l, e], e * 16)
    put("fng", inp["final_norm_g"])
    return pv


def pack_bd(inp, l):
    bd = np.zeros((128, 12, 128), np.float32)

    def blk(idx, w4, cg):
        for j in range(2):
            bd[64 * j:64 * j + 64, idx, 64 * j:64 * j + 64] = w4[2 * cg + j]

    for d in range(2):
        for cg in range(2):
            blk(d * 2 + cg, inp["w_rg_r"][l, d], cg)
            blk(4 + d * 2 + cg, inp["w_rg_i"][l, d], cg)
    for cg in range(2):
        blk(8 + cg, inp["w_pool"][l], cg)
        blk(10 + cg, inp["w_four"][l], cg)
    return bd


def make_consts():
    cst = np.zeros((128, 6, 128), np.float32)
    c = np.arange(64)
    ang = 2 * np.pi * np.outer(c, c) / 64.0
    for j in range(2):
        cst[64 * j:64 * j + 64, 0, 64 * j:64 * j + 64] = np.cos(ang) / 8.0
        cst[64 * j:64 * j + 64, 1, 64 * j:64 * j + 64] = -np.sin(ang) / 8.0
        cst[64 * j:64 * j + 64, 2, 64 * j:64 * j + 64] = 1.0 / 64.0
    cst[:, 3, :] = 1.0
    cst[:, 4, :] = np.eye(128)
    cst[:, 5, :] = np.triu(np.ones((128, 128), np.float32), 1)
    iot = np.zeros((128, 33 + NBLK), np.float32)
    iot[:, 32 + NBLK] = np.arange(128)
    iot[:, :32] = np.arange(32)[None, :]
    iot[:, 32:32 + NBLK] = (np.arange(NBLK) * BLK)[None, :]

    def dft(L):
        k = np.arange(L, dtype=np.int64)
        a = 2 * np.pi * ((np.outer(k, k) % L).astype(np.float64)) / L
        return np.stack([np.cos(a), np.sin(a)]).astype(np.float32) / np.sqrt(L)

    def tile_tab(t, L):
        nlt, nk = L // 128, L // 256
        return np.ascontiguousarray(t.reshape(2, nlt, 128, nk, 256).transpose(0, 3, 2, 1, 4))

    dftx = tile_tab(dft(LX), LX).astype(ml_dtypes.bfloat16)
    dftc = tile_tab(dft(LC), LC).astype(ml_dtypes.bfloat16)
    invc = np.ones((2, 128, SP), np.float32)
    for g, w in enumerate((2, 4, 8, 16)):
        for (c0, L) in ((PADW, LC), (2 * PADW + LC, LX)):
            t = np.arange(L)
            lo = np.clip(t - w // 2, 0, L)
            hi = np.clip(t + w - w // 2, 0, L)
            invc[g // 2, 64 * (g % 2):64 * (g % 2) + 64, c0:c0 + L] = 1.0 / (hi - lo)
    rows_n = LX // 64
    row = np.repeat(np.arange(rows_n), 64).astype(np.float32)
    col = np.tile(np.arange(64), rows_n).astype(np.float32)
    q = D // 4
    omega = (1.0 / (10000.0 ** (np.arange(q, dtype=np.float32) / q))).astype(np.float32)

    def emb(p):
        a = p[:, None] * omega[None, :]
        return np.concatenate([np.sin(a), np.cos(a)], axis=-1)

    pos = np.concatenate([emb(row), emb(col)], axis=-1).astype(np.float32)
    return dict(cst=cst, dftx=dftx, dftc=dftc, invc=invc, iot=iot, pos=np.ascontiguousarray(pos.T))


class Prog:
    ENG = ("pe", "dve", "act", "pool", "sp")
    KD = 8

    def __init__(self, nc, stack):
        self.nc = nc
        self.ops = {e: [] for e in self.ENG}
        self.cnt = {e: 0 for e in self.ENG}
        self.sems = {}
        for e in ("pe", "dve", "act", "pool"):
            self.sems[("e", e)] = stack.enter_context(nc.semaphore("s_" + e))
        for q in ("sp", "pool", "act"):
            for i in range(self.KD):
                self.sems[("d", q, i)] = stack.enter_context(nc.semaphore(f"d_{q}{i}"))
        self.dcnt = {q: 0 for q in ("sp", "pool", "act")}
        self.waited = {e: {} for e in self.ENG}
        self.state = {}
        self.bg = None
        self.bg_every = 12
        self._bgk = 0
        self._in_bg = False

    def _bg_step(self):
        self._in_bg = True
        try:
            next(self.bg)
        except StopIteration:
            self.bg = None
        self._in_bg = False

    def _tick(self):
        if self.bg is None or self._in_bg:
            return
        self._bgk += 1
        if self._bgk % self.bg_every == 0:
            self._bg_step()

    def drain(self):
        while self.bg is not None:
            self._bg_step()

    def _st(self, name, reg):
        d = self.state.setdefault(name, {})
        if reg not in d:
            d[reg] = {"w": None, "r": {}}
        return d[reg]

    def _states(self, name, reg):
        d = self.state.get(name, {})
        if reg is None:
            return list(d.values())
        out = []
        if reg in d:
            out.append(d[reg])
        if None in d:
            out.append(d[None])
        return out

    def _deps(self, reads, writes):
        evs = []
        for (name, reg) in reads:
            for st in self._states(name, reg):
                if st["w"] is not None:
                    evs.append(st["w"])
        for (name, reg) in writes:
            for st in self._states(name, reg):
                if st["w"] is not None:
                    evs.append(st["w"])
                evs.extend(st["r"].items())
        return evs

    def _commit(self, ev, reads, writes):
        for (name, reg) in reads:
            r = self._st(name, reg)["r"]
            if r.get(ev[0], 0) < ev[1]:
                r[ev[0]] = ev[1]
        for (name, reg) in writes:
            if reg is None:
                self.state[name] = {None: {"w": ev, "r": {}}}
            else:
                st = self._st(name, reg)
                st["w"] = ev
                st["r"] = {}

    def _waits(self, eng, evs):
        out = {}
        w = self.waited[eng]
        for (sk, v) in evs:
            if sk == ("e", "pe") and eng == "pe":
                continue
            if w.get(sk, 0) >= v:
                continue
            if out.get(sk, 0) < v:
                out[sk] = v
        for sk, v in out.items():
            w[sk] = v
        return list(out.items())

    @staticmethod
    def _keys(ks):
        return [(k, None) if isinstance(k, str) else (k[0], k[1]) for k in ks]

    def op(self, eng, fn, reads=(), writes=()):
        reads = self._keys(reads)
        writes = self._keys(writes)
        waits = self._waits(eng, self._deps(reads, writes))
        self.cnt[eng] += 1
        ev = (("e", eng), self.cnt[eng])
        self.ops[eng].append((waits, fn, ev, 1))
        self._commit(ev, reads, writes)
        self._tick()

    def dma(self, q, fn, reads=(), writes=()):
        reads = self._keys(reads)
        writes = self._keys(writes)
        n = self.dcnt[q]
        self.dcnt[q] += 1
        i = n % self.KD
        val = 16 * (n // self.KD + 1)
        evs = self._deps(reads, writes)
        if n >= self.KD:
            evs.append((("d", q, i), val - 16))
        waits = self._waits(q, evs)
        ev = (("d", q, i), val)
        self.ops[q].append((waits, fn, ev, 16))
        self._commit(ev, reads, writes)
        self._tick()

    def _all_events(self):
        evs = []
        for q in ("sp", "pool", "act"):
            n = self.dcnt[q]
            for i in range(self.KD):
                if n > i:
                    evs.append((("d", q, i), 16 * ((n - i + self.KD - 1) // self.KD)))
        for e in ("pe", "dve", "act", "pool"):
            if self.cnt[e]:
                evs.append((("e", e), self.cnt[e]))
        return evs

    def barrier(self):
        evs = self._all_events()
        for eng in self.ENG:
            waits = self._waits(eng, [ev for ev in evs if ev[0] != ("e", eng)])
            if waits:
                self.ops[eng].append((waits, None, None, 0))
        self.state = {}

    def emit(self):
        nc = self.nc
        sems = self.sems
        ops = self.ops
        self.ops = {e: [] for e in self.ENG}

        def run(name, e):
            for waits, fn, ev, inc in ops[name]:
                for (sk, v) in waits:
                    e.wait_ge(sems[sk], v)
                if fn is None:
                    continue
                fn(e).then_inc(sems[ev[0]], inc)

        with nc.Block() as block:
            @block.tensor
            def _(e):
                run("pe", e)

            @block.vector
            def _(e):
                run("dve", e)

            @block.scalar
            def _(e):
                run("act", e)

            @block.gpsimd
            def _(e):
                run("pool", e)

            @block.sync
            def _(e):
                run("sp", e)


def build_nc(debug=False, stop_after=None, depth=DEPTH, n_exp=NE, sparse=True):
    nc = bass.Bass("TRN2", target_bir_lowering=False)
    I = lambda n, s, d=F32: nc.dram_tensor(n, s, d, kind="ExternalInput").ap()
    xin = I("xin", [D, S]); cvec = I("cvec", [128, 8, 2]); pos = I("pos", [D, LX])
    w_mod = I("w_mod", [DEPTH, D, 6 * D]); pvd = I("pv", [DEPTH, 128, NPV]); bdd = I("bd", [DEPTH, 128, 12, 128])
    w_in = I("w_in", [DEPTH, D, 1536]); w_pw = I("w_pw", [DEPTH, 256, 256]); w_out = I("w_out", [DEPTH, D, D])
    w_router = I("w_router", [DEPTH, D, NE]); b_router = I("b_router", [DEPTH, NE])
    w_gu = I("w_gu", [DEPTH, n_exp, D, 2 * D]); w_down = I("w_down", [DEPTH, n_exp, D, D]); b_down = I("b_down", [DEPTH, NE, D])
    cstd = I("cst", [128, 6, 128]); dftx = I("dftx", [2, LX // 256, 128, LX // 128, 256], BF16); dftc = I("dftc", [2, LC // 256, 128, LC // 128, 256], BF16)
    invcd = I("invc", [2, 128, SP]); iotd = I("iot", [128, 33 + NBLK]); bgt_d = [I(f"bgt{i}", [NE * 128, 16]) for i in range(DEPTH)]
    outT = nc.dram_tensor("outT", [D, LX], F32, kind="ExternalOutput").ap()
    sk = "ExternalOutput" if debug else "Internal"
    res = nc.dram_tensor("res", [D, S], F32, kind=sk).ap()
    proj = nc.dram_tensor("proj", [1536, S], F32, kind=sk).ap()
    ycat = nc.dram_tensor("ycat", [D, S], BF16, kind=sk).ap()
    hT = nc.dram_tensor("hT", [D, S], BF16, kind=sk).ap()
    gTd = nc.dram_tensor("gTd", [NE, S], F32, kind=sk).ap()
    WGbs = [nc.dram_tensor(f"WGb{i}", [NE, 128, 8, 2048], BF16).ap() for i in range(DEPTH)]
    WDbs = [nc.dram_tensor(f"WDb{i}", [NE, 128, 8, 1024], BF16).ap() for i in range(DEPTH)]
    htm = nc.dram_tensor("htm", [S, D], BF16, kind=sk).ap(); xs_d = nc.dram_tensor("xs_d", [NSLOT, D], BF16, kind=sk).ap()
    ys_d = nc.dram_tensor("ys_d", [NSLOT, D], F32, kind=sk).ap()

    with contextlib.ExitStack() as top:
        P = Prog(nc, top)
        PS = [top.enter_context(nc.psum_tensor(f"ps{i}", [128, 512], F32)) for i in range(8)]
        psn = [f"ps{i}" for i in range(8)]

        _uid = [0]

        def SB(st, n, s, d=F32):
            _uid[0] += 1
            return st.enter_context(nc.sbuf_tensor(f"sb{_uid[0]}_{n}", s, d))

        cst = SB(top, "cst", [128, 6, 128]); cstb = SB(top, "cstb", [128, 6, 128], BF16)
        iot = SB(top, "iot", [128, 33 + NBLK])
        P.dma("sp", lambda e: e.dma_start(out=iot[:], in_=iotd), writes=["iot"])
        def ind_dma(e, **kw):
            return e.indirect_dma_start(**kw)

        rB = top.enter_context(nc.gpsimd.register("rB"))
        vbox = {}
        dmy = SB(top, "dmy", [128, 8])

        def init_pool(e):
            e.reg_mov(rB, NE * 128 - 1)
            vbox["v"] = e.snap(rB, donate=True)
            return e.memset(dmy[:], 0.0)

        P.op("pool", init_pool, writes=["dmy"])

        def precast_gen(l):
            WGb, WDb = WGbs[l], WDbs[l]
            for ex_ in range(n_exp):
                P.dma("pool", lambda e, ex_=ex_: e.dma_start(out=WGb[ex_, :, :, :], in_=w_gu[l, ex_].rearrange("(k p) f -> p k f", p=128)))
                yield
                P.dma("pool", lambda e, ex_=ex_: e.dma_start(out=WDb[ex_, :, :, :], in_=w_down[l, ex_].rearrange("(k p) f -> p k f", p=128)))
                yield

        if sparse and stop_after is None:
            P.bg = precast_gen(0)
            P.bg_every = 100
        P.dma("sp", lambda e: e.dma_start(out=cst[:], in_=cstd), writes=["cst"])
        P.op("dve", lambda e: e.tensor_copy(out=cstb[:], in_=cst[:]), reads=["cst"], writes=["cstb"])
        C64b, S64b, MAVb, ONEb, IDb, UTb = (cstb[:, i, :] for i in range(6))
        ID32 = cst[:, 4, :]
        cv = SB(top, "cv", [128, 8, 2])
        P.dma("sp", lambda e: e.dma_start(out=cv[:], in_=cvec), writes=["cv"])
        P.op("act", lambda e: e.activation(out=cv[:], in_=cv[:], func=AF.Silu), reads=["cv"], writes=["cv"])
        P.barrier(); P.emit()

        def rms_rstd(st_r, rk, sqb, sqk, n, psi, rstd, rstdk):
            P.op("act", lambda e: e.activation(out=sqb[:, :, :n], in_=st_r[:, :, :n], func=AF.Square), reads=[rk], writes=[sqk])
            for k in range(8):
                P.op("pe", lambda e, k=k: e.matmul(PS[psi][:, :n], lhsT=ONEb, rhs=sqb[:, k, :n], start=(k == 0), stop=(k == 7)),
                     reads=[sqk, "cstb"], writes=[psn[psi]])
            P.op("dve", lambda e: e.tensor_scalar(out=rstd[:, :n], in0=PS[psi][:, :n], scalar1=1.0 / D, scalar2=EPS, op0=ALU.mult, op1=ALU.add),
                 reads=[psn[psi]], writes=[rstdk])
            P.op("act", lambda e: e.activation(out=rstd[:, :n], in_=rstd[:, :n], func=AF.Sqrt), reads=[rstdk], writes=[rstdk])
            P.op("dve", lambda e: e.reciprocal(out=rstd[:, :n], in_=rstd[:, :n]), reads=[rstdk], writes=[rstdk])

        for l in range(depth):
            last = (l == DEPTH - 1)
            with contextlib.ExitStack() as lay:
                pv = SB(lay, "pvt", [128, NPV]); modT = SB(lay, "modT", [128, 48, 2]); A1 = SB(lay, "A1", [128, 8, 2]); A2 = SB(lay, "A2", [128, 8, 2])
                P.dma("sp", lambda e: e.dma_start(out=pv[:], in_=pvd[l]), writes=["pvt"])
                pc = lambda name, i=0: pv[:, PV[name] + i:PV[name] + i + 1]
                NT = S // 128
                idxf = SB(lay, "idxf", [128, NT, 4]); g4 = SB(lay, "g4", [128, NT, 4]); rank4 = SB(lay, "rank4", [128, NT, 4]); carry = SB(lay, "carry", [128, NE])
                destu = SB(lay, "destu", [128, NT * 4], U32); widx = SB(lay, "widx", [128, NBLK], U32)
                WGb, WDb = WGbs[l], WDbs[l]
                with contextlib.ExitStack() as ph:
                    wm = [SB(ph, f"wm{i}", [128, 8, 768]) for i in range(2)]
                    mrow = SB(ph, "mrow", [2, 6 * D])
                    for q in range(8):
                        b = q % 2
                        P.dma("sp", lambda e, b=b, q=q: e.dma_start(out=wm[b][:], in_=w_mod[l].rearrange("(k p) n -> p k n", p=128)[:, :, q * 768:(q + 1) * 768]), writes=[f"wm{b}"])
                        for hh in range(2):
                            pi = 1 + hh
                            for k in range(8):
                                P.op("pe", lambda e, b=b, hh=hh, pi=pi, k=k: e.matmul(PS[pi][0:2, 0:384], lhsT=cv[:, k, :], rhs=wm[b][:, k, hh * 384:(hh + 1) * 384], start=(k == 0), stop=(k == 7)),
                                     reads=[f"wm{b}", "cv"], writes=[psn[pi]])
                            P.op("act", lambda e, q=q, hh=hh, pi=pi: e.activation(out=mrow[:, q * 768 + hh * 384:q * 768 + (hh + 1) * 384], in_=PS[pi][0:2, 0:384], func=AF.Copy), reads=[psn[pi]], writes=["mrow"])
                    for j in range(48):
                        P.op("pe", lambda e, j=j: e.transpose(out=PS[0][:, 2 * j:2 * j + 2], in_=mrow[0:2, j * 128:(j + 1) * 128], identity=cst[0:2, 4, 0:2]), reads=["mrow", "cst"], writes=["ps0"])
                    psm = PS[0][:, 0:96].rearrange("p (j w) -> p j w", w=2)
                    for w in range(2):
                        P.op("dve", lambda e, w=w: e.tensor_tensor(out=modT[:, :, w], in0=psm[:, :, w], in1=pv[:, PV["b_mod"]:PV["b_mod"] + 48], op=ALU.add), reads=["ps0", "pvt"], writes=["modT"])
                        for (A, sc0, gn) in ((A1, 8, "n1g"), (A2, 32, "n2g")):
                            P.op("dve", lambda e, w=w, A=A, sc0=sc0: e.tensor_scalar(out=A[:, :, w], in0=modT[:, sc0:sc0 + 8, w], scalar1=1.0, scalar2=None, op0=ALU.add), reads=["modT"], writes=["A"])
                            P.op("dve", lambda e, w=w, A=A, gn=gn: e.tensor_tensor(out=A[:, :, w], in0=A[:, :, w], in1=pv[:, PV[gn]:PV[gn] + 8], op=ALU.mult), reads=["A", "pvt"], writes=["A"])
                    P.barrier(); P.emit()
                SH1 = lambda k, w: modT[:, k, w:w + 1]
                G1 = lambda k, w: modT[:, 16 + k, w:w + 1]
                SH2 = lambda k, w: modT[:, 24 + k, w:w + 1]
                G2 = lambda k, w: modT[:, 40 + k, w:w + 1]

                with contextlib.ExitStack() as ph:
                    wst = SB(ph, "wst", [128, 8, 1536]); wib = SB(ph, "wib", [128, 8, 1536], BF16)
                    P.dma("sp", lambda e: e.dma_start(out=wst[:], in_=w_in[l].rearrange("(k p) n -> p k n", p=128)), writes=["wst"])
                    for k in range(8):
                        P.op("pool", lambda e, k=k: e.tensor_copy(out=wib[:, k, :], in_=wst[:, k, :]), reads=["wst"], writes=[("wib", k)])
                    rb = [SB(ph, f"r{i}", [128, 8, 512]) for i in range(2)]
                    posb = [SB(ph, f"posb{i}", [128, 8, 512]) for i in range(2)] if l == 0 else None
                    sqb = SB(ph, "sqb", [128, 8, 512], BF16); tmp = SB(ph, "tmp", [128, 8, 512])
                    ub = [SB(ph, f"u{i}", [128, 8, 512], BF16) for i in range(2)]
                    rstd = SB(ph, "rstd", [128, 512]); ot = [SB(ph, f"ot{i}", [128, 512]) for i in range(4)]
                    oc = 0
                    for ci, (s0, n) in enumerate(CH):
                        b = ci % 2; w = 1 if ci == 0 else 0
                        if l == 0:
                            P.dma("sp", lambda e, b=b, s0=s0, n=n: e.dma_start(out=rb[b][:, :, :n], in_=xin.rearrange("(k p) s -> p k s", p=128)[:, :, s0:s0 + n]), writes=[f"r{b}"])
                            if ci > 0:
                                P.dma("act", lambda e, b=b, s0=s0, n=n: e.dma_start(out=posb[b][:, :, :n], in_=pos.rearrange("(k p) s -> p k s", p=128)[:, :, s0 - LC:s0 - LC + n]), writes=[f"posb{b}"])
                                P.op("pool", lambda e, b=b, n=n: e.tensor_tensor(out=rb[b][:, :, :n], in0=rb[b][:, :, :n], in1=posb[b][:, :, :n], op=ALU.add), reads=[f"r{b}", f"posb{b}"], writes=[f"r{b}"])
                            P.dma("act", lambda e, b=b, s0=s0, n=n: e.dma_start(out=res.rearrange("(k p) s -> p k s", p=128)[:, :, s0:s0 + n], in_=rb[b][:, :, :n]), reads=[f"r{b}"])
                        else:
                            P.dma("sp", lambda e, b=b, s0=s0, n=n: e.dma_start(out=rb[b][:, :, :n], in_=res.rearrange("(k p) s -> p k s", p=128)[:, :, s0:s0 + n]), writes=[f"r{b}"])
                        rms_rstd(rb[b], f"r{b}", sqb, "sqb", n, 7, rstd, "rstd")
                        for k in range(8):
                            P.op("dve", lambda e, b=b, k=k, n=n: e.tensor_tensor(out=tmp[:, k, :n], in0=rb[b][:, k, :n], in1=rstd[:, :n], op=ALU.mult), reads=[f"r{b}", "rstd"], writes=[("tmp", k)])
                            P.op("act", lambda e, b=b, k=k, n=n, w=w: e.activation(out=ub[b][:, k, :n], in_=tmp[:, k, :n], func=AF.Identity, scale=A1[:, k, w:w + 1], bias=SH1(k, w)), reads=[("tmp", k), "A", "modT"], writes=[(f"u{b}", k)])
                        for fc in range(12):
                            pi = fc % 4
                            for k in range(8):
                                P.op("pe", lambda e, b=b, k=k, fc=fc, pi=pi, n=n: e.matmul(PS[pi][:, :n], lhsT=wib[:, k, fc * 128:(fc + 1) * 128], rhs=ub[b][:, k, :n], start=(k == 0), stop=(k == 7)),
                                     reads=[(f"u{b}", k), ("wib", k)], writes=[psn[pi]])
                            o = oc % 4; oc += 1
                            P.op("act", lambda e, o=o, pi=pi, fc=fc, n=n: e.activation(out=ot[o][:, :n], in_=PS[pi][:, :n], func=AF.Identity, bias=pc("b_in", fc)), reads=[psn[pi], "pvt"], writes=[f"ot{o}"])
                            P.dma("sp", lambda e, o=o, fc=fc, s0=s0, n=n: e.dma_start(out=proj[fc * 128:(fc + 1) * 128, s0:s0 + n], in_=ot[o][:, :n]), reads=[f"ot{o}"])
                    P.barrier(); P.emit()
                if stop_after == "inproj":
                    break

                PCH = [(pcol(s0), n) for (s0, n) in CH]
                mixs = contextlib.ExitStack()
                bdst = SB(mixs, "bdst", [128, 12, 128]); bdb = SB(mixs, "bdb", [128, 12, 128], BF16)
                P.dma("sp", lambda e: e.dma_start(out=bdst[:], in_=bdd[l]), writes=["bdst"])
                P.op("dve", lambda e: e.tensor_copy(out=bdb[:], in_=bdst[:]), reads=["bdst"], writes=["bdb"])

                def load_pad(t, tk, row0, eng="sp"):
                    P.op("pool", lambda e: e.memset(t[:], 0.0), writes=[tk])
                    P.dma(eng, lambda e: e.dma_start(out=t[:, PADW:PADW + LC], in_=proj[row0:row0 + 128, 0:LC]), writes=[tk])
                    P.dma(eng, lambda e: e.dma_start(out=t[:, 2 * PADW + LC:2 * PADW + S], in_=proj[row0:row0 + 128, LC:S]), writes=[tk])

                def store_seg(t, tk, row0):
                    P.dma("sp", lambda e: e.dma_start(out=ycat[row0:row0 + 128, 0:LC], in_=t[:, PADW:PADW + LC]), reads=[tk])
                    P.dma("sp", lambda e: e.dma_start(out=ycat[row0:row0 + 128, LC:S], in_=t[:, 2 * PADW + LC:2 * PADW + S]), reads=[tk])

                with contextlib.ExitStack() as ph:
                    xa = SB(ph, "xa", [128, SP]); xc = SB(ph, "xc", [128, SP]); xcb = SB(ph, "xcb", [128, SP], BF16)
                    rg = SB(ph, "rg", [128, SP]); ig = SB(ph, "ig", [128, SP]); aa = SB(ph, "aa", [128, SP]); bt = SB(ph, "bt", [128, SP])
                    hh = [SB(ph, f"hh{i}", [128, SP]) for i in range(2)]; ga = rg; yab = SB(ph, "yab", [128, SP], BF16)
                    sm = SB(ph, "sm", [128, 4])
                    c0, c1 = PADW, SP - PADW
                    for cg in range(2):
                        load_pad(xa, "xa", cg * 128)
                        for d in range(2):
                            for j in range(4):
                                o = (j - 3) if d == 0 else (3 - j)
                                wj = pc("caw", (d * 4 + j) * 2 + cg)
                                if j == 0:
                                    P.op("dve", lambda e, o=o, wj=wj, d=d, cg=cg: e.tensor_scalar(out=xc[:, c0:c1], in0=xa[:, c0 + o:c1 + o], scalar1=wj, scalar2=pc("cab", d * 2 + cg), op0=ALU.mult, op1=ALU.add), reads=["xa", "pvt"], writes=["xc"])
                                else:
                                    P.op("dve", lambda e, o=o, wj=wj: e.scalar_tensor_tensor(out=xc[:, c0:c1], in0=xa[:, c0 + o:c1 + o], scalar=wj, in1=xc[:, c0:c1], op0=ALU.mult, op1=ALU.add), reads=["xa", "xc", "pvt"], writes=["xc"])
                            P.op("pool", lambda e: e.tensor_copy(out=xcb[:, c0:c1], in_=xc[:, c0:c1]), reads=["xc"], writes=["xcb"])
                            for gi, (gt_, gk, bn) in enumerate(((rg, "rg", "brr"), (ig, "ig", "bri"))):
                                for qi, (p0, n) in enumerate(PCH):
                                    pi = (gi * 9 + qi) % 4
                                    P.op("pe", lambda e, pi=pi, gi=gi, d=d, cg=cg, p0=p0, n=n: e.matmul(PS[pi][:, :n], lhsT=bdb[:, gi * 4 + d * 2 + cg, :], rhs=xcb[:, p0:p0 + n], start=True, stop=True), reads=["xcb", "bdb"], writes=[psn[pi]])
                                    P.op("act", lambda e, pi=pi, gt_=gt_, bn=bn, d=d, cg=cg, p0=p0, n=n: e.activation(out=gt_[:, p0:p0 + n], in_=PS[pi][:, :n], func=AF.Sigmoid, bias=pc(bn, d * 2 + cg)), reads=[psn[pi], "pvt"], writes=[(gk, qi)])
                            P.op("act", lambda e, d=d, cg=cg: e.activation(out=sm[:, 0:1], in_=pc("lam", d * 2 + cg), func=AF.Exp, scale=-1.0), reads=["pvt"], writes=["sm"])
                            P.op("dve", lambda e: e.tensor_scalar(out=sm[:, 0:1], in0=sm[:, 0:1], scalar1=1.0, scalar2=None, op0=ALU.add), reads=["sm"], writes=["sm"])
                            P.op("act", lambda e: e.activation(out=sm[:, 1:2], in_=sm[:, 0:1], func=AF.Ln), reads=["sm"], writes=["sm"])
                            P.op("dve", lambda e: e.tensor_scalar(out=sm[:, 2:3], in0=sm[:, 1:2], scalar1=-8.0, scalar2=None, op0=ALU.mult), reads=["sm"], writes=["sm"])
                            P.op("act", lambda e: e.activation(out=aa[:, c0:c1], in_=rg[:, c0:c1], func=AF.Exp, scale=sm[:, 2:3]), reads=["rg", "sm"], writes=["aa"])
                            P.op("pool", lambda e: e.tensor_tensor(out=bt[:, c0:c1], in0=aa[:, c0:c1], in1=aa[:, c0:c1], op=ALU.mult), reads=["aa"], writes=["bt"])
                            P.op("dve", lambda e: e.tensor_scalar(out=bt[:, c0:c1], in0=bt[:, c0:c1], scalar1=-1.0, scalar2=1.0, op0=ALU.mult, op1=ALU.add), reads=["bt"], writes=["bt"])
                            P.op("act", lambda e: e.activation(out=bt[:, c0:c1], in_=bt[:, c0:c1], func=AF.Sqrt), reads=["bt"], writes=["bt"])
                            P.op("pool", lambda e: e.tensor_tensor(out=ig[:, c0:c1], in0=ig[:, c0:c1], in1=xc[:, c0:c1], op=ALU.mult), reads=["ig", "xc"], writes=["ig"])
                            P.op("dve", lambda e: e.tensor_tensor(out=bt[:, c0:c1], in0=bt[:, c0:c1], in1=ig[:, c0:c1], op=ALU.mult), reads=["bt", "ig"], writes=["bt"])
                            h = hh[d]; hk = f"hh{d}"
                            sc_, sx_ = slice(PADW, PADW + LC), slice(2 * PADW + LC, 2 * PADW + S)
                            if d == 0:
                                P.op("dve", lambda e, h=h: e.tensor_tensor_scan(out=h[:, sc_], data0=aa[:, sc_], data1=bt[:, sc_], initial=0.0, op0=ALU.mult, op1=ALU.add), reads=["aa", "bt"], writes=[hk])
                                P.op("dve", lambda e, h=h: e.tensor_tensor_scan(out=h[:, sx_], data0=aa[:, sx_], data1=bt[:, sx_], initial=h[:, PADW + LC - 1:PADW + LC], op0=ALU.mult, op1=ALU.add), reads=["aa", "bt", hk], writes=[hk])
                            else:
                                P.op("dve", lambda e, h=h: e.tensor_tensor_scan(out=h[:, sc_][:, ::-1], data0=aa[:, sc_][:, ::-1], data1=bt[:, sc_][:, ::-1], initial=0.0, op0=ALU.mult, op1=ALU.add), reads=["aa", "bt"], writes=[hk])
                                P.op("dve", lambda e, h=h: e.tensor_tensor_scan(out=h[:, sx_][:, ::-1], data0=aa[:, sx_][:, ::-1], data1=bt[:, sx_][:, ::-1], initial=h[:, PADW:PADW + 1], op0=ALU.mult, op1=ALU.add), reads=["aa", "bt", hk], writes=[hk])
                        load_pad(ga, "rg", 256 + cg * 128)
                        P.op("pool", lambda e: e.tensor_tensor(out=hh[0][:, c0:c1], in0=hh[0][:, c0:c1], in1=hh[1][:, c0:c1], op=ALU.add), reads=["hh0", "hh1"], writes=["hh0"])
                        P.op("pool", lambda e: e.tensor_tensor(out=aa[:, c0:c1], in0=ga[:, c0:c1], in1=ga[:, c0:c1], op=ALU.mult), reads=["rg"], writes=["aa"])
                        P.op("dve", lambda e: e.tensor_scalar(out=aa[:, c0:c1], in0=aa[:, c0:c1], scalar1=0.044715, scalar2=1.0, op0=ALU.mult, op1=ALU.add), reads=["aa"], writes=["aa"])
                        P.op("pool", lambda e: e.tensor_tensor(out=aa[:, c0:c1], in0=aa[:, c0:c1], in1=ga[:, c0:c1], op=ALU.mult), reads=["aa", "rg"], writes=["aa"])
                        P.op("act", lambda e: e.activation(out=aa[:, c0:c1], in_=aa[:, c0:c1], func=AF.Sigmoid, scale=1.5957691216057308), reads=["aa"], writes=["aa"])
                        P.op("dve", lambda e: e.tensor_tensor(out=aa[:, c0:c1], in0=aa[:, c0:c1], in1=ga[:, c0:c1], op=ALU.mult), reads=["aa", "rg"], writes=["aa"])
                        P.op("dve", lambda e: e.tensor_tensor(out=yab[:, c0:c1], in0=aa[:, c0:c1], in1=hh[0][:, c0:c1], op=ALU.mult), reads=["aa", "hh0"], writes=["yab"])
                        store_seg(yab, "yab", cg * 128)
                    P.barrier(); P.emit()

                with contextlib.ExitStack() as ph:
                    xb = SB(ph, "xb", [128, SP]); wa = SB(ph, "wa", [128, SP]); wb = SB(ph, "wb", [128, SP]); ivc = SB(ph, "ivc", [128, SP])
                    pbf = SB(ph, "pbf", [128, SP], BF16); ob = [SB(ph, f"ob{i}", [128, 512], BF16) for i in range(2)]; sm = SB(ph, "smb", [128, 2])
                    for cg in range(2):
                        load_pad(xb, "xb", 512 + cg * 128)
                        P.dma("sp", lambda e, cg=cg: e.dma_start(out=ivc[:], in_=invcd[cg]), writes=["ivc"])
                        P.op("pool", lambda e: e.memset(wa[:], 0.0), writes=["wa"])
                        P.op("pool", lambda e: e.memset(wb[:], 0.0), writes=["wb"])
                        P.op("dve", lambda e: e.tensor_tensor(out=wa[:, 1:SP], in0=xb[:, 0:SP - 1], in1=xb[:, 1:SP], op=ALU.add), reads=["xb"], writes=["wa"])
                        P.op("dve", lambda e: e.tensor_tensor(out=wb[:, 1:SP - 1], in0=wa[:, 0:SP - 2], in1=wa[:, 2:SP], op=ALU.add), reads=["wa"], writes=["wb"])
                        if cg == 1:
                            P.op("dve", lambda e: e.tensor_tensor(out=wa[:, 3:SP - 3], in0=wb[:, 1:SP - 5], in1=wb[:, 5:SP - 1], op=ALU.add), reads=["wb"], writes=["wa"])
                            P.op("dve", lambda e: e.tensor_tensor(out=wb[:, 7:SP - 7], in0=wa[:, 3:SP - 11], in1=wa[:, 11:SP - 3], op=ALU.add), reads=["wa"], writes=["wb"])
                        c0, c1 = PADW, SP - PADW
                        P.op("dve", lambda e: e.tensor_tensor(out=wa[0:64, c0:c1], in0=wa[0:64, c0:c1], in1=ivc[0:64, c0:c1], op=ALU.mult), reads=["wa", "ivc"], writes=["wa"])
                        P.op("dve", lambda e: e.tensor_tensor(out=wa[64:128, c0:c1], in0=wb[64:128, c0:c1], in1=ivc[64:128, c0:c1], op=ALU.mult), reads=["wb", "wa", "ivc"], writes=["wa"])
                        P.op("dve", lambda e: e.tensor_tensor(out=pbf[:, c0:c1], in0=wa[:, c0:c1], in1=xb[:, c0:c1], op=ALU.subtract), reads=["wa", "xb"], writes=["pbf"])
                        P.op("dve", lambda e, cg=cg: e.tensor_tensor(out=sm[:, 0:1], in0=pc("bpool", cg), in1=pc("pscale", cg), op=ALU.mult), reads=["pvt"], writes=["smb"])
                        for qi, (p0, n) in enumerate(PCH):
                            pi = qi % 4; o = qi % 2; s0 = CH[qi][0]
                            P.op("pe", lambda e, pi=pi, cg=cg, p0=p0, n=n: e.matmul(PS[pi][:, :n], lhsT=bdb[:, 8 + cg, :], rhs=pbf[:, p0:p0 + n], start=True, stop=True), reads=["pbf", "bdb"], writes=[psn[pi]])
                            P.op("act", lambda e, pi=pi, o=o, cg=cg, n=n: e.activation(out=ob[o][:, :n], in_=PS[pi][:, :n], func=AF.Identity, scale=pc("pscale", cg), bias=sm[:, 0:1]), reads=[psn[pi], "pvt", "smb"], writes=[f"ob{o}"])
                            P.dma("sp", lambda e, o=o, cg=cg, s0=s0, n=n: e.dma_start(out=ycat[256 + cg * 128:256 + (cg + 1) * 128, s0:s0 + n], in_=ob[o][:, :n]), reads=[f"ob{o}"])
                    P.barrier(); P.emit()

                with contextlib.ExitStack() as ph:
                    xs = SB(ph, "xs", [128, 1, LX]); xsb = SB(ph, "xsb", [128, 2, LX], BF16)
                    XCS = SB(ph, "XCS", [128, 32, 512], BF16)
                    TB = [[SB(ph, f"tb{i}{j}", [128, 32, 256], BF16) for j in range(2)] for i in range(2)]
                    fb = [SB(ph, f"fb{i}", [128, 512], BF16) for i in range(2)]; ob = [SB(ph, f"oc{i}", [128, 512], BF16) for i in range(2)]
                    it = 0
                    for (s0, L, tab) in ((0, LC, dftc), (LC, LX, dftx)):
                        nlt = L // 128
                        for cg in range(2):
                            P.dma("sp", lambda e, cg=cg, s0=s0, L=L: e.dma_start(out=xs[:, 0, :L], in_=proj[768 + cg * 128:768 + (cg + 1) * 128, s0:s0 + L]), writes=["xs"])
                            P.op("pool", lambda e, cg=cg, L=L: e.tensor_copy(out=xsb[:, cg, :L], in_=xs[:, 0, :L]), reads=["xs"], writes=[("xsb", cg)])
                        for lt in range(nlt):
                            pi = lt % 2
                            for q, (cg, M) in enumerate(((0, C64b), (1, C64b), (0, S64b), (1, S64b))):
                                P.op("pe", lambda e, pi=pi, q=q, cg=cg, M=M, lt=lt: e.matmul(PS[pi][:, q * 128:(q + 1) * 128], lhsT=xsb[:, cg, lt * 128:(lt + 1) * 128], rhs=M, start=True, stop=True), reads=[("xsb", cg), "cstb"], writes=[psn[pi]])
                            eng = "act" if lt % 2 == 0 else "dve"
                            if eng == "act":
                                P.op("act", lambda e, pi=pi, lt=lt: e.activation(out=XCS[:, lt, :], in_=PS[pi][:, :], func=AF.Copy), reads=[psn[pi]], writes=[("XCS", lt)])
                            else:
                                P.op("dve", lambda e, pi=pi, lt=lt: e.tensor_copy(out=XCS[:, lt, :], in_=PS[pi][:, :]), reads=[psn[pi]], writes=[("XCS", lt)])
                        n = 256
                        nk = L // n
                        for kc in range(nk):
                            tb = TB[it % 2]; tk = f"tb{it % 2}"; it += 1
                            for j in range(2):
                                P.dma("sp" if j == 0 else "act", lambda e, tb=tb, j=j, kc=kc, nlt=nlt, n=n, tab=tab: e.dma_start(out=tb[j][:, :nlt, :n], in_=tab[j, kc]), writes=[tk + str(j)])
                            for cg in range(2):
                                pi = 2 + (kc * 2 + cg) % 2
                                for j in range(2):
                                    for lt in range(nlt):
                                        P.op("pe", lambda e, pi=pi, tb=tb, j=j, lt=lt, cg=cg, n=n, nlt=nlt: e.matmul(PS[pi][:, :n], lhsT=XCS[:, lt, j * 256 + cg * 128:j * 256 + (cg + 1) * 128], rhs=tb[j][:, lt, :n], start=(j == 0 and lt == 0), stop=(j == 1 and lt == nlt - 1)),
                                             reads=[("XCS", lt), tk + str(j)], writes=[psn[pi]])
                                o = (kc * 2 + cg) % 2
                                P.op("dve", lambda e, pi=pi, o=o, n=n: e.tensor_copy(out=fb[o][:, :n], in_=PS[pi][:, :n]), reads=[psn[pi]], writes=[f"fb{o}"])
                                P.op("pe", lambda e, o=o, cg=cg, n=n: e.matmul(PS[4 + o][:, :n], lhsT=bdb[:, 10 + cg, :], rhs=fb[o][:, :n], start=True, stop=True), reads=[f"fb{o}", "bdb"], writes=[psn[4 + o]])
                                P.op("act", lambda e, o=o, cg=cg, n=n: e.activation(out=ob[o][:, :n], in_=PS[4 + o][:, :n], func=AF.Identity, bias=pc("bfour", cg)), reads=[psn[4 + o], "pvt"], writes=[f"oc{o}"])
                                P.dma("sp", lambda e, o=o, cg=cg, s0=s0, kc=kc, n=n: e.dma_start(out=ycat[512 + cg * 128:512 + (cg + 1) * 128, s0 + kc * n:s0 + (kc + 1) * n], in_=ob[o][:, :n]), reads=[f"oc{o}"])
                    P.barrier(); P.emit()

                with contextlib.ExitStack() as ph:
                    xv = SB(ph, "xv", [128, SP]); xg = SB(ph, "xg", [128, SP]); vb = [SB(ph, f"vb{i}", [128, SP], BF16) for i in range(2)]
                    dg = [SB(ph, f"dg{i}", [128, 31, 128], BF16) for i in range(2)]
                    wpst = SB(ph, "wpst", [128, 2, 256]); wpb = SB(ph, "wpb", [128, 2, 256], BF16)
                    vc = SB(ph, "vc", [128, 512]); vcb = SB(ph, "vcb", [128, 512], BF16); cen = SB(ph, "cen", [128, 512]); sq2 = SB(ph, "sq2", [128, 512], BF16)
                    rs2 = SB(ph, "rs2", [128, 512]); sg2 = SB(ph, "sg2", [128, 512]); svb = [SB(ph, f"svb{i}", [128, 512], BF16) for i in range(2)]
                    ob = [SB(ph, f"od{i}", [128, 512], BF16) for i in range(2)]
                    P.dma("sp", lambda e: e.dma_start(out=wpst[:], in_=w_pw[l].rearrange("(k p) n -> p k n", p=128)), writes=["wpst"])
                    P.op("dve", lambda e: e.tensor_copy(out=wpb[:], in_=wpst[:]), reads=["wpst"], writes=["wpb"])
                    for cg in range(2):
                        load_pad(xv, "xv", 1024 + cg * 128)
                        load_pad(xg, "xg", 1280 + cg * 128)
                        P.op("act", lambda e: e.activation(out=xg[:], in_=xg[:], func=AF.Sigmoid), reads=["xg"], writes=["xg"])
                        P.op("dve", lambda e, cg=cg: e.tensor_tensor(out=vb[cg][:], in0=xv[:], in1=xg[:], op=ALU.mult), reads=["xv", "xg"], writes=[f"vb{cg}"])
                        for j in range(31):
                            P.op("dve", lambda e, cg=cg, j=j: e.tensor_scalar(out=dg[cg][:, j, :], in0=ID32, scalar1=pc("cdw", j * 2 + cg), scalar2=None, op0=ALU.mult), reads=["cst", "pvt"], writes=[f"dg{cg}"])
                    for qi, (p0, n) in enumerate(PCH):
                        s0 = CH[qi][0]
                        for cg in range(2):
                            for j in range(31):
                                P.op("pe", lambda e, cg=cg, j=j, p0=p0, n=n: e.matmul(PS[cg][:, :n], lhsT=dg[cg][:, j, :], rhs=vb[cg][:, p0 + j - 15:p0 + j - 15 + n], start=(j == 0), stop=(j == 30)), reads=[f"vb{cg}", f"dg{cg}"], writes=[psn[cg]])
                            P.op("act", lambda e, cg=cg, n=n: e.activation(out=vc[:, :n], in_=PS[cg][:, :n], func=AF.Identity, bias=pc("cdb", cg)), reads=[psn[cg], "pvt"], writes=["vc"])
                            P.op("pool", lambda e, n=n: e.tensor_copy(out=vcb[:, :n], in_=vc[:, :n]), reads=["vc"], writes=["vcb"])
                            P.op("pe", lambda e, n=n: e.matmul(PS[2][:, :n], lhsT=MAVb, rhs=vcb[:, :n], start=True, stop=True), reads=["vcb", "cstb"], writes=["ps2"])
                            P.op("dve", lambda e, n=n: e.tensor_tensor(out=cen[:, :n], in0=vc[:, :n], in1=PS[2][:, :n], op=ALU.subtract), reads=["vc", "ps2"], writes=["cen"])
                            P.op("act", lambda e, n=n: e.activation(out=sq2[:, :n], in_=cen[:, :n], func=AF.Square), reads=["cen"], writes=["sq2"])
                            P.op("pe", lambda e, n=n: e.matmul(PS[3][:, :n], lhsT=MAVb, rhs=sq2[:, :n], start=True, stop=True), reads=["sq2", "cstb"], writes=["ps3"])
                            P.op("dve", lambda e, n=n: e.tensor_scalar(out=rs2[:, :n], in0=PS[3][:, :n], scalar1=EPS, scalar2=None, op0=ALU.add), reads=["ps3"], writes=["rs2"])
                            P.op("act", lambda e, n=n: e.activation(out=rs2[:, :n], in_=rs2[:, :n], func=AF.Sqrt), reads=["rs2"], writes=["rs2"])
                            P.op("dve", lambda e, n=n: e.reciprocal(out=rs2[:, :n], in_=rs2[:, :n]), reads=["rs2"], writes=["rs2"])
                            P.op("dve", lambda e, n=n: e.tensor_tensor(out=cen[:, :n], in0=cen[:, :n], in1=rs2[:, :n], op=ALU.mult), reads=["cen", "rs2"], writes=["cen"])
                            P.op("act", lambda e, n=n, cg=cg: e.activation(out=cen[:, :n], in_=cen[:, :n], func=AF.Identity, scale=pc("lng", cg), bias=pc("lnb", cg)), reads=["cen", "pvt"], writes=["cen"])
                            P.op("act", lambda e, n=n: e.activation(out=sg2[:, :n], in_=cen[:, :n], func=AF.Sigmoid), reads=["cen"], writes=["sg2"])
                            P.op("dve", lambda e, n=n, cg=cg: e.tensor_tensor(out=svb[cg][:, :n], in0=cen[:, :n], in1=sg2[:, :n], op=ALU.mult), reads=["cen", "sg2"], writes=[f"svb{cg}"])
                        for oc_ in range(2):
                            for cg in range(2):
                                P.op("pe", lambda e, oc_=oc_, cg=cg, n=n: e.matmul(PS[4 + oc_][:, :n], lhsT=wpb[:, cg, oc_ * 128:(oc_ + 1) * 128], rhs=svb[cg][:, :n], start=(cg == 0), stop=(cg == 1)), reads=[f"svb{cg}", "wpb"], writes=[psn[4 + oc_]])
                            P.op("act", lambda e, oc_=oc_, n=n: e.activation(out=ob[oc_][:, :n], in_=PS[4 + oc_][:, :n], func=AF.Identity, bias=pc("bpw", oc_)), reads=[psn[4 + oc_], "pvt"], writes=[f"od{oc_}"])
                            P.dma("sp", lambda e, oc_=oc_, s0=s0, n=n: e.dma_start(out=ycat[768 + oc_ * 128:768 + (oc_ + 1) * 128, s0:s0 + n], in_=ob[oc_][:, :n]), reads=[f"od{oc_}"])
                    P.barrier(); P.emit()
                mixs.close()
                if stop_after == "mix":
                    break

                chs = list(enumerate(CH))
                if last:
                    chs = chs[1:]
                with contextlib.ExitStack() as ph:
                    wob = SB(ph, "wob", [128, 8, 1024], BF16)
                    GT = SB(ph, "GT", [NE, S])
                    wr = SB(ph, "wr", [128, 8, NE]); brb = SB(ph, "brb", [128, NE]); bdn = SB(ph, "bdn", [NE, D])
                    P.dma("sp", lambda e: e.dma_start(out=wr[:], in_=w_router[l].rearrange("(k p) n -> p k n", p=128)), writes=["wr"])
                    P.dma("sp", lambda e: e.dma_start(out=brb[:], in_=b_router[l:l + 1, :].partition_broadcast(128)), writes=["brb"])
                    P.dma("sp", lambda e: e.dma_start(out=bdn[:], in_=b_down[l]), writes=["bdn"])
                    yc = [SB(ph, f"yc{i}", [128, 8, 512], BF16) for i in range(2)]
                    rb = [SB(ph, f"r{i}", [128, 8, 512]) for i in range(2)]
                    yt = SB(ph, "yt", [128, 512]); sqb = SB(ph, "sqb", [128, 8, 512], BF16); rstd = SB(ph, "rstd", [128, 512])
                    tmp = SB(ph, "tmp", [128, 8, 512]); h32 = SB(ph, "h32", [128, 8, 512]); hb = SB(ph, "hbw", [128, 8, 512], BF16)
                    for hf_ in range(2):
                        P.dma("sp", lambda e, hf_=hf_: e.dma_start(out=tmp[:], in_=w_out[l].rearrange("(k p) n -> p k n", p=128)[:, :, hf_ * 512:(hf_ + 1) * 512]), writes=["tmp"])
                        P.op("pool", lambda e, hf_=hf_: e.tensor_copy(out=wob[:, :, hf_ * 512:(hf_ + 1) * 512], in_=tmp[:]), reads=["tmp"], writes=["wob"])
                    i8 = SB(ph, "i8", [128, 8], U32); mkb = SB(ph, "mkb", [128, NE], BF16); Rk = SB(ph, "Rk", [128, NE]); oh = SB(ph, "oh", [128, NE])
                    e4 = SB(ph, "e4", [128, 4]); htb = [SB(ph, f"htb{i}", [128, D], BF16) for i in range(2)]
                    P.op("pool", lambda e: e.memset(carry[:], 0.0), writes=["carry"])
                    lg = SB(ph, "lg", [128, NE]); t8 = SB(ph, "t8", [128, 8]); ex = SB(ph, "ex", [128, NE]); mk = SB(ph, "mk", [128, NE]); s1 = SB(ph, "s1", [128, 4])
                    for ci, (s0, n) in chs:
                        b = ci % 2; w = 1 if ci == 0 else 0
                        P.dma("sp", lambda e, b=b, s0=s0, n=n: e.dma_start(out=yc[b][:, :, :n], in_=ycat.rearrange("(k p) s -> p k s", p=128)[:, :, s0:s0 + n]), writes=[f"yc{b}"])
                        P.dma("act", lambda e, b=b, s0=s0, n=n: e.dma_start(out=rb[b][:, :, :n], in_=res.rearrange("(k p) s -> p k s", p=128)[:, :, s0:s0 + n]), writes=[f"r{b}"])
                        for dc in range(8):
                            pi = dc % 4
                            for k in range(8):
                                P.op("pe", lambda e, b=b, pi=pi, dc=dc, k=k, n=n: e.matmul(PS[pi][:, :n], lhsT=wob[:, k, dc * 128:(dc + 1) * 128], rhs=yc[b][:, k, :n], start=(k == 0), stop=(k == 7)), reads=[f"yc{b}", "wob"], writes=[psn[pi]])
                            P.op("act", lambda e, pi=pi, dc=dc, n=n: e.activation(out=yt[:, :n], in_=PS[pi][:, :n], func=AF.Identity, bias=pc("b_out", dc)), reads=[psn[pi], "pvt"], writes=["yt"])
                            P.op("dve", lambda e, b=b, dc=dc, n=n, w=w: e.scalar_tensor_tensor(out=rb[b][:, dc, :n], in0=yt[:, :n], scalar=G1(dc, w), in1=rb[b][:, dc, :n], op0=ALU.mult, op1=ALU.add), reads=["yt", "modT", f"r{b}"], writes=[f"r{b}"])
                        rms_rstd(rb[b], f"r{b}", sqb, "sqb", n, 7, rstd, "rstd")
                        for k in range(8):
                            P.op("dve", lambda e, b=b, k=k, n=n: e.tensor_tensor(out=tmp[:, k, :n], in0=rb[b][:, k, :n], in1=rstd[:, :n], op=ALU.mult), reads=[f"r{b}", "rstd"], writes=[("tmp", k)])
                            P.op("act", lambda e, k=k, n=n, w=w: e.activation(out=h32[:, k, :n], in_=tmp[:, k, :n], func=AF.Identity, scale=A2[:, k, w:w + 1], bias=SH2(k, w)), reads=[("tmp", k), "A", "modT"], writes=[("h32", k)])
                            P.op("pool", lambda e, k=k, n=n: e.tensor_copy(out=hb[:, k, :n], in_=h32[:, k, :n]), reads=[("h32", k)], writes=[("hbw", k)])
                        P.dma("sp", lambda e, s0=s0, n=n: e.dma_start(out=hT.rearrange("(k p) s -> p k s", p=128)[:, :, s0:s0 + n], in_=hb[:, :, :n]), reads=["hbw"])
                        for t0 in range(0, n, 128):
                            for k in range(8):
                                P.op("pe", lambda e, k=k, t0=t0: e.matmul(PS[4][:, 0:NE], lhsT=h32[:, k, t0:t0 + 128], rhs=wr[:, k, :], start=(k == 0), stop=(k == 7)), reads=[("h32", k), "wr"], writes=["ps4"])
                            P.op("dve", lambda e: e.tensor_tensor(out=lg[:], in0=PS[4][:, 0:NE], in1=brb[:], op=ALU.add), reads=["ps4", "brb"], writes=["lg"])
                            P.op("dve", lambda e: e.max(out=t8[:], in_=lg[:]), reads=["lg"], writes=["t8"])
                            P.op("dve", lambda e: e.tensor_scalar(out=s1[:, 0:1], in0=t8[:, 0:1], scalar1=-1.0, scalar2=None, op0=ALU.mult), reads=["t8"], writes=["s1"])
                            P.op("act", lambda e: e.activation(out=ex[:], in_=lg[:], func=AF.Exp, bias=s1[:, 0:1]), reads=["lg", "s1"], writes=["ex"])
                            P.op("dve", lambda e: e.tensor_scalar(out=mk[:], in0=lg[:], scalar1=t8[:, 3:4], scalar2=None, op0=ALU.is_ge), reads=["lg", "t8"], writes=["mk"])
                            P.op("dve", lambda e: e.tensor_tensor(out=ex[:], in0=ex[:], in1=mk[:], op=ALU.mult), reads=["ex", "mk"], writes=["ex"])
                            P.op("dve", lambda e: e.tensor_reduce(out=s1[:, 1:2], in_=ex[:], axis=AX.X, op=ALU.add), reads=["ex"], writes=["s1"])
                            P.op("dve", lambda e: e.reciprocal(out=s1[:, 2:3], in_=s1[:, 1:2]), reads=["s1"], writes=["s1"])
                            P.op("dve", lambda e: e.tensor_scalar(out=ex[:], in0=ex[:], scalar1=s1[:, 2:3], scalar2=None, op0=ALU.mult), reads=["ex", "s1"], writes=["ex"])
                            if sparse:
                                tg = (s0 + t0) // 128
                                P.op("dve", lambda e: e.max_index(out=i8[:], in_max=t8[:], in_values=lg[:]), reads=["lg", "t8"], writes=["i8"])
                                P.op("dve", lambda e, tg=tg: e.tensor_copy(out=idxf[:, tg, :], in_=i8[:, 0:4]), reads=["i8"], writes=["idxf"])
                                P.op("act", lambda e: e.activation(out=e4[:], in_=t8[:, 0:4], func=AF.Exp, bias=s1[:, 0:1]), reads=["t8", "s1"], writes=["e4"])
                                P.op("dve", lambda e: e.tensor_reduce(out=s1[:, 3:4], in_=e4[:], axis=AX.X, op=ALU.add), reads=["e4"], writes=["s1"])
                                P.op("dve", lambda e: e.reciprocal(out=s1[:, 3:4], in_=s1[:, 3:4]), reads=["s1"], writes=["s1"])
                                P.op("dve", lambda e, tg=tg: e.tensor_scalar(out=g4[:, tg, :], in0=e4[:], scalar1=s1[:, 3:4], scalar2=None, op0=ALU.mult), reads=["e4", "s1"], writes=["g4"])
                                P.op("pool", lambda e: e.tensor_copy(out=mkb[:], in_=mk[:]), reads=["mk"], writes=["mkb"])
                                P.op("pe", lambda e: e.matmul(PS[6][:, 0:NE], lhsT=UTb, rhs=mkb[:], start=True, stop=True), reads=["mkb", "cstb"], writes=["ps6"])
                                P.op("pe", lambda e: e.matmul(PS[6][:, 64:64 + NE], lhsT=ONEb, rhs=mkb[:], start=True, stop=True), reads=["mkb", "cstb"], writes=["ps6"])
                                P.op("dve", lambda e: e.tensor_tensor(out=Rk[:], in0=PS[6][:, 0:NE], in1=carry[:], op=ALU.add), reads=["ps6", "carry"], writes=["Rk"])
                                P.op("dve", lambda e: e.tensor_tensor(out=carry[:], in0=PS[6][:, 64:64 + NE], in1=carry[:], op=ALU.add), reads=["ps6", "carry"], writes=["carry"])
                                for k4 in range(4):
                                    P.op("dve", lambda e, tg=tg, k4=k4: e.tensor_scalar(out=oh[:], in0=iot[:, 0:NE], scalar1=idxf[:, tg, k4:k4 + 1], scalar2=None, op0=ALU.is_equal), reads=["iot", "idxf"], writes=["oh"])
                                    P.op("dve", lambda e: e.tensor_tensor(out=oh[:], in0=oh[:], in1=Rk[:], op=ALU.mult), reads=["oh", "Rk"], writes=["oh"])
                                    P.op("dve", lambda e, tg=tg, k4=k4: e.tensor_reduce(out=rank4[:, tg, k4:k4 + 1], in_=oh[:], axis=AX.X, op=ALU.add), reads=["oh"], writes=["rank4"])
                                hq = tg % 2
                                psb = PS[6][:].bitcast(BF16)
                                for k in range(8):
                                    P.op("pe", lambda e, k=k, t0=t0, psb=psb: e.transpose(out=psb[:, 256 + k * 96:256 + k * 96 + 128] if False else PS[3][:].bitcast(BF16)[:, k * 128:(k + 1) * 128], in_=hb[:, k, t0:t0 + 128], identity=IDb), reads=[("hbw", k), "cstb"], writes=["ps3"])
                                P.op("act", lambda e, hq=hq: e.activation(out=htb[hq][:], in_=PS[3][:].bitcast(BF16), func=AF.Copy), reads=["ps3"], writes=[f"htb{hq}"])
                                P.dma("act", lambda e, hq=hq, s0=s0, t0=t0: e.dma_start(out=htm[s0 + t0:s0 + t0 + 128, :], in_=htb[hq][:]), reads=[f"htb{hq}"])
                            P.op("pe", lambda e: e.transpose(out=PS[5][0:NE, 0:128], in_=ex[:], identity=ID32), reads=["ex", "cst"], writes=["ps5"])
                            P.op("act", lambda e, s0=s0, t0=t0: e.activation(out=GT[:, s0 + t0:s0 + t0 + 128], in_=PS[5][0:NE, 0:128], func=AF.Copy), reads=["ps5"], writes=[("GT", s0 + t0)])
                        for dc in range(8):
                            pi = 2 + dc % 2
                            P.op("pe", lambda e, pi=pi, dc=dc, s0=s0, n=n: e.matmul(PS[pi][:, :n], lhsT=bdn[:, dc * 128:(dc + 1) * 128], rhs=GT[:, s0:s0 + n], start=True, stop=True), reads=["GT", "bdn"], writes=[psn[pi]])
                            P.op("dve", lambda e, pi=pi, b=b, dc=dc, n=n, w=w: e.scalar_tensor_tensor(out=rb[b][:, dc, :n], in0=PS[pi][:, :n], scalar=G2(dc, w), in1=rb[b][:, dc, :n], op0=ALU.mult, op1=ALU.add), reads=[psn[pi], "modT", f"r{b}"], writes=[f"r{b}"])
                        P.dma("sp", lambda e, b=b, s0=s0, n=n: e.dma_start(out=res.rearrange("(k p) s -> p k s", p=128)[:, :, s0:s0 + n], in_=rb[b][:, :, :n]), reads=[f"r{b}"])
                        P.dma("sp", lambda e, s0=s0, n=n: e.dma_start(out=gTd[:, s0:s0 + n], in_=GT[:, s0:s0 + n]), reads=["GT"])
                    P.barrier(); P.emit()
                if stop_after == "outproj":
                    break

                if sparse:
                    nblk = (4 * 128 * len([1 for ci_, (s0_, n_) in chs for _ in range(n_ // 128)])) // BLK + NE
                    tiles = [(s0_ + t0_) // 128 for ci_, (s0_, n_) in chs for t0_ in range(0, n_, 128)]
                    P.drain()
                    with contextlib.ExitStack() as ph:
                        ci_t = SB(ph, "ci_t", [128, NE], I32); padf = SB(ph, "padf", [128, NE]); pend = SB(ph, "pend", [128, NE]); pst = SB(ph, "pst", [128, NE])
                        one32 = SB(ph, "one32", [128, NE]); bef = SB(ph, "bef", [128, NBLK]); oh = SB(ph, "oh2", [128, NE]); pst4 = SB(ph, "pst4", [128, NT, 4])
                        hl = [SB(ph, f"hl{i}", [128, D], BF16) for i in range(3)]
                        P.op("pool", lambda e: e.memset(one32[:], 1.0), writes=["one32"])
                        P.op("pool", lambda e: e.memset(pst4[:], 0.0), writes=["pst4"])
                        P.op("dve", lambda e: e.tensor_scalar(out=padf[:], in0=carry[:], scalar1=float(BLK - 1), scalar2=None, op0=ALU.add), reads=["carry"], writes=["padf"])
                        P.op("dve", lambda e: e.tensor_copy(out=ci_t[:], in_=padf[:]), reads=["padf"], writes=["ci_t"])
                        P.op("dve", lambda e: e.tensor_scalar(out=ci_t[:], in0=ci_t[:], scalar1=8, scalar2=8, op0=ALU.arith_shift_right, op1=ALU.logical_shift_left), reads=["ci_t"], writes=["ci_t"])
                        P.op("dve", lambda e: e.tensor_copy(out=padf[:], in_=ci_t[:]), reads=["ci_t"], writes=["padf"])
                        P.op("dve", lambda e: e.tensor_tensor_scan(out=pend[:], data0=one32[:], data1=padf[:], initial=0.0, op0=ALU.mult, op1=ALU.add), reads=["one32", "padf"], writes=["pend"])
                        P.op("dve", lambda e: e.tensor_tensor(out=pst[:], in0=pend[:], in1=padf[:], op=ALU.subtract), reads=["pend", "padf"], writes=["pst"])
                        P.op("pool", lambda e: e.memset(bef[:], 0.0), writes=["bef"])
                        for ex_ in range(NE):
                            P.op("dve", lambda e, ex_=ex_: e.scalar_tensor_tensor(out=bef[:], in0=iot[:, 32:32 + NBLK], scalar=pend[:, ex_:ex_ + 1], in1=bef[:], op0=ALU.is_ge, op1=ALU.add), reads=["iot", "pend", "bef"], writes=["bef"])
                        P.op("dve", lambda e: e.tensor_scalar(out=bef[:], in0=bef[:], scalar1=float(NE - 1), scalar2=None, op0=ALU.min), reads=["bef"], writes=["bef"])
                        skp = SB(ph, "skp", [128, NBLK])
                        P.op("dve", lambda e: e.tensor_tensor(out=skp[:, 2:NBLK], in0=bef[:, 2:NBLK], in1=bef[:, 0:NBLK - 2], op=ALU.is_equal), reads=["bef"], writes=["skp"])
                        P.op("dve", lambda e: e.tensor_scalar(out=bef[:], in0=bef[:], scalar1=128.0, scalar2=iot[:, 32 + NBLK:33 + NBLK], op0=ALU.mult, op1=ALU.add), reads=["bef", "iot"], writes=["bef"])
                        P.op("dve", lambda e: e.scalar_tensor_tensor(out=bef[:, 2:NBLK], in0=skp[:, 2:NBLK], scalar=1048576.0, in1=bef[:, 2:NBLK], op0=ALU.mult, op1=ALU.add), reads=["bef", "skp"], writes=["bef"])
                        P.op("dve", lambda e: e.tensor_copy(out=widx[:], in_=bef[:]), reads=["bef"], writes=["widx"])
                        tv = [SB(ph, f"tv{i}", [128, NT * 4]) for i in range(2)]
                        for ex_ in range(NE):
                            q_ = ex_ % 2
                            P.op("dve", lambda e, ex_=ex_, q_=q_: e.tensor_scalar(out=tv[q_][:], in0=idxf[:].rearrange("p t k -> p (t k)"), scalar1=float(ex_), scalar2=pst[:, ex_:ex_ + 1], op0=ALU.is_equal, op1=ALU.mult), reads=["idxf", "pst"], writes=[f"tv{q_}"])
                            P.op("pool", lambda e, q_=q_: e.tensor_tensor(out=pst4[:].rearrange("p t k -> p (t k)"), in0=pst4[:].rearrange("p t k -> p (t k)"), in1=tv[q_][:], op=ALU.add), reads=["pst4", f"tv{q_}"], writes=["pst4"])
                        P.op("dve", lambda e: e.tensor_tensor(out=pst4[:], in0=pst4[:], in1=rank4[:], op=ALU.add), reads=["pst4", "rank4"], writes=["pst4"])
                        P.op("dve", lambda e: e.tensor_copy(out=destu[:], in_=pst4[:].rearrange("p t k -> p (t k)")), reads=["pst4"], writes=["destu"])
                        for i_, tg in enumerate(tiles):
                            hq = i_ % 3
                            P.dma("sp", lambda e, hq=hq, tg=tg: e.dma_start(out=hl[hq][:], in_=htm[tg * 128:(tg + 1) * 128, :]), writes=[f"hl{hq}"])
                            for k4 in range(4):
                                P.dma("pool", lambda e, hq=hq, tg=tg, k4=k4: ind_dma(e, out=xs_d, out_offset=bass.IndirectOffsetOnAxis(ap=destu[:, tg * 4 + k4:tg * 4 + k4 + 1], axis=0), in_=hl[hq][:, :], in_offset=None), reads=[f"hl{hq}", "destu"])
                        P.barrier(); P.emit()

                    with contextlib.ExitStack() as ph:
                        wg = [SB(ph, f"wg{i}", [128, 8, 2048], BF16) for i in range(2)]
                        bgc = [SB(ph, f"bgc{i}", [128, 16]) for i in range(2)]
                        wd = [SB(ph, f"wd{i}", [128, 8, 1024], BF16) for i in range(2)]
                        xsb = [SB(ph, f"xsb{i}", [128, 2, D], BF16) for i in range(2)]
                        xT = [SB(ph, f"xT{i}", [128, 8, BLK], BF16) for i in range(2)]
                        aT = [SB(ph, f"aT{i}", [128, 8, BLK], BF16) for i in range(2)]
                        ysb = [SB(ph, f"ysb{i}", [128, 2, D]) for i in range(2)]
                        gt = [SB(ph, f"gt{i}", [128, BLK]) for i in range(2)]; sg = [SB(ph, f"sg{i}", [128, BLK]) for i in range(2)]; up = [SB(ph, f"up{i}", [128, BLK]) for i in range(2)]
                        onr = SB(ph, "onr", [1, BLK], BF16)
                        P.op("pool", lambda e: e.memset(onr[:], 1.0), writes=["onr"])

                        def wload(b):
                            p_ = b % 2
                            P.dma("pool", lambda e: ind_dma(e, out=wg[p_][:].rearrange("p k f -> p (k f)"), out_offset=None, in_=WGb.rearrange("e p k f -> (e p) (k f)"), in_offset=bass.IndirectOffsetOnAxis(ap=widx[:, b:b + 1], axis=0), bounds_check=vbox["v"], oob_is_err=False), reads=["widx"], writes=[f"wg{p_}"])
                            P.dma("pool", lambda e: ind_dma(e, out=wd[p_][:].rearrange("p k f -> p (k f)"), out_offset=None, in_=WDb.rearrange("e p k f -> (e p) (k f)"), in_offset=bass.IndirectOffsetOnAxis(ap=widx[:, b:b + 1], axis=0), bounds_check=vbox["v"], oob_is_err=False), reads=["widx"], writes=[f"wd{p_}"])
                            P.dma("pool", lambda e: ind_dma(e, out=bgc[p_][:, :], out_offset=None, in_=bgt_d[l], in_offset=bass.IndirectOffsetOnAxis(ap=widx[:, b:b + 1], axis=0), bounds_check=vbox["v"], oob_is_err=False), reads=["widx"], writes=[f"bgc{p_}"])
                            P.dma("sp", lambda e: e.dma_start(out=xsb[p_][:], in_=xs_d[b * BLK:(b + 1) * BLK, :].rearrange("(j p) d -> p j d", p=128)), writes=[f"xsb{p_}"])

                        def block(b):
                            p_ = b % 2
                            if b + 1 < nblk:
                                wload(b + 1)
                            for j in range(2):
                                psb = PS[6 + j][:].bitcast(BF16)
                                for k in range(8):
                                    P.op("pe", lambda e, j=j, k=k, psb=psb: e.transpose(out=psb[:, k * 128:(k + 1) * 128], in_=xsb[p_][:, j, k * 128:(k + 1) * 128], identity=IDb), reads=[f"xsb{p_}", "cstb"], writes=[psn[6 + j]])
                                if j == 0:
                                    P.op("act", lambda e, psb=psb: e.activation(out=xT[p_][:, :, 0:128], in_=psb.rearrange("p (k t) -> p k t", t=128), func=AF.Copy), reads=[psn[6]], writes=[(f"xT{p_}", 0)])
                                else:
                                    P.op("dve", lambda e, psb=psb: e.tensor_copy(out=xT[p_][:, :, 128:256], in_=psb.rearrange("p (k t) -> p k t", t=128)), reads=[psn[7]], writes=[(f"xT{p_}", 1)])
                            for fc in range(8):
                                q = fc % 2
                                for hf in range(2):
                                    c0_ = hf * 1024 + fc * 128
                                    for k in range(8):
                                        P.op("pe", lambda e, q=q, hf=hf, c0_=c0_, k=k: e.matmul(PS[q][:, hf * BLK:(hf + 1) * BLK], lhsT=wg[p_][:, k, c0_:c0_ + 128], rhs=xT[p_][:, k, :], start=(k == 0), stop=(k == 7)), reads=[f"wg{p_}", f"xT{p_}"], writes=[psn[q]])
                                P.op("dve", lambda e, q=q, fc=fc: e.tensor_scalar(out=gt[q][:], in0=PS[q][:, 0:BLK], scalar1=bgc[p_][:, fc:fc + 1], scalar2=7.0, op0=ALU.add, op1=ALU.min), reads=[psn[q], f"bgc{p_}"], writes=[f"gt{q}"])
                                P.op("act", lambda e, q=q: e.activation(out=sg[q][:], in_=gt[q][:], func=AF.Sigmoid, scale=1.702), reads=[f"gt{q}"], writes=[f"sg{q}"])
                                P.op("act", lambda e, q=q, fc=fc: e.activation(out=up[q][:], in_=PS[q][:, BLK:2 * BLK], func=AF.Identity, bias=bgc[p_][:, 8 + fc:9 + fc]), reads=[psn[q], f"bgc{p_}"], writes=[f"up{q}"])
                                P.op("pool", lambda e, q=q: e.tensor_scalar(out=up[q][:], in0=up[q][:], scalar1=7.0, scalar2=-7.0, op0=ALU.min, op1=ALU.max), reads=[f"up{q}"], writes=[f"up{q}"])
                                P.op("pool", lambda e, q=q: e.tensor_tensor(out=gt[q][:], in0=gt[q][:], in1=sg[q][:], op=ALU.mult), reads=[f"gt{q}", f"sg{q}"], writes=[f"gt{q}"])
                                P.op("dve", lambda e, q=q, fc=fc: e.scalar_tensor_tensor(out=aT[p_][:, fc, :], in0=up[q][:], scalar=1.0, in1=gt[q][:], op0=ALU.add, op1=ALU.mult), reads=[f"up{q}", f"gt{q}"], writes=[(f"aT{p_}", fc)])
                            for j in range(2):
                                for dh in range(2):
                                    q = 2 + (j * 2 + dh) % 4
                                    for fc in range(8):
                                        P.op("pe", lambda e, q=q, j=j, dh=dh, fc=fc: e.matmul(PS[q][:, :], lhsT=aT[p_][:, fc, j * 128:(j + 1) * 128], rhs=wd[p_][:, fc, dh * 512:(dh + 1) * 512], start=(fc == 0), stop=(fc == 7)), reads=[(f"aT{p_}", fc), f"wd{p_}"], writes=[psn[q]])
                                    if dh == 0:
                                        P.op("act", lambda e, q=q, j=j, dh=dh: e.activation(out=ysb[p_][:, j, dh * 512:(dh + 1) * 512], in_=PS[q][:, :], func=AF.Copy), reads=[psn[q]], writes=[(f"ysb{p_}", j * 2 + dh)])
                                    else:
                                        P.op("pool" if False else "dve", lambda e, q=q, j=j, dh=dh: e.tensor_copy(out=ysb[p_][:, j, dh * 512:(dh + 1) * 512], in_=PS[q][:, :]), reads=[psn[q]], writes=[(f"ysb{p_}", j * 2 + dh)])
                            P.dma("sp", lambda e: e.dma_start(out=ys_d[b * BLK:(b + 1) * BLK, :].rearrange("(j p) d -> p j d", p=128), in_=ysb[p_][:]), reads=[f"ysb{p_}"])

                        if l + 1 < depth and stop_after is None:
                            P.bg = precast_gen(l + 1)
                            P.bg_every = 500
                        wload(0)
                        for b in range(nblk):
                            block(b)
                        P.bg_every = 100
                        P.barrier(); P.emit()

                    with contextlib.ExitStack() as ph:
                        gb = [[SB(ph, f"gb{i}{k}", [128, D]) for k in range(4)] for i in range(2)]
                        acc = [SB(ph, f"acc{i}", [128, D]) for i in range(4)]
                        sqbc = SB(ph, "sqbc", [128, 8, 512], BF16); rstdc = SB(ph, "rstdc", [128, 512])
                        rb = [SB(ph, f"r{i}", [128, 8, 512]) for i in range(2)]
                        it_ = 0
                        for ci, (s0, n) in chs:
                            b = ci % 2; w = 1 if ci == 0 else 0
                            P.dma("sp", lambda e, b=b, s0=s0, n=n: e.dma_start(out=rb[b][:, :, :n], in_=res.rearrange("(k p) s -> p k s", p=128)[:, :, s0:s0 + n]), writes=[f"r{b}"])
                            for ti, t0 in enumerate(range(0, n, 128)):
                                tg = (s0 + t0) // 128
                                gq = it_ % 2; it_ += 1
                                for k4 in range(4):
                                    P.dma("pool", lambda e, gq=gq, k4=k4, tg=tg: ind_dma(e, out=gb[gq][k4][:, :], out_offset=None, in_=ys_d, in_offset=bass.IndirectOffsetOnAxis(ap=destu[:, tg * 4 + k4:tg * 4 + k4 + 1], axis=0)), reads=["destu"], writes=[f"gb{gq}{k4}"])
                                P.op("dve", lambda e, gq=gq, ti=ti, tg=tg: e.tensor_scalar(out=acc[ti][:], in0=gb[gq][0][:], scalar1=g4[:, tg, 0:1], scalar2=None, op0=ALU.mult), reads=[f"gb{gq}0", "g4"], writes=[f"acc{ti}"])
                                for k4 in range(1, 4):
                                    P.op("dve", lambda e, gq=gq, ti=ti, tg=tg, k4=k4: e.scalar_tensor_tensor(out=acc[ti][:], in0=gb[gq][k4][:], scalar=g4[:, tg, k4:k4 + 1], in1=acc[ti][:], op0=ALU.mult, op1=ALU.add), reads=[f"gb{gq}{k4}", "g4", f"acc{ti}"], writes=[f"acc{ti}"])
                            for k in range(8):
                                q = k % 4
                                for ti, t0 in enumerate(range(0, n, 128)):
                                    P.op("pe", lambda e, q=q, k=k, ti=ti, t0=t0: e.transpose(out=PS[q][:, t0:t0 + 128], in_=acc[ti][:, k * 128:(k + 1) * 128], identity=ID32), reads=[f"acc{ti}", "cst"], writes=[psn[q]])
                                P.op("dve", lambda e, q=q, k=k, b=b, n=n, w=w: e.scalar_tensor_tensor(out=rb[b][:, k, :n], in0=PS[q][:, :n], scalar=G2(k, w), in1=rb[b][:, k, :n], op0=ALU.mult, op1=ALU.add), reads=[psn[q], "modT", f"r{b}"], writes=[f"r{b}"])
                            if last and stop_after is None:
                                rms_rstd(rb[b], f"r{b}", sqbc, "sqbc", n, 7, rstdc, "rstdc")
                                for k in range(8):
                                    P.op("dve", lambda e, b=b, k=k, n=n: e.scalar_tensor_tensor(out=rb[b][:, k, :n], in0=rb[b][:, k, :n], scalar=pc("fng", k), in1=rstdc[:, :n], op0=ALU.mult, op1=ALU.mult), reads=[f"r{b}", "rstdc", "pvt"], writes=[f"r{b}"])
                                P.dma("sp", lambda e, b=b, s0=s0, n=n: e.dma_start(out=outT.rearrange("(k p) s -> p k s", p=128)[:, :, s0 - LC:s0 - LC + n], in_=rb[b][:, :, :n]), reads=[f"r{b}"])
                            else:
                                P.dma("sp", lambda e, b=b, s0=s0, n=n: e.dma_start(out=res.rearrange("(k p) s -> p k s", p=128)[:, :, s0:s0 + n], in_=rb[b][:, :, :n]), reads=[f"r{b}"])
                        P.barrier(); P.emit()
                    continue

                with contextlib.ExitStack() as ph:
                    wg = [SB(ph, f"wg{i}", [128, 8, 2048], BF16) for i in range(2)]
                    wd = [SB(ph, f"wd{i}", [128, 8, 1024], BF16) for i in range(2)]
                    stg = [SB(ph, f"stg{i}", [128, 2048]) for i in range(3)]
                    hbuf = [SB(ph, f"hb{i}", [128, 8, 512], BF16) for i in range(2)]
                    act_ = SB(ph, "actT", [128, 8, 512], BF16)
                    gbc = [SB(ph, f"gbc{i}", [128, 512]) for i in range(2)]
                    gt = [SB(ph, f"gt{i}", [128, 512]) for i in range(2)]; sg = [SB(ph, f"sg{i}", [128, 512]) for i in range(2)]; up = [SB(ph, f"up{i}", [128, 512]) for i in range(2)]
                    rtb = [SB(ph, f"rt{i}", [128, 8, 512]) for i in range(2)]; ty = [SB(ph, f"ty{i}", [128, 512]) for i in range(2)]
                    scn = [0]

                    def wstep(ex_, k):
                        wb_ = ex_ % 2
                        s_ = scn[0] % 3; scn[0] += 1
                        if k < 8:
                            P.dma("act", lambda e: e.dma_start(out=stg[s_][:, :], in_=w_gu[l, ex_, k * 128:(k + 1) * 128, :]), writes=[f"stg{s_}"])
                            P.op("pool", lambda e: e.tensor_copy(out=wg[wb_][:, k, :], in_=stg[s_][:, :]), reads=[f"stg{s_}"], writes=[(f"wg{wb_}", k)])
                        else:
                            k2 = k - 8
                            P.dma("act", lambda e: e.dma_start(out=stg[s_][:, 0:1024], in_=w_down[l, ex_, k2 * 128:(k2 + 1) * 128, :]), writes=[f"stg{s_}"])
                            P.op("pool", lambda e: e.tensor_copy(out=wd[wb_][:, k2, :], in_=stg[s_][:, 0:1024]), reads=[f"stg{s_}"], writes=[(f"wd{wb_}", k2)])

                    work = [(ex_, j, ci, s0, n) for ex_ in range(n_exp) for j, (ci, (s0, n)) in enumerate(chs)]

                    def loads(i):
                        ex_, j, ci, s0, n = work[i]
                        b_ = i % 2
                        P.dma("sp", lambda e: e.dma_start(out=hbuf[b_][:, :, :n], in_=hT.rearrange("(k p) s -> p k s", p=128)[:, :, s0:s0 + n]), writes=[f"hb{b_}"])
                        P.dma("sp", lambda e: e.dma_start(out=gbc[b_][:, :n], in_=gTd[ex_:ex_ + 1, s0:s0 + n].partition_broadcast(128)), writes=[f"gbc{b_}"])
                        P.dma("sp", lambda e: e.dma_start(out=rtb[b_][:, :, :n], in_=res.rearrange("(k p) s -> p k s", p=128)[:, :, s0:s0 + n]), reads=[("res", ci)], writes=[f"rt{b_}"])

                    for k in range(16):
                        wstep(0, k)
                    loads(0)
                    nch = len(chs)
                    def compute(i):
                        ex_, j, ci, s0, n = work[i]
                        wb_ = ex_ % 2; b_ = i % 2
                        w = 1 if ci == 0 else 0
                        if ex_ + 1 < n_exp:
                            for k in range(16):
                                if k * nch // 16 == j:
                                    wstep(ex_ + 1, k)
                        if i + 1 < len(work):
                            loads(i + 1)
                        for fc in range(8):
                            q = fc % 2
                            for k in range(8):
                                P.op("pe", lambda e, q=q, fc=fc, k=k: e.matmul(PS[q][:, :n], lhsT=wg[wb_][:, k, fc * 128:(fc + 1) * 128], rhs=hbuf[b_][:, k, :n], start=(k == 0), stop=(k == 7)), reads=[(f"wg{wb_}", k), f"hb{b_}"], writes=[psn[q]])
                            for k in range(8):
                                P.op("pe", lambda e, q=q, fc=fc, k=k: e.matmul(PS[2 + q][:, :n], lhsT=wg[wb_][:, k, 1024 + fc * 128:1024 + (fc + 1) * 128], rhs=hbuf[b_][:, k, :n], start=(k == 0), stop=(k == 7)), reads=[(f"wg{wb_}", k), f"hb{b_}"], writes=[psn[2 + q]])
                            bg = pc("b_gu", ex_ * 16 + fc); bu = pc("b_gu", ex_ * 16 + 8 + fc)
                            P.op("dve", lambda e, q=q, bg=bg: e.tensor_scalar(out=gt[q][:, :n], in0=PS[q][:, :n], scalar1=bg, scalar2=7.0, op0=ALU.add, op1=ALU.min), reads=[psn[q], "pvt"], writes=[f"gt{q}"])
                            P.op("act", lambda e, q=q: e.activation(out=sg[q][:, :n], in_=gt[q][:, :n], func=AF.Sigmoid, scale=1.702), reads=[f"gt{q}"], writes=[f"sg{q}"])
                            P.op("act", lambda e, q=q, bu=bu: e.activation(out=up[q][:, :n], in_=PS[2 + q][:, :n], func=AF.Identity, bias=bu), reads=[psn[2 + q], "pvt"], writes=[f"up{q}"])
                            P.op("pool", lambda e, q=q: e.tensor_scalar(out=up[q][:, :n], in0=up[q][:, :n], scalar1=7.0, scalar2=-7.0, op0=ALU.min, op1=ALU.max), reads=[f"up{q}"], writes=[f"up{q}"])
                            P.op("pool", lambda e, q=q: e.tensor_tensor(out=gt[q][:, :n], in0=gt[q][:, :n], in1=sg[q][:, :n], op=ALU.mult), reads=[f"gt{q}", f"sg{q}"], writes=[f"gt{q}"])
                            P.op("dve", lambda e, q=q, fc=fc: e.scalar_tensor_tensor(out=act_[:, fc, :n], in0=up[q][:, :n], scalar=1.0, in1=gt[q][:, :n], op0=ALU.add, op1=ALU.mult), reads=[f"up{q}", f"gt{q}"], writes=[("actT", fc)])
                        for dc in range(8):
                            q = dc % 2
                            for fc in range(8):
                                P.op("pe", lambda e, q=q, dc=dc, fc=fc: e.matmul(PS[4 + q][:, :n], lhsT=wd[wb_][:, fc, dc * 128:(dc + 1) * 128], rhs=act_[:, fc, :n], start=(fc == 0), stop=(fc == 7)), reads=[(f"wd{wb_}", fc), ("actT", fc)], writes=[psn[4 + q]])
                            P.op("dve", lambda e, q=q, dc=dc: e.scalar_tensor_tensor(out=ty[q][:, :n], in0=PS[4 + q][:, :n], scalar=G2(dc, w), in1=gbc[b_][:, :n], op0=ALU.mult, op1=ALU.mult), reads=[psn[4 + q], "modT", f"gbc{b_}"], writes=[f"ty{q}"])
                            P.op("pool", lambda e, q=q, dc=dc: e.tensor_tensor(out=rtb[b_][:, dc, :n], in0=rtb[b_][:, dc, :n], in1=ty[q][:, :n], op=ALU.add), reads=[f"rt{b_}", f"ty{q}"], writes=[f"rt{b_}"])
                        P.dma("sp", lambda e: e.dma_start(out=res.rearrange("(k p) s -> p k s", p=128)[:, :, s0:s0 + n], in_=rtb[b_][:, :, :n]), reads=[f"rt{b_}"], writes=[("res", ci)])
                    for i in range(len(work)):
                        compute(i)
                    P.barrier(); P.emit()

        if stop_after is None and not (sparse and depth == DEPTH):
            with contextlib.ExitStack() as ph:
                pv = SB(ph, "pvf", [128, NPV])
                P.dma("sp", lambda e: e.dma_start(out=pv[:], in_=pvd[0]), writes=["pvf"])
                rb = [SB(ph, f"r{i}", [128, 8, 512]) for i in range(2)]; sqb = SB(ph, "sqb", [128, 8, 512], BF16); rstd = SB(ph, "rstd", [128, 512])
                ob = [SB(ph, f"of{i}", [128, 8, 512]) for i in range(2)]
                for ci, (s0, n) in list(enumerate(CH))[1:]:
                    b = ci % 2
                    P.dma("sp", lambda e, b=b, s0=s0, n=n: e.dma_start(out=rb[b][:, :, :n], in_=res.rearrange("(k p) s -> p k s", p=128)[:, :, s0:s0 + n]), writes=[f"r{b}"])
                    rms_rstd(rb[b], f"r{b}", sqb, "sqb", n, 7, rstd, "rstd")
                    for k in range(8):
                        P.op("dve", lambda e, b=b, k=k, n=n: e.scalar_tensor_tensor(out=ob[b][:, k, :n], in0=rb[b][:, k, :n], scalar=pv[:, PV["fng"] + k:PV["fng"] + k + 1], in1=rstd[:, :n], op0=ALU.mult, op1=ALU.mult), reads=[f"r{b}", "rstd", "pvf"], writes=[f"of{b}"])
                    P.dma("sp", lambda e, b=b, s0=s0, n=n: e.dma_start(out=outT.rearrange("(k p) s -> p k s", p=128)[:, :, s0 - LC:s0 - LC + n], in_=ob[b][:, :, :n]), reads=[f"of{b}"])
                P.barrier(); P.emit()
    return nc


_CONSTS = None


def make_in_maps(inputs, cores=range(8)):
    global _CONSTS
    if _CONSTS is None:
        _CONSTS = make_consts()
    c = _CONSTS
    f = lambda a: np.ascontiguousarray(np.asarray(a, np.float32))
    shared = dict(pos=c["pos"], w_mod=f(inputs["w_mod"]), pv=np.stack([pack_pv(inputs, l) for l in range(DEPTH)]),
                  bd=np.stack([pack_bd(inputs, l) for l in range(DEPTH)]), w_in=f(inputs["w_in"]), w_pw=f(inputs["w_pw"]),
                  w_out=f(inputs["w_out"]), w_router=f(inputs["w_router"]), b_router=f(inputs["b_router"]), w_gu=f(inputs["w_gu"]),
                  w_down=f(inputs["w_down"]), b_down=f(inputs["b_down"]),
                  **{f"bgt{i}": np.ascontiguousarray(f(inputs["b_gu"][i]).reshape(NE, 16, 128).transpose(0, 2, 1).reshape(NE * 128, 16)) for i in range(DEPTH)}, cst=c["cst"], dftx=c["dftx"], dftc=c["dftc"],
                  invc=c["invc"], iot=c["iot"])
    maps = []
    for b in cores:
        xin = np.ascontiguousarray(np.concatenate([inputs["ctx"][b], inputs["x"][b]], axis=0).T.astype(np.float32))
        cvec = np.stack([_cols(inputs["c"][b]), _cols(inputs["c_ctx"])], axis=-1)
        m = dict(shared)
        m["xin"] = xin
        m["cvec"] = np.ascontiguousarray(cvec.astype(np.float32))
        maps.append(m)
    return maps


def kernel(**inputs):
    inputs = {k: np.asarray(v) for k, v in inputs.items()}
    nc = build_nc()
    maps = make_in_maps(inputs)
    res = run_bass_kernel_spmd(nc, maps, core_ids=list(range(8)))
    out = np.stack([np.ascontiguousarray(r["outT"].T) for r in res.results], axis=0)
    return out.astype(np.float32)
```

```python
import contextlib
import numpy as np
import ml_dtypes
import concourse.bass as bass
import concourse.mybir as mybir
from concourse.bass_utils import run_bass_kernel_spmd

F32 = mybir.dt.float32
BF16 = mybir.dt.bfloat16
AF = mybir.ActivationFunctionType
ALU = mybir.AluOpType
AX = mybir.AxisListType

D = 1024
LC = 256
LX = 4096
S = LC + LX
PADW = 16
SP = S + 3 * PADW
NE = 32
DEPTH = 2
EPS = 1e-6
BLK = 256
NBLK = 100
NSLOT = NBLK * BLK
U32 = mybir.dt.uint32
I32 = mybir.dt.int32
CH = [(0, 256)] + [(256 + 512 * i, 512) for i in range(8)]


def pcol(s):
    return s + PADW if s < LC else s + 2 * PADW


PV = {}
_o = 0
for _n, _k in [("b_mod", 48), ("n1g", 8), ("n2g", 8), ("b_in", 12), ("caw", 16), ("cab", 4), ("brr", 4), ("bri", 4),
               ("lam", 4), ("bpool", 2), ("pscale", 2), ("bfour", 2), ("cdw", 62), ("cdb", 2), ("lng", 2), ("lnb", 2),
               ("bpw", 2), ("b_out", 8), ("b_gu", 512), ("fng", 8)]:
    PV[_n] = _o
    _o += _k
NPV = _o


def _cols(v):
    v = np.asarray(v, np.float32).reshape(-1)
    return v.reshape(-1, 128).T


def pack_pv(inp, l):
    pv = np.zeros((128, NPV), np.float32)

    def put(name, v, off=0):
        c = _cols(v)
        pv[:, PV[name] + off:PV[name] + off + c.shape[1]] = c

    put("b_mod", inp["b_mod"][l]); put("n1g", inp["norm1_g"][l]); put("n2g", inp["norm2_g"][l]); put("b_in", inp["b_in"][l])
    for d in range(2):
        for j in range(4):
            put("caw", inp["conv_a_w"][l, d, j], (d * 4 + j) * 2)
        put("cab", inp["conv_a_b"][l, d], d * 2)
        put("brr", inp["b_rg_r"][l, d], d * 2)
        put("bri", inp["b_rg_i"][l, d], d * 2)
        put("lam", inp["rg_lambda"][l, d], d * 2)
    put("bpool", inp["b_pool"][l]); put("pscale", inp["pool_scale"][l]); put("bfour", inp["b_four"][l])
    for j in range(31):
        put("cdw", inp["conv_d_w"][l, j], j * 2)
    put("cdb", inp["conv_d_b"][l]); put("lng", inp["ln_d_g"][l]); put("lnb", inp["ln_d_b"][l]); put("bpw", inp["b_pw"][l])
    put("b_out", inp["b_out"][l])
    for e in range(NE):
        put("b_gu", inp["b_gu"][l, e], e * 16)
    put("fng", inp["final_norm_g"])
    return pv


def pack_bd(inp, l):
    bd = np.zeros((128, 12, 128), np.float32)

    def blk(idx, w4, cg):
        for j in range(2):
            bd[64 * j:64 * j + 64, idx, 64 * j:64 * j + 64] = w4[2 * cg + j]

    for d in range(2):
        for cg in range(2):
            blk(d * 2 + cg, inp["w_rg_r"][l, d], cg)
            blk(4 + d * 2 + cg, inp["w_rg_i"][l, d], cg)
    for cg in range(2):
        blk(8 + cg, inp["w_pool"][l], cg)
        blk(10 + cg, inp["w_four"][l], cg)
    return bd


def make_consts():
    cst = np.zeros((128, 6, 128), np.float32)
    c = np.arange(64)
    ang = 2 * np.pi * np.outer(c, c) / 64.0
    for j in range(2):
        cst[64 * j:64 * j + 64, 0, 64 * j:64 * j + 64] = np.cos(ang) / 8.0
        cst[64 * j:64 * j + 64, 1, 64 * j:64 * j + 64] = -np.sin(ang) / 8.0
        cst[64 * j:64 * j + 64, 2, 64 * j:64 * j + 64] = 1.0 / 64.0
    cst[:, 3, :] = 1.0
    cst[:, 4, :] = np.eye(128)
    cst[:, 5, :] = np.triu(np.ones((128, 128), np.float32), 1)
    iot = np.zeros((128, 33 + NBLK), np.float32)
    iot[:, 32 + NBLK] = np.arange(128)
    iot[:, :32] = np.arange(32)[None, :]
    iot[:, 32:32 + NBLK] = (np.arange(NBLK) * BLK)[None, :]

    def dft(L):
        k = np.arange(L, dtype=np.int64)
        a = 2 * np.pi * ((np.outer(k, k) % L).astype(np.float64)) / L
        return np.stack([np.cos(a), np.sin(a)]).astype(np.float32) / np.sqrt(L)

    def tile_tab(t, L):
        nlt, nk = L // 128, L // 256
        return np.ascontiguousarray(t.reshape(2, nlt, 128, nk, 256).transpose(0, 3, 2, 1, 4))

    dftx = tile_tab(dft(LX), LX).astype(ml_dtypes.bfloat16)
    dftc = tile_tab(dft(LC), LC).astype(ml_dtypes.bfloat16)
    invc = np.ones((2, 128, SP), np.float32)
    for g, w in enumerate((2, 4, 8, 16)):
        for (c0, L) in ((PADW, LC), (2 * PADW + LC, LX)):
            t = np.arange(L)
            lo = np.clip(t - w // 2, 0, L)
            hi = np.clip(t + w - w // 2, 0, L)
            invc[g // 2, 64 * (g % 2):64 * (g % 2) + 64, c0:c0 + L] = 1.0 / (hi - lo)
    rows_n = LX // 64
    row = np.repeat(np.arange(rows_n), 64).astype(np.float32)
    col = np.tile(np.arange(64), rows_n).astype(np.float32)
    q = D // 4
    omega = (1.0 / (10000.0 ** (np.arange(q, dtype=np.float32) / q))).astype(np.float32)

    def emb(p):
        a = p[:, None] * omega[None, :]
        return np.concatenate([np.sin(a), np.cos(a)], axis=-1)

    pos = np.concatenate([emb(row), emb(col)], axis=-1).astype(np.float32)
    return dict(cst=cst, dftx=dftx, dftc=dftc, invc=invc, iot=iot, pos=np.ascontiguousarray(pos.T))


class Prog:
    ENG = ("pe", "dve", "act", "pool", "sp")
    KD = 8

    def __init__(self, nc, stack):
        self.nc = nc
        self.ops = {e: [] for e in self.ENG}
        self.cnt = {e: 0 for e in self.ENG}
        self.sems = {}
        for e in ("pe", "dve", "act", "pool"):
            self.sems[("e", e)] = stack.enter_context(nc.semaphore("s_" + e))
        for q in ("sp", "pool", "act"):
            for i in range(self.KD):
                self.sems[("d", q, i)] = stack.enter_context(nc.semaphore(f"d_{q}{i}"))
        self.dcnt = {q: 0 for q in ("sp", "pool", "act")}
        self.waited = {e: {} for e in self.ENG}
        self.state = {}
        self.bg = None
        self.bg_every = 12
        self._bgk = 0
        self._in_bg = False

    def _bg_step(self):
        self._in_bg = True
        try:
            next(self.bg)
        except StopIteration:
            self.bg = None
        self._in_bg = False

    def _tick(self):
        if self.bg is None or self._in_bg:
            return
        self._bgk += 1
        if self._bgk % self.bg_every == 0:
            self._bg_step()

    def drain(self):
        while self.bg is not None:
            self._bg_step()

    def _st(self, name, reg):
        d = self.state.setdefault(name, {})
        if reg not in d:
            d[reg] = {"w": None, "r": {}}
        return d[reg]

    def _states(self, name, reg):
        d = self.state.get(name, {})
        if reg is None:
            return list(d.values())
        out = []
        if reg in d:
            out.append(d[reg])
        if None in d:
            out.append(d[None])
        return out

    def _deps(self, reads, writes):
        evs = []
        for (name, reg) in reads:
            for st in self._states(name, reg):
                if st["w"] is not None:
                    evs.append(st["w"])
        for (name, reg) in writes:
            for st in self._states(name, reg):
                if st["w"] is not None:
                    evs.append(st["w"])
                evs.extend(st["r"].items())
        return evs

    def _commit(self, ev, reads, writes):
        for (name, reg) in reads:
            r = self._st(name, reg)["r"]
            if r.get(ev[0], 0) < ev[1]:
                r[ev[0]] = ev[1]
        for (name, reg) in writes:
            if reg is None:
                self.state[name] = {None: {"w": ev, "r": {}}}
            else:
                st = self._st(name, reg)
                st["w"] = ev
                st["r"] = {}

    def _waits(self, eng, evs):
        out = {}
        w = self.waited[eng]
        for (sk, v) in evs:
            if sk == ("e", "pe") and eng == "pe":
                continue
            if w.get(sk, 0) >= v:
                continue
            if out.get(sk, 0) < v:
                out[sk] = v
        for sk, v in out.items():
            w[sk] = v
        return list(out.items())

    @staticmethod
    def _keys(ks):
        return [(k, None) if isinstance(k, str) else (k[0], k[1]) for k in ks]

    def op(self, eng, fn, reads=(), writes=()):
        reads = self._keys(reads)
        writes = self._keys(writes)
        waits = self._waits(eng, self._deps(reads, writes))
        self.cnt[eng] += 1
        ev = (("e", eng), self.cnt[eng])
        self.ops[eng].append((waits, fn, ev, 1))
        self._commit(ev, reads, writes)
        self._tick()

    def dma(self, q, fn, reads=(), writes=()):
        reads = self._keys(reads)
        writes = self._keys(writes)
        n = self.dcnt[q]
        self.dcnt[q] += 1
        i = n % self.KD
        val = 16 * (n // self.KD + 1)
        evs = self._deps(reads, writes)
        if n >= self.KD:
            evs.append((("d", q, i), val - 16))
        waits = self._waits(q, evs)
        ev = (("d", q, i), val)
        self.ops[q].append((waits, fn, ev, 16))
        self._commit(ev, reads, writes)
        self._tick()

    def _all_events(self):
        evs = []
        for q in ("sp", "pool", "act"):
            n = self.dcnt[q]
            for i in range(self.KD):
                if n > i:
                    evs.append((("d", q, i), 16 * ((n - i + self.KD - 1) // self.KD)))
        for e in ("pe", "dve", "act", "pool"):
            if self.cnt[e]:
                evs.append((("e", e), self.cnt[e]))
        return evs

    def barrier(self):
        evs = self._all_events()
        for eng in self.ENG:
            waits = self._waits(eng, [ev for ev in evs if ev[0] != ("e", eng)])
            if waits:
                self.ops[eng].append((waits, None, None, 0))
        self.state = {}

    def emit(self):
        nc = self.nc
        sems = self.sems
        ops = self.ops
        self.ops = {e: [] for e in self.ENG}

        def run(name, e):
            for waits, fn, ev, inc in ops[name]:
                for (sk, v) in waits:
                    e.wait_ge(sems[sk], v)
                if fn is None:
                    continue
                fn(e).then_inc(sems[ev[0]], inc)

        with nc.Block() as block:
            @block.tensor
            def _(e):
                run("pe", e)

            @block.vector
            def _(e):
                run("dve", e)

            @block.scalar
            def _(e):
                run("act", e)

            @block.gpsimd
            def _(e):
                run("pool", e)

            @block.sync
            def _(e):
                run("sp", e)


def build_nc(debug=False, stop_after=None, depth=DEPTH, n_exp=NE, sparse=True):
    nc = bass.Bass("TRN2", target_bir_lowering=False)
    I = lambda n, s, d=F32: nc.dram_tensor(n, s, d, kind="ExternalInput").ap()
    xin = I("xin", [D, S]); cvec = I("cvec", [128, 8, 2]); pos = I("pos", [D, LX])
    w_mod = I("w_mod", [DEPTH, D, 6 * D]); pvd = I("pv", [DEPTH, 128, NPV]); bdd = I("bd", [DEPTH, 128, 12, 128])
    w_in = I("w_in", [DEPTH, D, 1536]); w_pw = I("w_pw", [DEPTH, 256, 256]); w_out = I("w_out", [DEPTH, D, D])
    w_router = I("w_router", [DEPTH, D, NE]); b_router = I("b_router", [DEPTH, NE])
    w_gu = I("w_gu", [DEPTH, n_exp, D, 2 * D]); w_down = I("w_down", [DEPTH, n_exp, D, D]); b_down = I("b_down", [DEPTH, NE, D])
    cstd = I("cst", [128, 6, 128]); dftx = I("dftx", [2, LX // 256, 128, LX // 128, 256], BF16); dftc = I("dftc", [2, LC // 256, 128, LC // 128, 256], BF16)
    invcd = I("invc", [2, 128, SP]); iotd = I("iot", [128, 33 + NBLK]); b_gu_d = I("b_gu", [DEPTH, NE, 2 * D])
    outT = nc.dram_tensor("outT", [D, LX], F32, kind="ExternalOutput").ap()
    sk = "ExternalOutput" if debug else "Internal"
    res = nc.dram_tensor("res", [D, S], F32, kind=sk).ap()
    proj = nc.dram_tensor("proj", [1536, S], F32, kind=sk).ap()
    ycat = nc.dram_tensor("ycat", [D, S], BF16, kind=sk).ap()
    hT = nc.dram_tensor("hT", [D, S], BF16, kind=sk).ap()
    gTd = nc.dram_tensor("gTd", [NE, S], F32, kind=sk).ap()
    WGbs = [nc.dram_tensor(f"WGb{i}", [NE, 128, 9, 2048], BF16).ap() for i in range(DEPTH)]
    WDbs = [nc.dram_tensor(f"WDb{i}", [NE, 128, 8, 1024], BF16).ap() for i in range(DEPTH)]
    htm = nc.dram_tensor("htm", [S, D], BF16, kind=sk).ap(); xs_d = nc.dram_tensor("xs_d", [NSLOT, D], BF16, kind=sk).ap()
    ys_d = nc.dram_tensor("ys_d", [NSLOT, D], F32, kind=sk).ap()

    with contextlib.ExitStack() as top:
        P = Prog(nc, top)
        PS = [top.enter_context(nc.psum_tensor(f"ps{i}", [128, 512], F32)) for i in range(8)]
        psn = [f"ps{i}" for i in range(8)]

        _uid = [0]

        def SB(st, n, s, d=F32):
            _uid[0] += 1
            return st.enter_context(nc.sbuf_tensor(f"sb{_uid[0]}_{n}", s, d))

        cst = SB(top, "cst", [128, 6, 128]); cstb = SB(top, "cstb", [128, 6, 128], BF16)
        iot = SB(top, "iot", [128, 33 + NBLK])
        P.dma("sp", lambda e: e.dma_start(out=iot[:], in_=iotd), writes=["iot"])
        def ind_dma(e, **kw):
            return e.indirect_dma_start(**kw)

        rB = top.enter_context(nc.gpsimd.register("rB"))
        vbox = {}
        dmy = SB(top, "dmy", [128, 8])

        def init_pool(e):
            e.reg_mov(rB, NE * 128 - 1)
            vbox["v"] = e.snap(rB, donate=True)
            return e.memset(dmy[:], 0.0)

        P.op("pool", init_pool, writes=["dmy"])

        def precast_gen(l):
            WGb, WDb = WGbs[l], WDbs[l]
            P.dma("pool", lambda e: e.dma_start(out=WGb[:, 0, 8, :], in_=b_gu_d[l]))
            yield
            for ex_ in range(n_exp):
                P.dma("pool", lambda e, ex_=ex_: e.dma_start(out=WGb[ex_, :, 0:8, :], in_=w_gu[l, ex_].rearrange("(k p) f -> p k f", p=128)))
                yield
                P.dma("pool", lambda e, ex_=ex_: e.dma_start(out=WDb[ex_, :, :, :], in_=w_down[l, ex_].rearrange("(k p) f -> p k f", p=128)))
                yield

        if sparse and stop_after is None:
            P.bg = precast_gen(0)
            P.bg_every = 100
        P.dma("sp", lambda e: e.dma_start(out=cst[:], in_=cstd), writes=["cst"])
        P.op("dve", lambda e: e.tensor_copy(out=cstb[:], in_=cst[:]), reads=["cst"], writes=["cstb"])
        C64b, S64b, MAVb, ONEb, IDb, UTb = (cstb[:, i, :] for i in range(6))
        ID32 = cst[:, 4, :]
        cv = SB(top, "cv", [128, 8, 2])
        P.dma("sp", lambda e: e.dma_start(out=cv[:], in_=cvec), writes=["cv"])
        P.op("act", lambda e: e.activation(out=cv[:], in_=cv[:], func=AF.Silu), reads=["cv"], writes=["cv"])
        P.barrier(); P.emit()

        def rms_rstd(st_r, rk, sqb, sqk, n, psi, rstd, rstdk):
            P.op("act", lambda e: e.activation(out=sqb[:, :, :n], in_=st_r[:, :, :n], func=AF.Square), reads=[rk], writes=[sqk])
            for k in range(8):
                P.op("pe", lambda e, k=k: e.matmul(PS[psi][:, :n], lhsT=ONEb, rhs=sqb[:, k, :n], start=(k == 0), stop=(k == 7)),
                     reads=[sqk, "cstb"], writes=[psn[psi]])
            P.op("dve", lambda e: e.tensor_scalar(out=rstd[:, :n], in0=PS[psi][:, :n], scalar1=1.0 / D, scalar2=EPS, op0=ALU.mult, op1=ALU.add),
                 reads=[psn[psi]], writes=[rstdk])
            P.op("act", lambda e: e.activation(out=rstd[:, :n], in_=rstd[:, :n], func=AF.Sqrt), reads=[rstdk], writes=[rstdk])
            P.op("dve", lambda e: e.reciprocal(out=rstd[:, :n], in_=rstd[:, :n]), reads=[rstdk], writes=[rstdk])

        for l in range(depth):
            last = (l == DEPTH - 1)
            with contextlib.ExitStack() as lay:
                pv = SB(lay, "pvt", [128, NPV]); modT = SB(lay, "modT", [128, 48, 2]); A1 = SB(lay, "A1", [128, 8, 2]); A2 = SB(lay, "A2", [128, 8, 2])
                P.dma("sp", lambda e: e.dma_start(out=pv[:], in_=pvd[l]), writes=["pvt"])
                pc = lambda name, i=0: pv[:, PV[name] + i:PV[name] + i + 1]
                NT = S // 128
                idxf = SB(lay, "idxf", [128, NT, 4]); g4 = SB(lay, "g4", [128, NT, 4]); rank4 = SB(lay, "rank4", [128, NT, 4]); carry = SB(lay, "carry", [128, NE])
                destu = SB(lay, "destu", [128, NT * 4], U32); widx = SB(lay, "widx", [128, NBLK], U32)
                WGb, WDb = WGbs[l], WDbs[l]
                with contextlib.ExitStack() as ph:
                    wm = [SB(ph, f"wm{i}", [128, 8, 768]) for i in range(2)]
                    mrow = SB(ph, "mrow", [2, 6 * D])
                    for q in range(8):
                        b = q % 2
                        P.dma("sp", lambda e, b=b, q=q: e.dma_start(out=wm[b][:], in_=w_mod[l].rearrange("(k p) n -> p k n", p=128)[:, :, q * 768:(q + 1) * 768]), writes=[f"wm{b}"])
                        for hh in range(2):
                            pi = 1 + hh
                            for k in range(8):
                                P.op("pe", lambda e, b=b, hh=hh, pi=pi, k=k: e.matmul(PS[pi][0:2, 0:384], lhsT=cv[:, k, :], rhs=wm[b][:, k, hh * 384:(hh + 1) * 384], start=(k == 0), stop=(k == 7)),
                                     reads=[f"wm{b}", "cv"], writes=[psn[pi]])
                            P.op("act", lambda e, q=q, hh=hh, pi=pi: e.activation(out=mrow[:, q * 768 + hh * 384:q * 768 + (hh + 1) * 384], in_=PS[pi][0:2, 0:384], func=AF.Copy), reads=[psn[pi]], writes=["mrow"])
                    for j in range(48):
                        P.op("pe", lambda e, j=j: e.transpose(out=PS[0][:, 2 * j:2 * j + 2], in_=mrow[0:2, j * 128:(j + 1) * 128], identity=cst[0:2, 4, 0:2]), reads=["mrow", "cst"], writes=["ps0"])
                    psm = PS[0][:, 0:96].rearrange("p (j w) -> p j w", w=2)
                    for w in range(2):
                        P.op("dve", lambda e, w=w: e.tensor_tensor(out=modT[:, :, w], in0=psm[:, :, w], in1=pv[:, PV["b_mod"]:PV["b_mod"] + 48], op=ALU.add), reads=["ps0", "pvt"], writes=["modT"])
                        for (A, sc0, gn) in ((A1, 8, "n1g"), (A2, 32, "n2g")):
                            P.op("dve", lambda e, w=w, A=A, sc0=sc0: e.tensor_scalar(out=A[:, :, w], in0=modT[:, sc0:sc0 + 8, w], scalar1=1.0, scalar2=None, op0=ALU.add), reads=["modT"], writes=["A"])
                            P.op("dve", lambda e, w=w, A=A, gn=gn: e.tensor_tensor(out=A[:, :, w], in0=A[:, :, w], in1=pv[:, PV[gn]:PV[gn] + 8], op=ALU.mult), reads=["A", "pvt"], writes=["A"])
                    P.barrier(); P.emit()
                SH1 = lambda k, w: modT[:, k, w:w + 1]
                G1 = lambda k, w: modT[:, 16 + k, w:w + 1]
                SH2 = lambda k, w: modT[:, 24 + k, w:w + 1]
                G2 = lambda k, w: modT[:, 40 + k, w:w + 1]

                with contextlib.ExitStack() as ph:
                    wst = SB(ph, "wst", [128, 8, 1536]); wib = SB(ph, "wib", [128, 8, 1536], BF16)
                    P.dma("sp", lambda e: e.dma_start(out=wst[:], in_=w_in[l].rearrange("(k p) n -> p k n", p=128)), writes=["wst"])
                    for k in range(8):
                        P.op("pool", lambda e, k=k: e.tensor_copy(out=wib[:, k, :], in_=wst[:, k, :]), reads=["wst"], writes=[("wib", k)])
                    rb = [SB(ph, f"r{i}", [128, 8, 512]) for i in range(2)]
                    posb = [SB(ph, f"posb{i}", [128, 8, 512]) for i in range(2)] if l == 0 else None
                    sqb = SB(ph, "sqb", [128, 8, 512], BF16); tmp = SB(ph, "tmp", [128, 8, 512])
                    ub = [SB(ph, f"u{i}", [128, 8, 512], BF16) for i in range(2)]
                    rstd = SB(ph, "rstd", [128, 512]); ot = [SB(ph, f"ot{i}", [128, 512]) for i in range(4)]
                    oc = 0
                    for ci, (s0, n) in enumerate(CH):
                        b = ci % 2; w = 1 if ci == 0 else 0
                        if l == 0:
                            P.dma("sp", lambda e, b=b, s0=s0, n=n: e.dma_start(out=rb[b][:, :, :n], in_=xin.rearrange("(k p) s -> p k s", p=128)[:, :, s0:s0 + n]), writes=[f"r{b}"])
                            if ci > 0:
                                P.dma("act", lambda e, b=b, s0=s0, n=n: e.dma_start(out=posb[b][:, :, :n], in_=pos.rearrange("(k p) s -> p k s", p=128)[:, :, s0 - LC:s0 - LC + n]), writes=[f"posb{b}"])
                                P.op("pool", lambda e, b=b, n=n: e.tensor_tensor(out=rb[b][:, :, :n], in0=rb[b][:, :, :n], in1=posb[b][:, :, :n], op=ALU.add), reads=[f"r{b}", f"posb{b}"], writes=[f"r{b}"])
                            P.dma("act", lambda e, b=b, s0=s0, n=n: e.dma_start(out=res.rearrange("(k p) s -> p k s", p=128)[:, :, s0:s0 + n], in_=rb[b][:, :, :n]), reads=[f"r{b}"])
                        else:
                            P.dma("sp", lambda e, b=b, s0=s0, n=n: e.dma_start(out=rb[b][:, :, :n], in_=res.rearrange("(k p) s -> p k s", p=128)[:, :, s0:s0 + n]), writes=[f"r{b}"])
                        rms_rstd(rb[b], f"r{b}", sqb, "sqb", n, 7, rstd, "rstd")
                        for k in range(8):
                            P.op("dve", lambda e, b=b, k=k, n=n: e.tensor_tensor(out=tmp[:, k, :n], in0=rb[b][:, k, :n], in1=rstd[:, :n], op=ALU.mult), reads=[f"r{b}", "rstd"], writes=[("tmp", k)])
                            P.op("act", lambda e, b=b, k=k, n=n, w=w: e.activation(out=ub[b][:, k, :n], in_=tmp[:, k, :n], func=AF.Identity, scale=A1[:, k, w:w + 1], bias=SH1(k, w)), reads=[("tmp", k), "A", "modT"], writes=[(f"u{b}", k)])
                        for fc in range(12):
                            pi = fc % 4
                            for k in range(8):
                                P.op("pe", lambda e, b=b, k=k, fc=fc, pi=pi, n=n: e.matmul(PS[pi][:, :n], lhsT=wib[:, k, fc * 128:(fc + 1) * 128], rhs=ub[b][:, k, :n], start=(k == 0), stop=(k == 7)),
                                     reads=[(f"u{b}", k), ("wib", k)], writes=[psn[pi]])
                            o = oc % 4; oc += 1
                            P.op("act", lambda e, o=o, pi=pi, fc=fc, n=n: e.activation(out=ot[o][:, :n], in_=PS[pi][:, :n], func=AF.Identity, bias=pc("b_in", fc)), reads=[psn[pi], "pvt"], writes=[f"ot{o}"])
                            P.dma("sp", lambda e, o=o, fc=fc, s0=s0, n=n: e.dma_start(out=proj[fc * 128:(fc + 1) * 128, s0:s0 + n], in_=ot[o][:, :n]), reads=[f"ot{o}"])
                    P.barrier(); P.emit()
                if stop_after == "inproj":
                    break

                PCH = [(pcol(s0), n) for (s0, n) in CH]
                mixs = contextlib.ExitStack()
                bdst = SB(mixs, "bdst", [128, 12, 128]); bdb = SB(mixs, "bdb", [128, 12, 128], BF16)
                P.dma("sp", lambda e: e.dma_start(out=bdst[:], in_=bdd[l]), writes=["bdst"])
                P.op("dve", lambda e: e.tensor_copy(out=bdb[:], in_=bdst[:]), reads=["bdst"], writes=["bdb"])

                def load_pad(t, tk, row0, eng="sp"):
                    P.op("pool", lambda e: e.memset(t[:], 0.0), writes=[tk])
                    P.dma(eng, lambda e: e.dma_start(out=t[:, PADW:PADW + LC], in_=proj[row0:row0 + 128, 0:LC]), writes=[tk])
                    P.dma(eng, lambda e: e.dma_start(out=t[:, 2 * PADW + LC:2 * PADW + S], in_=proj[row0:row0 + 128, LC:S]), writes=[tk])

                def store_seg(t, tk, row0):
                    P.dma("sp", lambda e: e.dma_start(out=ycat[row0:row0 + 128, 0:LC], in_=t[:, PADW:PADW + LC]), reads=[tk])
                    P.dma("sp", lambda e: e.dma_start(out=ycat[row0:row0 + 128, LC:S], in_=t[:, 2 * PADW + LC:2 * PADW + S]), reads=[tk])

                with contextlib.ExitStack() as ph:
                    xa = SB(ph, "xa", [128, SP]); xc = SB(ph, "xc", [128, SP]); xcb = SB(ph, "xcb", [128, SP], BF16)
                    rg = SB(ph, "rg", [128, SP]); ig = SB(ph, "ig", [128, SP]); aa = SB(ph, "aa", [128, SP]); bt = SB(ph, "bt", [128, SP])
                    hh = [SB(ph, f"hh{i}", [128, SP]) for i in range(2)]; ga = rg; yab = SB(ph, "yab", [128, SP], BF16)
                    sm = SB(ph, "sm", [128, 4])
                    c0, c1 = PADW, SP - PADW
                    for cg in range(2):
                        load_pad(xa, "xa", cg * 128)
                        for d in range(2):
                            for j in range(4):
                                o = (j - 3) if d == 0 else (3 - j)
                                wj = pc("caw", (d * 4 + j) * 2 + cg)
                                if j == 0:
                                    P.op("dve", lambda e, o=o, wj=wj, d=d, cg=cg: e.tensor_scalar(out=xc[:, c0:c1], in0=xa[:, c0 + o:c1 + o], scalar1=wj, scalar2=pc("cab", d * 2 + cg), op0=ALU.mult, op1=ALU.add), reads=["xa", "pvt"], writes=["xc"])
                                else:
                                    P.op("dve", lambda e, o=o, wj=wj: e.scalar_tensor_tensor(out=xc[:, c0:c1], in0=xa[:, c0 + o:c1 + o], scalar=wj, in1=xc[:, c0:c1], op0=ALU.mult, op1=ALU.add), reads=["xa", "xc", "pvt"], writes=["xc"])
                            P.op("pool", lambda e: e.tensor_copy(out=xcb[:, c0:c1], in_=xc[:, c0:c1]), reads=["xc"], writes=["xcb"])
                            for gi, (gt_, gk, bn) in enumerate(((rg, "rg", "brr"), (ig, "ig", "bri"))):
                                for qi, (p0, n) in enumerate(PCH):
                                    pi = (gi * 9 + qi) % 4
                                    P.op("pe", lambda e, pi=pi, gi=gi, d=d, cg=cg, p0=p0, n=n: e.matmul(PS[pi][:, :n], lhsT=bdb[:, gi * 4 + d * 2 + cg, :], rhs=xcb[:, p0:p0 + n], start=True, stop=True), reads=["xcb", "bdb"], writes=[psn[pi]])
                                    P.op("act", lambda e, pi=pi, gt_=gt_, bn=bn, d=d, cg=cg, p0=p0, n=n: e.activation(out=gt_[:, p0:p0 + n], in_=PS[pi][:, :n], func=AF.Sigmoid, bias=pc(bn, d * 2 + cg)), reads=[psn[pi], "pvt"], writes=[(gk, qi)])
                            P.op("act", lambda e, d=d, cg=cg: e.activation(out=sm[:, 0:1], in_=pc("lam", d * 2 + cg), func=AF.Exp, scale=-1.0), reads=["pvt"], writes=["sm"])
                            P.op("dve", lambda e: e.tensor_scalar(out=sm[:, 0:1], in0=sm[:, 0:1], scalar1=1.0, scalar2=None, op0=ALU.add), reads=["sm"], writes=["sm"])
                            P.op("act", lambda e: e.activation(out=sm[:, 1:2], in_=sm[:, 0:1], func=AF.Ln), reads=["sm"], writes=["sm"])
                            P.op("dve", lambda e: e.tensor_scalar(out=sm[:, 2:3], in0=sm[:, 1:2], scalar1=-8.0, scalar2=None, op0=ALU.mult), reads=["sm"], writes=["sm"])
                            P.op("act", lambda e: e.activation(out=aa[:, c0:c1], in_=rg[:, c0:c1], func=AF.Exp, scale=sm[:, 2:3]), reads=["rg", "sm"], writes=["aa"])
                            P.op("pool", lambda e: e.tensor_tensor(out=bt[:, c0:c1], in0=aa[:, c0:c1], in1=aa[:, c0:c1], op=ALU.mult), reads=["aa"], writes=["bt"])
                            P.op("dve", lambda e: e.tensor_scalar(out=bt[:, c0:c1], in0=bt[:, c0:c1], scalar1=-1.0, scalar2=1.0, op0=ALU.mult, op1=ALU.add), reads=["bt"], writes=["bt"])
                            P.op("act", lambda e: e.activation(out=bt[:, c0:c1], in_=bt[:, c0:c1], func=AF.Sqrt), reads=["bt"], writes=["bt"])
                            P.op("pool", lambda e: e.tensor_tensor(out=ig[:, c0:c1], in0=ig[:, c0:c1], in1=xc[:, c0:c1], op=ALU.mult), reads=["ig", "xc"], writes=["ig"])
                            P.op("dve", lambda e: e.tensor_tensor(out=bt[:, c0:c1], in0=bt[:, c0:c1], in1=ig[:, c0:c1], op=ALU.mult), reads=["bt", "ig"], writes=["bt"])
                            h = hh[d]; hk = f"hh{d}"
                            sc_, sx_ = slice(PADW, PADW + LC), slice(2 * PADW + LC, 2 * PADW + S)
                            if d == 0:
                                P.op("dve", lambda e, h=h: e.tensor_tensor_scan(out=h[:, sc_], data0=aa[:, sc_], data1=bt[:, sc_], initial=0.0, op0=ALU.mult, op1=ALU.add), reads=["aa", "bt"], writes=[hk])
                                P.op("dve", lambda e, h=h: e.tensor_tensor_scan(out=h[:, sx_], data0=aa[:, sx_], data1=bt[:, sx_], initial=h[:, PADW + LC - 1:PADW + LC], op0=ALU.mult, op1=ALU.add), reads=["aa", "bt", hk], writes=[hk])
                            else:
                                P.op("dve", lambda e, h=h: e.tensor_tensor_scan(out=h[:, sc_][:, ::-1], data0=aa[:, sc_][:, ::-1], data1=bt[:, sc_][:, ::-1], initial=0.0, op0=ALU.mult, op1=ALU.add), reads=["aa", "bt"], writes=[hk])
                                P.op("dve", lambda e, h=h: e.tensor_tensor_scan(out=h[:, sx_][:, ::-1], data0=aa[:, sx_][:, ::-1], data1=bt[:, sx_][:, ::-1], initial=h[:, PADW:PADW + 1], op0=ALU.mult, op1=ALU.add), reads=["aa", "bt", hk], writes=[hk])
                        load_pad(ga, "rg", 256 + cg * 128)
                        P.op("pool", lambda e: e.tensor_tensor(out=hh[0][:, c0:c1], in0=hh[0][:, c0:c1], in1=hh[1][:, c0:c1], op=ALU.add), reads=["hh0", "hh1"], writes=["hh0"])
                        P.op("pool", lambda e: e.tensor_tensor(out=aa[:, c0:c1], in0=ga[:, c0:c1], in1=ga[:, c0:c1], op=ALU.mult), reads=["rg"], writes=["aa"])
                        P.op("dve", lambda e: e.tensor_scalar(out=aa[:, c0:c1], in0=aa[:, c0:c1], scalar1=0.044715, scalar2=1.0, op0=ALU.mult, op1=ALU.add), reads=["aa"], writes=["aa"])
                        P.op("pool", lambda e: e.tensor_tensor(out=aa[:, c0:c1], in0=aa[:, c0:c1], in1=ga[:, c0:c1], op=ALU.mult), reads=["aa", "rg"], writes=["aa"])
                        P.op("act", lambda e: e.activation(out=aa[:, c0:c1], in_=aa[:, c0:c1], func=AF.Sigmoid, scale=1.5957691216057308), reads=["aa"], writes=["aa"])
                        P.op("dve", lambda e: e.tensor_tensor(out=aa[:, c0:c1], in0=aa[:, c0:c1], in1=ga[:, c0:c1], op=ALU.mult), reads=["aa", "rg"], writes=["aa"])
                        P.op("dve", lambda e: e.tensor_tensor(out=yab[:, c0:c1], in0=aa[:, c0:c1], in1=hh[0][:, c0:c1], op=ALU.mult), reads=["aa", "hh0"], writes=["yab"])
                        store_seg(yab, "yab", cg * 128)
                    P.barrier(); P.emit()

                with contextlib.ExitStack() as ph:
                    xb = SB(ph, "xb", [128, SP]); wa = SB(ph, "wa", [128, SP]); wb = SB(ph, "wb", [128, SP]); ivc = SB(ph, "ivc", [128, SP])
                    pbf = SB(ph, "pbf", [128, SP], BF16); ob = [SB(ph, f"ob{i}", [128, 512], BF16) for i in range(2)]; sm = SB(ph, "smb", [128, 2])
                    for cg in range(2):
                        load_pad(xb, "xb", 512 + cg * 128)
                        P.dma("sp", lambda e, cg=cg: e.dma_start(out=ivc[:], in_=invcd[cg]), writes=["ivc"])
                        P.op("pool", lambda e: e.memset(wa[:], 0.0), writes=["wa"])
                        P.op("pool", lambda e: e.memset(wb[:], 0.0), writes=["wb"])
                        P.op("dve", lambda e: e.tensor_tensor(out=wa[:, 1:SP], in0=xb[:, 0:SP - 1], in1=xb[:, 1:SP], op=ALU.add), reads=["xb"], writes=["wa"])
                        P.op("dve", lambda e: e.tensor_tensor(out=wb[:, 1:SP - 1], in0=wa[:, 0:SP - 2], in1=wa[:, 2:SP], op=ALU.add), reads=["wa"], writes=["wb"])
                        if cg == 1:
                            P.op("dve", lambda e: e.tensor_tensor(out=wa[:, 3:SP - 3], in0=wb[:, 1:SP - 5], in1=wb[:, 5:SP - 1], op=ALU.add), reads=["wb"], writes=["wa"])
                            P.op("dve", lambda e: e.tensor_tensor(out=wb[:, 7:SP - 7], in0=wa[:, 3:SP - 11], in1=wa[:, 11:SP - 3], op=ALU.add), reads=["wa"], writes=["wb"])
                        c0, c1 = PADW, SP - PADW
                        P.op("dve", lambda e: e.tensor_tensor(out=wa[0:64, c0:c1], in0=wa[0:64, c0:c1], in1=ivc[0:64, c0:c1], op=ALU.mult), reads=["wa", "ivc"], writes=["wa"])
                        P.op("dve", lambda e: e.tensor_tensor(out=wa[64:128, c0:c1], in0=wb[64:128, c0:c1], in1=ivc[64:128, c0:c1], op=ALU.mult), reads=["wb", "wa", "ivc"], writes=["wa"])
                        P.op("dve", lambda e: e.tensor_tensor(out=pbf[:, c0:c1], in0=wa[:, c0:c1], in1=xb[:, c0:c1], op=ALU.subtract), reads=["wa", "xb"], writes=["pbf"])
                        P.op("dve", lambda e, cg=cg: e.tensor_tensor(out=sm[:, 0:1], in0=pc("bpool", cg), in1=pc("pscale", cg), op=ALU.mult), reads=["pvt"], writes=["smb"])
                        for qi, (p0, n) in enumerate(PCH):
                            pi = qi % 4; o = qi % 2; s0 = CH[qi][0]
                            P.op("pe", lambda e, pi=pi, cg=cg, p0=p0, n=n: e.matmul(PS[pi][:, :n], lhsT=bdb[:, 8 + cg, :], rhs=pbf[:, p0:p0 + n], start=True, stop=True), reads=["pbf", "bdb"], writes=[psn[pi]])
                            P.op("act", lambda e, pi=pi, o=o, cg=cg, n=n: e.activation(out=ob[o][:, :n], in_=PS[pi][:, :n], func=AF.Identity, scale=pc("pscale", cg), bias=sm[:, 0:1]), reads=[psn[pi], "pvt", "smb"], writes=[f"ob{o}"])
                            P.dma("sp", lambda e, o=o, cg=cg, s0=s0, n=n: e.dma_start(out=ycat[256 + cg * 128:256 + (cg + 1) * 128, s0:s0 + n], in_=ob[o][:, :n]), reads=[f"ob{o}"])
                    P.barrier(); P.emit()

                with contextlib.ExitStack() as ph:
                    xs = SB(ph, "xs", [128, 1, LX]); xsb = SB(ph, "xsb", [128, 2, LX], BF16)
                    XCS = SB(ph, "XCS", [128, 32, 512], BF16)
                    TB = [[SB(ph, f"tb{i}{j}", [128, 32, 256], BF16) for j in range(2)] for i in range(2)]
                    fb = [SB(ph, f"fb{i}", [128, 512], BF16) for i in range(2)]; ob = [SB(ph, f"oc{i}", [128, 512], BF16) for i in range(2)]
                    it = 0
                    for (s0, L, tab) in ((0, LC, dftc), (LC, LX, dftx)):
                        nlt = L // 128
                        for cg in range(2):
                            P.dma("sp", lambda e, cg=cg, s0=s0, L=L: e.dma_start(out=xs[:, 0, :L], in_=proj[768 + cg * 128:768 + (cg + 1) * 128, s0:s0 + L]), writes=["xs"])
                            P.op("pool", lambda e, cg=cg, L=L: e.tensor_copy(out=xsb[:, cg, :L], in_=xs[:, 0, :L]), reads=["xs"], writes=[("xsb", cg)])
                        for lt in range(nlt):
                            pi = lt % 2
                            for q, (cg, M) in enumerate(((0, C64b), (1, C64b), (0, S64b), (1, S64b))):
                                P.op("pe", lambda e, pi=pi, q=q, cg=cg, M=M, lt=lt: e.matmul(PS[pi][:, q * 128:(q + 1) * 128], lhsT=xsb[:, cg, lt * 128:(lt + 1) * 128], rhs=M, start=True, stop=True), reads=[("xsb", cg), "cstb"], writes=[psn[pi]])
                            eng = "act" if lt % 2 == 0 else "dve"
                            if eng == "act":
                                P.op("act", lambda e, pi=pi, lt=lt: e.activation(out=XCS[:, lt, :], in_=PS[pi][:, :], func=AF.Copy), reads=[psn[pi]], writes=[("XCS", lt)])
                            else:
                                P.op("dve", lambda e, pi=pi, lt=lt: e.tensor_copy(out=XCS[:, lt, :], in_=PS[pi][:, :]), reads=[psn[pi]], writes=[("XCS", lt)])
                        n = 256
                        nk = L // n
                        for kc in range(nk):
                            tb = TB[it % 2]; tk = f"tb{it % 2}"; it += 1
                            for j in range(2):
                                P.dma("sp" if j == 0 else "act", lambda e, tb=tb, j=j, kc=kc, nlt=nlt, n=n, tab=tab: e.dma_start(out=tb[j][:, :nlt, :n], in_=tab[j, kc]), writes=[tk + str(j)])
                            for cg in range(2):
                                pi = 2 + (kc * 2 + cg) % 2
                                for j in range(2):
                                    for lt in range(nlt):
                                        P.op("pe", lambda e, pi=pi, tb=tb, j=j, lt=lt, cg=cg, n=n, nlt=nlt: e.matmul(PS[pi][:, :n], lhsT=XCS[:, lt, j * 256 + cg * 128:j * 256 + (cg + 1) * 128], rhs=tb[j][:, lt, :n], start=(j == 0 and lt == 0), stop=(j == 1 and lt == nlt - 1)),
                                             reads=[("XCS", lt), tk + str(j)], writes=[psn[pi]])
                                o = (kc * 2 + cg) % 2
                                P.op("dve", lambda e, pi=pi, o=o, n=n: e.tensor_copy(out=fb[o][:, :n], in_=PS[pi][:, :n]), reads=[psn[pi]], writes=[f"fb{o}"])
                                P.op("pe", lambda e, o=o, cg=cg, n=n: e.matmul(PS[4 + o][:, :n], lhsT=bdb[:, 10 + cg, :], rhs=fb[o][:, :n], start=True, stop=True), reads=[f"fb{o}", "bdb"], writes=[psn[4 + o]])
                                P.op("act", lambda e, o=o, cg=cg, n=n: e.activation(out=ob[o][:, :n], in_=PS[4 + o][:, :n], func=AF.Identity, bias=pc("bfour", cg)), reads=[psn[4 + o], "pvt"], writes=[f"oc{o}"])
                                P.dma("sp", lambda e, o=o, cg=cg, s0=s0, kc=kc, n=n: e.dma_start(out=ycat[512 + cg * 128:512 + (cg + 1) * 128, s0 + kc * n:s0 + (kc + 1) * n], in_=ob[o][:, :n]), reads=[f"oc{o}"])
                    P.barrier(); P.emit()

                with contextlib.ExitStack() as ph:
                    xv = SB(ph, "xv", [128, SP]); xg = SB(ph, "xg", [128, SP]); vb = [SB(ph, f"vb{i}", [128, SP], BF16) for i in range(2)]
                    dg = [SB(ph, f"dg{i}", [128, 31, 128], BF16) for i in range(2)]
                    wpst = SB(ph, "wpst", [128, 2, 256]); wpb = SB(ph, "wpb", [128, 2, 256], BF16)
                    vc = SB(ph, "vc", [128, 512]); vcb = SB(ph, "vcb", [128, 512], BF16); cen = SB(ph, "cen", [128, 512]); sq2 = SB(ph, "sq2", [128, 512], BF16)
                    rs2 = SB(ph, "rs2", [128, 512]); sg2 = SB(ph, "sg2", [128, 512]); svb = [SB(ph, f"svb{i}", [128, 512], BF16) for i in range(2)]
                    ob = [SB(ph, f"od{i}", [128, 512], BF16) for i in range(2)]
                    P.dma("sp", lambda e: e.dma_start(out=wpst[:], in_=w_pw[l].rearrange("(k p) n -> p k n", p=128)), writes=["wpst"])
                    P.op("dve", lambda e: e.tensor_copy(out=wpb[:], in_=wpst[:]), reads=["wpst"], writes=["wpb"])
                    for cg in range(2):
                        load_pad(xv, "xv", 1024 + cg * 128)
                        load_pad(xg, "xg", 1280 + cg * 128)
                        P.op("act", lambda e: e.activation(out=xg[:], in_=xg[:], func=AF.Sigmoid), reads=["xg"], writes=["xg"])
                        P.op("dve", lambda e, cg=cg: e.tensor_tensor(out=vb[cg][:], in0=xv[:], in1=xg[:], op=ALU.mult), reads=["xv", "xg"], writes=[f"vb{cg}"])
                        for j in range(31):
                            P.op("dve", lambda e, cg=cg, j=j: e.tensor_scalar(out=dg[cg][:, j, :], in0=ID32, scalar1=pc("cdw", j * 2 + cg), scalar2=None, op0=ALU.mult), reads=["cst", "pvt"], writes=[f"dg{cg}"])
                    for qi, (p0, n) in enumerate(PCH):
                        s0 = CH[qi][0]
                        for cg in range(2):
                            for j in range(31):
                                P.op("pe", lambda e, cg=cg, j=j, p0=p0, n=n: e.matmul(PS[cg][:, :n], lhsT=dg[cg][:, j, :], rhs=vb[cg][:, p0 + j - 15:p0 + j - 15 + n], start=(j == 0), stop=(j == 30)), reads=[f"vb{cg}", f"dg{cg}"], writes=[psn[cg]])
                            P.op("act", lambda e, cg=cg, n=n: e.activation(out=vc[:, :n], in_=PS[cg][:, :n], func=AF.Identity, bias=pc("cdb", cg)), reads=[psn[cg], "pvt"], writes=["vc"])
                            P.op("pool", lambda e, n=n: e.tensor_copy(out=vcb[:, :n], in_=vc[:, :n]), reads=["vc"], writes=["vcb"])
                            P.op("pe", lambda e, n=n: e.matmul(PS[2][:, :n], lhsT=MAVb, rhs=vcb[:, :n], start=True, stop=True), reads=["vcb", "cstb"], writes=["ps2"])
                            P.op("dve", lambda e, n=n: e.tensor_tensor(out=cen[:, :n], in0=vc[:, :n], in1=PS[2][:, :n], op=ALU.subtract), reads=["vc", "ps2"], writes=["cen"])
                            P.op("act", lambda e, n=n: e.activation(out=sq2[:, :n], in_=cen[:, :n], func=AF.Square), reads=["cen"], writes=["sq2"])
                            P.op("pe", lambda e, n=n: e.matmul(PS[3][:, :n], lhsT=MAVb, rhs=sq2[:, :n], start=True, stop=True), reads=["sq2", "cstb"], writes=["ps3"])
                            P.op("dve", lambda e, n=n: e.tensor_scalar(out=rs2[:, :n], in0=PS[3][:, :n], scalar1=EPS, scalar2=None, op0=ALU.add), reads=["ps3"], writes=["rs2"])
                            P.op("act", lambda e, n=n: e.activation(out=rs2[:, :n], in_=rs2[:, :n], func=AF.Sqrt), reads=["rs2"], writes=["rs2"])
                            P.op("dve", lambda e, n=n: e.reciprocal(out=rs2[:, :n], in_=rs2[:, :n]), reads=["rs2"], writes=["rs2"])
                            P.op("dve", lambda e, n=n: e.tensor_tensor(out=cen[:, :n], in0=cen[:, :n], in1=rs2[:, :n], op=ALU.mult), reads=["cen", "rs2"], writes=["cen"])
                            P.op("act", lambda e, n=n, cg=cg: e.activation(out=cen[:, :n], in_=cen[:, :n], func=AF.Identity, scale=pc("lng", cg), bias=pc("lnb", cg)), reads=["cen", "pvt"], writes=["cen"])
                            P.op("act", lambda e, n=n: e.activation(out=sg2[:, :n], in_=cen[:, :n], func=AF.Sigmoid), reads=["cen"], writes=["sg2"])
                            P.op("dve", lambda e, n=n, cg=cg: e.tensor_tensor(out=svb[cg][:, :n], in0=cen[:, :n], in1=sg2[:, :n], op=ALU.mult), reads=["cen", "sg2"], writes=[f"svb{cg}"])
                        for oc_ in range(2):
                            for cg in range(2):
                                P.op("pe", lambda e, oc_=oc_, cg=cg, n=n: e.matmul(PS[4 + oc_][:, :n], lhsT=wpb[:, cg, oc_ * 128:(oc_ + 1) * 128], rhs=svb[cg][:, :n], start=(cg == 0), stop=(cg == 1)), reads=[f"svb{cg}", "wpb"], writes=[psn[4 + oc_]])
                            P.op("act", lambda e, oc_=oc_, n=n: e.activation(out=ob[oc_][:, :n], in_=PS[4 + oc_][:, :n], func=AF.Identity, bias=pc("bpw", oc_)), reads=[psn[4 + oc_], "pvt"], writes=[f"od{oc_}"])
                            P.dma("sp", lambda e, oc_=oc_, s0=s0, n=n: e.dma_start(out=ycat[768 + oc_ * 128:768 + (oc_ + 1) * 128, s0:s0 + n], in_=ob[oc_][:, :n]), reads=[f"od{oc_}"])
                    P.barrier(); P.emit()
                mixs.close()
                if stop_after == "mix":
                    break

                chs = list(enumerate(CH))
                if last:
                    chs = chs[1:]
                with contextlib.ExitStack() as ph:
                    wob = SB(ph, "wob", [128, 8, 1024], BF16)
                    GT = SB(ph, "GT", [NE, S])
                    wr = SB(ph, "wr", [128, 8, NE]); brb = SB(ph, "brb", [128, NE]); bdn = SB(ph, "bdn", [NE, D])
                    P.dma("sp", lambda e: e.dma_start(out=wr[:], in_=w_router[l].rearrange("(k p) n -> p k n", p=128)), writes=["wr"])
                    P.dma("sp", lambda e: e.dma_start(out=brb[:], in_=b_router[l:l + 1, :].partition_broadcast(128)), writes=["brb"])
                    P.dma("sp", lambda e: e.dma_start(out=bdn[:], in_=b_down[l]), writes=["bdn"])
                    yc = [SB(ph, f"yc{i}", [128, 8, 512], BF16) for i in range(2)]
                    rb = [SB(ph, f"r{i}", [128, 8, 512]) for i in range(2)]
                    yt = SB(ph, "yt", [128, 512]); sqb = SB(ph, "sqb", [128, 8, 512], BF16); rstd = SB(ph, "rstd", [128, 512])
                    tmp = SB(ph, "tmp", [128, 8, 512]); h32 = SB(ph, "h32", [128, 8, 512]); hb = SB(ph, "hbw", [128, 8, 512], BF16)
                    for hf_ in range(2):
                        P.dma("sp", lambda e, hf_=hf_: e.dma_start(out=tmp[:], in_=w_out[l].rearrange("(k p) n -> p k n", p=128)[:, :, hf_ * 512:(hf_ + 1) * 512]), writes=["tmp"])
                        P.op("pool", lambda e, hf_=hf_: e.tensor_copy(out=wob[:, :, hf_ * 512:(hf_ + 1) * 512], in_=tmp[:]), reads=["tmp"], writes=["wob"])
                    i8 = SB(ph, "i8", [128, 8], U32); mkb = SB(ph, "mkb", [128, NE], BF16); Rk = SB(ph, "Rk", [128, NE]); oh = SB(ph, "oh", [128, NE])
                    e4 = SB(ph, "e4", [128, 4]); htb = [SB(ph, f"htb{i}", [128, D], BF16) for i in range(2)]
                    P.op("pool", lambda e: e.memset(carry[:], 0.0), writes=["carry"])
                    lg = SB(ph, "lg", [128, NE]); t8 = SB(ph, "t8", [128, 8]); ex = SB(ph, "ex", [128, NE]); mk = SB(ph, "mk", [128, NE]); s1 = SB(ph, "s1", [128, 4])
                    for ci, (s0, n) in chs:
                        b = ci % 2; w = 1 if ci == 0 else 0
                        P.dma("sp", lambda e, b=b, s0=s0, n=n: e.dma_start(out=yc[b][:, :, :n], in_=ycat.rearrange("(k p) s -> p k s", p=128)[:, :, s0:s0 + n]), writes=[f"yc{b}"])
                        P.dma("act", lambda e, b=b, s0=s0, n=n: e.dma_start(out=rb[b][:, :, :n], in_=res.rearrange("(k p) s -> p k s", p=128)[:, :, s0:s0 + n]), writes=[f"r{b}"])
                        for dc in range(8):
                            pi = dc % 4
                            for k in range(8):
                                P.op("pe", lambda e, b=b, pi=pi, dc=dc, k=k, n=n: e.matmul(PS[pi][:, :n], lhsT=wob[:, k, dc * 128:(dc + 1) * 128], rhs=yc[b][:, k, :n], start=(k == 0), stop=(k == 7)), reads=[f"yc{b}", "wob"], writes=[psn[pi]])
                            P.op("act", lambda e, pi=pi, dc=dc, n=n: e.activation(out=yt[:, :n], in_=PS[pi][:, :n], func=AF.Identity, bias=pc("b_out", dc)), reads=[psn[pi], "pvt"], writes=["yt"])
                            P.op("dve", lambda e, b=b, dc=dc, n=n, w=w: e.scalar_tensor_tensor(out=rb[b][:, dc, :n], in0=yt[:, :n], scalar=G1(dc, w), in1=rb[b][:, dc, :n], op0=ALU.mult, op1=ALU.add), reads=["yt", "modT", f"r{b}"], writes=[f"r{b}"])
                        rms_rstd(rb[b], f"r{b}", sqb, "sqb", n, 7, rstd, "rstd")
                        for k in range(8):
                            P.op("dve", lambda e, b=b, k=k, n=n: e.tensor_tensor(out=tmp[:, k, :n], in0=rb[b][:, k, :n], in1=rstd[:, :n], op=ALU.mult), reads=[f"r{b}", "rstd"], writes=[("tmp", k)])
                            P.op("act", lambda e, k=k, n=n, w=w: e.activation(out=h32[:, k, :n], in_=tmp[:, k, :n], func=AF.Identity, scale=A2[:, k, w:w + 1], bias=SH2(k, w)), reads=[("tmp", k), "A", "modT"], writes=[("h32", k)])
                            P.op("pool", lambda e, k=k, n=n: e.tensor_copy(out=hb[:, k, :n], in_=h32[:, k, :n]), reads=[("h32", k)], writes=[("hbw", k)])
                        P.dma("sp", lambda e, s0=s0, n=n: e.dma_start(out=hT.rearrange("(k p) s -> p k s", p=128)[:, :, s0:s0 + n], in_=hb[:, :, :n]), reads=["hbw"])
                        for t0 in range(0, n, 128):
                            for k in range(8):
                                P.op("pe", lambda e, k=k, t0=t0: e.matmul(PS[4][:, 0:NE], lhsT=h32[:, k, t0:t0 + 128], rhs=wr[:, k, :], start=(k == 0), stop=(k == 7)), reads=[("h32", k), "wr"], writes=["ps4"])
                            P.op("dve", lambda e: e.tensor_tensor(out=lg[:], in0=PS[4][:, 0:NE], in1=brb[:], op=ALU.add), reads=["ps4", "brb"], writes=["lg"])
                            P.op("dve", lambda e: e.max(out=t8[:], in_=lg[:]), reads=["lg"], writes=["t8"])
                            P.op("dve", lambda e: e.tensor_scalar(out=s1[:, 0:1], in0=t8[:, 0:1], scalar1=-1.0, scalar2=None, op0=ALU.mult), reads=["t8"], writes=["s1"])
                            P.op("act", lambda e: e.activation(out=ex[:], in_=lg[:], func=AF.Exp, bias=s1[:, 0:1]), reads=["lg", "s1"], writes=["ex"])
                            P.op("dve", lambda e: e.tensor_scalar(out=mk[:], in0=lg[:], scalar1=t8[:, 3:4], scalar2=None, op0=ALU.is_ge), reads=["lg", "t8"], writes=["mk"])
                            P.op("dve", lambda e: e.tensor_tensor(out=ex[:], in0=ex[:], in1=mk[:], op=ALU.mult), reads=["ex", "mk"], writes=["ex"])
                            P.op("dve", lambda e: e.tensor_reduce(out=s1[:, 1:2], in_=ex[:], axis=AX.X, op=ALU.add), reads=["ex"], writes=["s1"])
                            P.op("dve", lambda e: e.reciprocal(out=s1[:, 2:3], in_=s1[:, 1:2]), reads=["s1"], writes=["s1"])
                            P.op("dve", lambda e: e.tensor_scalar(out=ex[:], in0=ex[:], scalar1=s1[:, 2:3], scalar2=None, op0=ALU.mult), reads=["ex", "s1"], writes=["ex"])
                            if sparse:
                                tg = (s0 + t0) // 128
                                P.op("dve", lambda e: e.max_index(out=i8[:], in_max=t8[:], in_values=lg[:]), reads=["lg", "t8"], writes=["i8"])
                                P.op("dve", lambda e, tg=tg: e.tensor_copy(out=idxf[:, tg, :], in_=i8[:, 0:4]), reads=["i8"], writes=["idxf"])
                                P.op("act", lambda e: e.activation(out=e4[:], in_=t8[:, 0:4], func=AF.Exp, bias=s1[:, 0:1]), reads=["t8", "s1"], writes=["e4"])
                                P.op("dve", lambda e: e.tensor_reduce(out=s1[:, 3:4], in_=e4[:], axis=AX.X, op=ALU.add), reads=["e4"], writes=["s1"])
                                P.op("dve", lambda e: e.reciprocal(out=s1[:, 3:4], in_=s1[:, 3:4]), reads=["s1"], writes=["s1"])
                                P.op("dve", lambda e, tg=tg: e.tensor_scalar(out=g4[:, tg, :], in0=e4[:], scalar1=s1[:, 3:4], scalar2=None, op0=ALU.mult), reads=["e4", "s1"], writes=["g4"])
                                P.op("pool", lambda e: e.tensor_copy(out=mkb[:], in_=mk[:]), reads=["mk"], writes=["mkb"])
                                P.op("pe", lambda e: e.matmul(PS[6][:, 0:NE], lhsT=UTb, rhs=mkb[:], start=True, stop=True), reads=["mkb", "cstb"], writes=["ps6"])
                                P.op("pe", lambda e: e.matmul(PS[6][:, 64:64 + NE], lhsT=ONEb, rhs=mkb[:], start=True, stop=True), reads=["mkb", "cstb"], writes=["ps6"])
                                P.op("dve", lambda e: e.tensor_tensor(out=Rk[:], in0=PS[6][:, 0:NE], in1=carry[:], op=ALU.add), reads=["ps6", "carry"], writes=["Rk"])
                                P.op("dve", lambda e: e.tensor_tensor(out=carry[:], in0=PS[6][:, 64:64 + NE], in1=carry[:], op=ALU.add), reads=["ps6", "carry"], writes=["carry"])
                                for k4 in range(4):
                                    P.op("dve", lambda e, tg=tg, k4=k4: e.tensor_scalar(out=oh[:], in0=iot[:, 0:NE], scalar1=idxf[:, tg, k4:k4 + 1], scalar2=None, op0=ALU.is_equal), reads=["iot", "idxf"], writes=["oh"])
                                    P.op("dve", lambda e: e.tensor_tensor(out=oh[:], in0=oh[:], in1=Rk[:], op=ALU.mult), reads=["oh", "Rk"], writes=["oh"])
                                    P.op("dve", lambda e, tg=tg, k4=k4: e.tensor_reduce(out=rank4[:, tg, k4:k4 + 1], in_=oh[:], axis=AX.X, op=ALU.add), reads=["oh"], writes=["rank4"])
                                hq = tg % 2
                                psb = PS[6][:].bitcast(BF16)
                                for k in range(8):
                                    P.op("pe", lambda e, k=k, t0=t0, psb=psb: e.transpose(out=psb[:, 256 + k * 96:256 + k * 96 + 128] if False else PS[3][:].bitcast(BF16)[:, k * 128:(k + 1) * 128], in_=hb[:, k, t0:t0 + 128], identity=IDb), reads=[("hbw", k), "cstb"], writes=["ps3"])
                                P.op("act", lambda e, hq=hq: e.activation(out=htb[hq][:], in_=PS[3][:].bitcast(BF16), func=AF.Copy), reads=["ps3"], writes=[f"htb{hq}"])
                                P.dma("act", lambda e, hq=hq, s0=s0, t0=t0: e.dma_start(out=htm[s0 + t0:s0 + t0 + 128, :], in_=htb[hq][:]), reads=[f"htb{hq}"])
                            P.op("pe", lambda e: e.transpose(out=PS[5][0:NE, 0:128], in_=ex[:], identity=ID32), reads=["ex", "cst"], writes=["ps5"])
                            P.op("act", lambda e, s0=s0, t0=t0: e.activation(out=GT[:, s0 + t0:s0 + t0 + 128], in_=PS[5][0:NE, 0:128], func=AF.Copy), reads=["ps5"], writes=[("GT", s0 + t0)])
                        for dc in range(8):
                            pi = 2 + dc % 2
                            P.op("pe", lambda e, pi=pi, dc=dc, s0=s0, n=n: e.matmul(PS[pi][:, :n], lhsT=bdn[:, dc * 128:(dc + 1) * 128], rhs=GT[:, s0:s0 + n], start=True, stop=True), reads=["GT", "bdn"], writes=[psn[pi]])
                            P.op("dve", lambda e, pi=pi, b=b, dc=dc, n=n, w=w: e.scalar_tensor_tensor(out=rb[b][:, dc, :n], in0=PS[pi][:, :n], scalar=G2(dc, w), in1=rb[b][:, dc, :n], op0=ALU.mult, op1=ALU.add), reads=[psn[pi], "modT", f"r{b}"], writes=[f"r{b}"])
                        P.dma("sp", lambda e, b=b, s0=s0, n=n: e.dma_start(out=res.rearrange("(k p) s -> p k s", p=128)[:, :, s0:s0 + n], in_=rb[b][:, :, :n]), reads=[f"r{b}"])
                        P.dma("sp", lambda e, s0=s0, n=n: e.dma_start(out=gTd[:, s0:s0 + n], in_=GT[:, s0:s0 + n]), reads=["GT"])
                    P.barrier(); P.emit()
                if stop_after == "outproj":
                    break

                if sparse:
                    nblk = (4 * 128 * len([1 for ci_, (s0_, n_) in chs for _ in range(n_ // 128)])) // BLK + NE
                    tiles = [(s0_ + t0_) // 128 for ci_, (s0_, n_) in chs for t0_ in range(0, n_, 128)]
                    P.drain()
                    with contextlib.ExitStack() as ph:
                        ci_t = SB(ph, "ci_t", [128, NE], I32); padf = SB(ph, "padf", [128, NE]); pend = SB(ph, "pend", [128, NE]); pst = SB(ph, "pst", [128, NE])
                        one32 = SB(ph, "one32", [128, NE]); bef = SB(ph, "bef", [128, NBLK]); oh = SB(ph, "oh2", [128, NE]); pst4 = SB(ph, "pst4", [128, NT, 4])
                        hl = [SB(ph, f"hl{i}", [128, D], BF16) for i in range(3)]
                        P.op("pool", lambda e: e.memset(one32[:], 1.0), writes=["one32"])
                        P.op("pool", lambda e: e.memset(pst4[:], 0.0), writes=["pst4"])
                        P.op("dve", lambda e: e.tensor_scalar(out=padf[:], in0=carry[:], scalar1=float(BLK - 1), scalar2=None, op0=ALU.add), reads=["carry"], writes=["padf"])
                        P.op("dve", lambda e: e.tensor_copy(out=ci_t[:], in_=padf[:]), reads=["padf"], writes=["ci_t"])
                        P.op("dve", lambda e: e.tensor_scalar(out=ci_t[:], in0=ci_t[:], scalar1=8, scalar2=8, op0=ALU.arith_shift_right, op1=ALU.logical_shift_left), reads=["ci_t"], writes=["ci_t"])
                        P.op("dve", lambda e: e.tensor_copy(out=padf[:], in_=ci_t[:]), reads=["ci_t"], writes=["padf"])
                        P.op("dve", lambda e: e.tensor_tensor_scan(out=pend[:], data0=one32[:], data1=padf[:], initial=0.0, op0=ALU.mult, op1=ALU.add), reads=["one32", "padf"], writes=["pend"])
                        P.op("dve", lambda e: e.tensor_tensor(out=pst[:], in0=pend[:], in1=padf[:], op=ALU.subtract), reads=["pend", "padf"], writes=["pst"])
                        P.op("pool", lambda e: e.memset(bef[:], 0.0), writes=["bef"])
                        for ex_ in range(NE):
                            P.op("dve", lambda e, ex_=ex_: e.scalar_tensor_tensor(out=bef[:], in0=iot[:, 32:32 + NBLK], scalar=pend[:, ex_:ex_ + 1], in1=bef[:], op0=ALU.is_ge, op1=ALU.add), reads=["iot", "pend", "bef"], writes=["bef"])
                        P.op("dve", lambda e: e.tensor_scalar(out=bef[:], in0=bef[:], scalar1=float(NE - 1), scalar2=None, op0=ALU.min), reads=["bef"], writes=["bef"])
                        skp = SB(ph, "skp", [128, NBLK])
                        P.op("dve", lambda e: e.tensor_tensor(out=skp[:, 2:NBLK], in0=bef[:, 2:NBLK], in1=bef[:, 0:NBLK - 2], op=ALU.is_equal), reads=["bef"], writes=["skp"])
                        P.op("dve", lambda e: e.tensor_scalar(out=bef[:], in0=bef[:], scalar1=128.0, scalar2=iot[:, 32 + NBLK:33 + NBLK], op0=ALU.mult, op1=ALU.add), reads=["bef", "iot"], writes=["bef"])
                        P.op("dve", lambda e: e.scalar_tensor_tensor(out=bef[:, 2:NBLK], in0=skp[:, 2:NBLK], scalar=1048576.0, in1=bef[:, 2:NBLK], op0=ALU.mult, op1=ALU.add), reads=["bef", "skp"], writes=["bef"])
                        P.op("dve", lambda e: e.tensor_copy(out=widx[:], in_=bef[:]), reads=["bef"], writes=["widx"])
                        tv = [SB(ph, f"tv{i}", [128, NT * 4]) for i in range(2)]
                        for ex_ in range(NE):
                            q_ = ex_ % 2
                            P.op("dve", lambda e, ex_=ex_, q_=q_: e.tensor_scalar(out=tv[q_][:], in0=idxf[:].rearrange("p t k -> p (t k)"), scalar1=float(ex_), scalar2=pst[:, ex_:ex_ + 1], op0=ALU.is_equal, op1=ALU.mult), reads=["idxf", "pst"], writes=[f"tv{q_}"])
                            P.op("pool", lambda e, q_=q_: e.tensor_tensor(out=pst4[:].rearrange("p t k -> p (t k)"), in0=pst4[:].rearrange("p t k -> p (t k)"), in1=tv[q_][:], op=ALU.add), reads=["pst4", f"tv{q_}"], writes=["pst4"])
                        P.op("dve", lambda e: e.tensor_tensor(out=pst4[:], in0=pst4[:], in1=rank4[:], op=ALU.add), reads=["pst4", "rank4"], writes=["pst4"])
                        P.op("dve", lambda e: e.tensor_copy(out=destu[:], in_=pst4[:].rearrange("p t k -> p (t k)")), reads=["pst4"], writes=["destu"])
                        for i_, tg in enumerate(tiles):
                            hq = i_ % 3
                            P.dma("sp", lambda e, hq=hq, tg=tg: e.dma_start(out=hl[hq][:], in_=htm[tg * 128:(tg + 1) * 128, :]), writes=[f"hl{hq}"])
                            for k4 in range(4):
                                P.dma("pool", lambda e, hq=hq, tg=tg, k4=k4: ind_dma(e, out=xs_d, out_offset=bass.IndirectOffsetOnAxis(ap=destu[:, tg * 4 + k4:tg * 4 + k4 + 1], axis=0), in_=hl[hq][:, :], in_offset=None), reads=[f"hl{hq}", "destu"])
                        P.barrier(); P.emit()

                    with contextlib.ExitStack() as ph:
                        wg = [SB(ph, f"wg{i}", [128, 9, 2048], BF16) for i in range(2)]
                        wd = [SB(ph, f"wd{i}", [128, 8, 1024], BF16) for i in range(2)]
                        xsb = [SB(ph, f"xsb{i}", [128, 2, D], BF16) for i in range(2)]
                        xT = [SB(ph, f"xT{i}", [128, 8, BLK], BF16) for i in range(2)]
                        aT = [SB(ph, f"aT{i}", [128, 8, BLK], BF16) for i in range(2)]
                        ysb = [SB(ph, f"ysb{i}", [128, 2, D]) for i in range(2)]
                        gt = [SB(ph, f"gt{i}", [128, BLK]) for i in range(2)]; sg = [SB(ph, f"sg{i}", [128, BLK]) for i in range(2)]; up = [SB(ph, f"up{i}", [128, BLK]) for i in range(2)]
                        onr = SB(ph, "onr", [1, BLK], BF16)
                        P.op("pool", lambda e: e.memset(onr[:], 1.0), writes=["onr"])

                        def wload(b):
                            p_ = b % 2
                            P.dma("pool", lambda e: ind_dma(e, out=wg[p_][:].rearrange("p k f -> p (k f)"), out_offset=None, in_=WGb.rearrange("e p k f -> (e p) (k f)"), in_offset=bass.IndirectOffsetOnAxis(ap=widx[:, b:b + 1], axis=0), bounds_check=vbox["v"], oob_is_err=False), reads=["widx"], writes=[f"wg{p_}"])
                            P.dma("pool", lambda e: ind_dma(e, out=wd[p_][:].rearrange("p k f -> p (k f)"), out_offset=None, in_=WDb.rearrange("e p k f -> (e p) (k f)"), in_offset=bass.IndirectOffsetOnAxis(ap=widx[:, b:b + 1], axis=0), bounds_check=vbox["v"], oob_is_err=False), reads=["widx"], writes=[f"wd{p_}"])
                            P.dma("sp", lambda e: e.dma_start(out=xsb[p_][:], in_=xs_d[b * BLK:(b + 1) * BLK, :].rearrange("(j p) d -> p j d", p=128)), writes=[f"xsb{p_}"])

                        def block(b):
                            p_ = b % 2
                            if b + 1 < nblk:
                                wload(b + 1)
                            for j in range(2):
                                psb = PS[6 + j][:].bitcast(BF16)
                                for k in range(8):
                                    P.op("pe", lambda e, j=j, k=k, psb=psb: e.transpose(out=psb[:, k * 128:(k + 1) * 128], in_=xsb[p_][:, j, k * 128:(k + 1) * 128], identity=IDb), reads=[f"xsb{p_}", "cstb"], writes=[psn[6 + j]])
                                if j == 0:
                                    P.op("act", lambda e, psb=psb: e.activation(out=xT[p_][:, :, 0:128], in_=psb.rearrange("p (k t) -> p k t", t=128), func=AF.Copy), reads=[psn[6]], writes=[(f"xT{p_}", 0)])
                                else:
                                    P.op("dve", lambda e, psb=psb: e.tensor_copy(out=xT[p_][:, :, 128:256], in_=psb.rearrange("p (k t) -> p k t", t=128)), reads=[psn[7]], writes=[(f"xT{p_}", 1)])
                            for fc in range(8):
                                q = fc % 2
                                for hf in range(2):
                                    c0_ = hf * 1024 + fc * 128
                                    for k in range(8):
                                        P.op("pe", lambda e, q=q, hf=hf, c0_=c0_, k=k: e.matmul(PS[q][:, hf * BLK:(hf + 1) * BLK], lhsT=wg[p_][:, k, c0_:c0_ + 128], rhs=xT[p_][:, k, :], start=(k == 0), stop=False), reads=[f"wg{p_}", f"xT{p_}"], writes=[psn[q]])
                                    P.op("pe", lambda e, q=q, hf=hf, c0_=c0_: e.matmul(PS[q][:, hf * BLK:(hf + 1) * BLK], lhsT=wg[p_][0:1, 8, c0_:c0_ + 128], rhs=onr[0:1, :], start=False, stop=True), reads=[f"wg{p_}", "onr"], writes=[psn[q]])
                                P.op("dve", lambda e, q=q: e.tensor_scalar(out=gt[q][:], in0=PS[q][:, 0:BLK], scalar1=7.0, scalar2=None, op0=ALU.min), reads=[psn[q]], writes=[f"gt{q}"])
                                P.op("act", lambda e, q=q: e.activation(out=sg[q][:], in_=gt[q][:], func=AF.Sigmoid, scale=1.702), reads=[f"gt{q}"], writes=[f"sg{q}"])
                                P.op("dve", lambda e, q=q: e.tensor_scalar(out=up[q][:], in0=PS[q][:, BLK:2 * BLK], scalar1=7.0, scalar2=-7.0, op0=ALU.min, op1=ALU.max), reads=[psn[q]], writes=[f"up{q}"])
                                P.op("pool", lambda e, q=q: e.tensor_tensor(out=gt[q][:], in0=gt[q][:], in1=sg[q][:], op=ALU.mult), reads=[f"gt{q}", f"sg{q}"], writes=[f"gt{q}"])
                                P.op("dve", lambda e, q=q, fc=fc: e.scalar_tensor_tensor(out=aT[p_][:, fc, :], in0=up[q][:], scalar=1.0, in1=gt[q][:], op0=ALU.add, op1=ALU.mult), reads=[f"up{q}", f"gt{q}"], writes=[(f"aT{p_}", fc)])
                            for j in range(2):
                                for dh in range(2):
                                    q = 2 + (j * 2 + dh) % 4
                                    for fc in range(8):
                                        P.op("pe", lambda e, q=q, j=j, dh=dh, fc=fc: e.matmul(PS[q][:, :], lhsT=aT[p_][:, fc, j * 128:(j + 1) * 128], rhs=wd[p_][:, fc, dh * 512:(dh + 1) * 512], start=(fc == 0), stop=(fc == 7)), reads=[(f"aT{p_}", fc), f"wd{p_}"], writes=[psn[q]])
                                    if dh == 0:
                                        P.op("act", lambda e, q=q, j=j, dh=dh: e.activation(out=ysb[p_][:, j, dh * 512:(dh + 1) * 512], in_=PS[q][:, :], func=AF.Copy), reads=[psn[q]], writes=[(f"ysb{p_}", j * 2 + dh)])
                                    else:
                                        P.op("pool" if False else "dve", lambda e, q=q, j=j, dh=dh: e.tensor_copy(out=ysb[p_][:, j, dh * 512:(dh + 1) * 512], in_=PS[q][:, :]), reads=[psn[q]], writes=[(f"ysb{p_}", j * 2 + dh)])
                            P.dma("sp", lambda e: e.dma_start(out=ys_d[b * BLK:(b + 1) * BLK, :].rearrange("(j p) d -> p j d", p=128), in_=ysb[p_][:]), reads=[f"ysb{p_}"])

                        if l + 1 < depth and stop_after is None:
                            P.bg = precast_gen(l + 1)
                            P.bg_every = 500
                        wload(0)
                        for b in range(nblk):
                            block(b)
                        P.bg_every = 100
                        P.barrier(); P.emit()

                    with contextlib.ExitStack() as ph:
                        gb = [[SB(ph, f"gb{i}{k}", [128, D]) for k in range(4)] for i in range(2)]
                        acc = [SB(ph, f"acc{i}", [128, D]) for i in range(4)]
                        sqbc = SB(ph, "sqbc", [128, 8, 512], BF16); rstdc = SB(ph, "rstdc", [128, 512])
                        rb = [SB(ph, f"r{i}", [128, 8, 512]) for i in range(2)]
                        it_ = 0
                        for ci, (s0, n) in chs:
                            b = ci % 2; w = 1 if ci == 0 else 0
                            P.dma("sp", lambda e, b=b, s0=s0, n=n: e.dma_start(out=rb[b][:, :, :n], in_=res.rearrange("(k p) s -> p k s", p=128)[:, :, s0:s0 + n]), writes=[f"r{b}"])
                            for ti, t0 in enumerate(range(0, n, 128)):
                                tg = (s0 + t0) // 128
                                gq = it_ % 2; it_ += 1
                                for k4 in range(4):
                                    P.dma("pool", lambda e, gq=gq, k4=k4, tg=tg: ind_dma(e, out=gb[gq][k4][:, :], out_offset=None, in_=ys_d, in_offset=bass.IndirectOffsetOnAxis(ap=destu[:, tg * 4 + k4:tg * 4 + k4 + 1], axis=0)), reads=["destu"], writes=[f"gb{gq}{k4}"])
                                P.op("dve", lambda e, gq=gq, ti=ti, tg=tg: e.tensor_scalar(out=acc[ti][:], in0=gb[gq][0][:], scalar1=g4[:, tg, 0:1], scalar2=None, op0=ALU.mult), reads=[f"gb{gq}0", "g4"], writes=[f"acc{ti}"])
                                for k4 in range(1, 4):
                                    P.op("dve", lambda e, gq=gq, ti=ti, tg=tg, k4=k4: e.scalar_tensor_tensor(out=acc[ti][:], in0=gb[gq][k4][:], scalar=g4[:, tg, k4:k4 + 1], in1=acc[ti][:], op0=ALU.mult, op1=ALU.add), reads=[f"gb{gq}{k4}", "g4", f"acc{ti}"], writes=[f"acc{ti}"])
                            for k in range(8):
                                q = k % 4
                                for ti, t0 in enumerate(range(0, n, 128)):
                                    P.op("pe", lambda e, q=q, k=k, ti=ti, t0=t0: e.transpose(out=PS[q][:, t0:t0 + 128], in_=acc[ti][:, k * 128:(k + 1) * 128], identity=ID32), reads=[f"acc{ti}", "cst"], writes=[psn[q]])
                                P.op("dve", lambda e, q=q, k=k, b=b, n=n, w=w: e.scalar_tensor_tensor(out=rb[b][:, k, :n], in0=PS[q][:, :n], scalar=G2(k, w), in1=rb[b][:, k, :n], op0=ALU.mult, op1=ALU.add), reads=[psn[q], "modT", f"r{b}"], writes=[f"r{b}"])
                            if last and stop_after is None:
                                rms_rstd(rb[b], f"r{b}", sqbc, "sqbc", n, 7, rstdc, "rstdc")
                                for k in range(8):
                                    P.op("dve", lambda e, b=b, k=k, n=n: e.scalar_tensor_tensor(out=rb[b][:, k, :n], in0=rb[b][:, k, :n], scalar=pc("fng", k), in1=rstdc[:, :n], op0=ALU.mult, op1=ALU.mult), reads=[f"r{b}", "rstdc", "pvt"], writes=[f"r{b}"])
                                P.dma("sp", lambda e, b=b, s0=s0, n=n: e.dma_start(out=outT.rearrange("(k p) s -> p k s", p=128)[:, :, s0 - LC:s0 - LC + n], in_=rb[b][:, :, :n]), reads=[f"r{b}"])
                            else:
                                P.dma("sp", lambda e, b=b, s0=s0, n=n: e.dma_start(out=res.rearrange("(k p) s -> p k s", p=128)[:, :, s0:s0 + n], in_=rb[b][:, :, :n]), reads=[f"r{b}"])
                        P.barrier(); P.emit()
                    continue

                with contextlib.ExitStack() as ph:
                    wg = [SB(ph, f"wg{i}", [128, 8, 2048], BF16) for i in range(2)]
                    wd = [SB(ph, f"wd{i}", [128, 8, 1024], BF16) for i in range(2)]
                    stg = [SB(ph, f"stg{i}", [128, 2048]) for i in range(3)]
                    hbuf = [SB(ph, f"hb{i}", [128, 8, 512], BF16) for i in range(2)]
                    act_ = SB(ph, "actT", [128, 8, 512], BF16)
                    gbc = [SB(ph, f"gbc{i}", [128, 512]) for i in range(2)]
                    gt = [SB(ph, f"gt{i}", [128, 512]) for i in range(2)]; sg = [SB(ph, f"sg{i}", [128, 512]) for i in range(2)]; up = [SB(ph, f"up{i}", [128, 512]) for i in range(2)]
                    rtb = [SB(ph, f"rt{i}", [128, 8, 512]) for i in range(2)]; ty = [SB(ph, f"ty{i}", [128, 512]) for i in range(2)]
                    scn = [0]

                    def wstep(ex_, k):
                        wb_ = ex_ % 2
                        s_ = scn[0] % 3; scn[0] += 1
                        if k < 8:
                            P.dma("act", lambda e: e.dma_start(out=stg[s_][:, :], in_=w_gu[l, ex_, k * 128:(k + 1) * 128, :]), writes=[f"stg{s_}"])
                            P.op("pool", lambda e: e.tensor_copy(out=wg[wb_][:, k, :], in_=stg[s_][:, :]), reads=[f"stg{s_}"], writes=[(f"wg{wb_}", k)])
                        else:
                            k2 = k - 8
                            P.dma("act", lambda e: e.dma_start(out=stg[s_][:, 0:1024], in_=w_down[l, ex_, k2 * 128:(k2 + 1) * 128, :]), writes=[f"stg{s_}"])
                            P.op("pool", lambda e: e.tensor_copy(out=wd[wb_][:, k2, :], in_=stg[s_][:, 0:1024]), reads=[f"stg{s_}"], writes=[(f"wd{wb_}", k2)])

                    work = [(ex_, j, ci, s0, n) for ex_ in range(n_exp) for j, (ci, (s0, n)) in enumerate(chs)]

                    def loads(i):
                        ex_, j, ci, s0, n = work[i]
                        b_ = i % 2
                        P.dma("sp", lambda e: e.dma_start(out=hbuf[b_][:, :, :n], in_=hT.rearrange("(k p) s -> p k s", p=128)[:, :, s0:s0 + n]), writes=[f"hb{b_}"])
                        P.dma("sp", lambda e: e.dma_start(out=gbc[b_][:, :n], in_=gTd[ex_:ex_ + 1, s0:s0 + n].partition_broadcast(128)), writes=[f"gbc{b_}"])
                        P.dma("sp", lambda e: e.dma_start(out=rtb[b_][:, :, :n], in_=res.rearrange("(k p) s -> p k s", p=128)[:, :, s0:s0 + n]), reads=[("res", ci)], writes=[f"rt{b_}"])

                    for k in range(16):
                        wstep(0, k)
                    loads(0)
                    nch = len(chs)
                    def compute(i):
                        ex_, j, ci, s0, n = work[i]
                        wb_ = ex_ % 2; b_ = i % 2
                        w = 1 if ci == 0 else 0
                        if ex_ + 1 < n_exp:
                            for k in range(16):
                                if k * nch // 16 == j:
                                    wstep(ex_ + 1, k)
                        if i + 1 < len(work):
                            loads(i + 1)
                        for fc in range(8):
                            q = fc % 2
                            for k in range(8):
                                P.op("pe", lambda e, q=q, fc=fc, k=k: e.matmul(PS[q][:, :n], lhsT=wg[wb_][:, k, fc * 128:(fc + 1) * 128], rhs=hbuf[b_][:, k, :n], start=(k == 0), stop=(k == 7)), reads=[(f"wg{wb_}", k), f"hb{b_}"], writes=[psn[q]])
                            for k in range(8):
                                P.op("pe", lambda e, q=q, fc=fc, k=k: e.matmul(PS[2 + q][:, :n], lhsT=wg[wb_][:, k, 1024 + fc * 128:1024 + (fc + 1) * 128], rhs=hbuf[b_][:, k, :n], start=(k == 0), stop=(k == 7)), reads=[(f"wg{wb_}", k), f"hb{b_}"], writes=[psn[2 + q]])
                            bg = pc("b_gu", ex_ * 16 + fc); bu = pc("b_gu", ex_ * 16 + 8 + fc)
                            P.op("dve", lambda e, q=q, bg=bg: e.tensor_scalar(out=gt[q][:, :n], in0=PS[q][:, :n], scalar1=bg, scalar2=7.0, op0=ALU.add, op1=ALU.min), reads=[psn[q], "pvt"], writes=[f"gt{q}"])
                            P.op("act", lambda e, q=q: e.activation(out=sg[q][:, :n], in_=gt[q][:, :n], func=AF.Sigmoid, scale=1.702), reads=[f"gt{q}"], writes=[f"sg{q}"])
                            P.op("act", lambda e, q=q, bu=bu: e.activation(out=up[q][:, :n], in_=PS[2 + q][:, :n], func=AF.Identity, bias=bu), reads=[psn[2 + q], "pvt"], writes=[f"up{q}"])
                            P.op("pool", lambda e, q=q: e.tensor_scalar(out=up[q][:, :n], in0=up[q][:, :n], scalar1=7.0, scalar2=-7.0, op0=ALU.min, op1=ALU.max), reads=[f"up{q}"], writes=[f"up{q}"])
                            P.op("pool", lambda e, q=q: e.tensor_tensor(out=gt[q][:, :n], in0=gt[q][:, :n], in1=sg[q][:, :n], op=ALU.mult), reads=[f"gt{q}", f"sg{q}"], writes=[f"gt{q}"])
                            P.op("dve", lambda e, q=q, fc=fc: e.scalar_tensor_tensor(out=act_[:, fc, :n], in0=up[q][:, :n], scalar=1.0, in1=gt[q][:, :n], op0=ALU.add, op1=ALU.mult), reads=[f"up{q}", f"gt{q}"], writes=[("actT", fc)])
                        for dc in range(8):
                            q = dc % 2
                            for fc in range(8):
                                P.op("pe", lambda e, q=q, dc=dc, fc=fc: e.matmul(PS[4 + q][:, :n], lhsT=wd[wb_][:, fc, dc * 128:(dc + 1) * 128], rhs=act_[:, fc, :n], start=(fc == 0), stop=(fc == 7)), reads=[(f"wd{wb_}", fc), ("actT", fc)], writes=[psn[4 + q]])
                            P.op("dve", lambda e, q=q, dc=dc: e.scalar_tensor_tensor(out=ty[q][:, :n], in0=PS[4 + q][:, :n], scalar=G2(dc, w), in1=gbc[b_][:, :n], op0=ALU.mult, op1=ALU.mult), reads=[psn[4 + q], "modT", f"gbc{b_}"], writes=[f"ty{q}"])
                            P.op("pool", lambda e, q=q, dc=dc: e.tensor_tensor(out=rtb[b_][:, dc, :n], in0=rtb[b_][:, dc, :n], in1=ty[q][:, :n], op=ALU.add), reads=[f"rt{b_}", f"ty{q}"], writes=[f"rt{b_}"])
                        P.dma("sp", lambda e: e.dma_start(out=res.rearrange("(k p) s -> p k s", p=128)[:, :, s0:s0 + n], in_=rtb[b_][:, :, :n]), reads=[f"rt{b_}"], writes=[("res", ci)])
                    for i in range(len(work)):
                        compute(i)
                    P.barrier(); P.emit()

        if stop_after is None and not (sparse and depth == DEPTH):
            with contextlib.ExitStack() as ph:
                pv = SB(ph, "pvf", [128, NPV])
                P.dma("sp", lambda e: e.dma_start(out=pv[:], in_=pvd[0]), writes=["pvf"])
                rb = [SB(ph, f"r{i}", [128, 8, 512]) for i in range(2)]; sqb = SB(ph, "sqb", [128, 8, 512], BF16); rstd = SB(ph, "rstd", [128, 512])
                ob = [SB(ph, f"of{i}", [128, 8, 512]) for i in range(2)]
                for ci, (s0, n) in list(enumerate(CH))[1:]:
                    b = ci % 2
                    P.dma("sp", lambda e, b=b, s0=s0, n=n: e.dma_start(out=rb[b][:, :, :n], in_=res.rearrange("(k p) s -> p k s", p=128)[:, :, s0:s0 + n]), writes=[f"r{b}"])
                    rms_rstd(rb[b], f"r{b}", sqb, "sqb", n, 7, rstd, "rstd")
                    for k in range(8):
                        P.op("dve", lambda e, b=b, k=k, n=n: e.scalar_tensor_tensor(out=ob[b][:, k, :n], in0=rb[b][:, k, :n], scalar=pv[:, PV["fng"] + k:PV["fng"] + k + 1], in1=rstd[:, :n], op0=ALU.mult, op1=ALU.mult), reads=[f"r{b}", "rstd", "pvf"], writes=[f"of{b}"])
                    P.dma("sp", lambda e, b=b, s0=s0, n=n: e.dma_start(out=outT.rearrange("(k p) s -> p k s", p=128)[:, :, s0 - LC:s0 - LC + n], in_=ob[b][:, :, :n]), reads=[f"of{b}"])
                P.barrier(); P.emit()
    return nc


_CONSTS = None


def make_in_maps(inputs, cores=range(8)):
    global _CONSTS
    if _CONSTS is None:
        _CONSTS = make_consts()
    c = _CONSTS
    f = lambda a: np.ascontiguousarray(np.asarray(a, np.float32))
    shared = dict(pos=c["pos"], w_mod=f(inputs["w_mod"]), pv=np.stack([pack_pv(inputs, l) for l in range(DEPTH)]),
                  bd=np.stack([pack_bd(inputs, l) for l in range(DEPTH)]), w_in=f(inputs["w_in"]), w_pw=f(inputs["w_pw"]),
                  w_out=f(inputs["w_out"]), w_router=f(inputs["w_router"]), b_router=f(inputs["b_router"]), w_gu=f(inputs["w_gu"]),
                  w_down=f(inputs["w_down"]), b_down=f(inputs["b_down"]), b_gu=f(inputs["b_gu"]), cst=c["cst"], dftx=c["dftx"], dftc=c["dftc"],
                  invc=c["invc"], iot=c["iot"])
    maps = []
    for b in cores:
        xin = np.ascontiguousarray(np.concatenate([inputs["ctx"][b], inputs["x"][b]], axis=0).T.astype(np.float32))
        cvec = np.stack([_cols(inputs["c"][b]), _cols(inputs["c_ctx"])], axis=-1)
        m = dict(shared)
        m["xin"] = xin
        m["cvec"] = np.ascontiguousarray(cvec.astype(np.float32))
        maps.append(m)
    return maps


def kernel(**inputs):
    inputs = {k: np.asarray(v) for k, v in inputs.items()}
    nc = build_nc()
    maps = make_in_maps(inputs)
    res = run_bass_kernel_spmd(nc, maps, core_ids=list(range(8)))
    out = np.stack([np.ascontiguousarray(r["outT"].T) for r in res.results], axis=0)
    return out.astype(np.float32)
```

```python
import contextlib
import numpy as np
import ml_dtypes
import concourse.bass as bass
import concourse.mybir as mybir
from concourse.bass_utils import run_bass_kernel_spmd

F32 = mybir.dt.float32
BF16 = mybir.dt.bfloat16
AF = mybir.ActivationFunctionType
ALU = mybir.AluOpType
AX = mybir.AxisListType

D = 1024
LC = 256
LX = 4096
S = LC + LX
PADW = 16
SP = S + 3 * PADW
NE = 32
DEPTH = 2
EPS = 1e-6
BLK = 256
NBLK = 100
NSLOT = NBLK * BLK
U32 = mybir.dt.uint32
I32 = mybir.dt.int32
CH = [(0, 256)] + [(256 + 512 * i, 512) for i in range(8)]


def pcol(s):
    return s + PADW if s < LC else s + 2 * PADW


PV = {}
_o = 0
for _n, _k in [("b_mod", 48), ("n1g", 8), ("n2g", 8), ("b_in", 12), ("caw", 16), ("cab", 4), ("brr", 4), ("bri", 4),
               ("lam", 4), ("bpool", 2), ("pscale", 2), ("bfour", 2), ("cdw", 62), ("cdb", 2), ("lng", 2), ("lnb", 2),
               ("bpw", 2), ("b_out", 8), ("b_gu", 512), ("fng", 8)]:
    PV[_n] = _o
    _o += _k
NPV = _o


def _cols(v):
    v = np.asarray(v, np.float32).reshape(-1)
    return v.reshape(-1, 128).T


def pack_pv(inp, l):
    pv = np.zeros((128, NPV), np.float32)

    def put(name, v, off=0):
        c = _cols(v)
        pv[:, PV[name] + off:PV[name] + off + c.shape[1]] = c

    put("b_mod", inp["b_mod"][l]); put("n1g", inp["norm1_g"][l]); put("n2g", inp["norm2_g"][l]); put("b_in", inp["b_in"][l])
    for d in range(2):
        for j in range(4):
            put("caw", inp["conv_a_w"][l, d, j], (d * 4 + j) * 2)
        put("cab", inp["conv_a_b"][l, d], d * 2)
        put("brr", inp["b_rg_r"][l, d], d * 2)
        put("bri", inp["b_rg_i"][l, d], d * 2)
        put("lam", inp["rg_lambda"][l, d], d * 2)
    put("bpool", inp["b_pool"][l]); put("pscale", inp["pool_scale"][l]); put("bfour", inp["b_four"][l])
    for j in range(31):
        put("cdw", inp["conv_d_w"][l, j], j * 2)
    put("cdb", inp["conv_d_b"][l]); put("lng", inp["ln_d_g"][l]); put("lnb", inp["ln_d_b"][l]); put("bpw", inp["b_pw"][l])
    put("b_out", inp["b_out"][l])
    for e in range(NE):
        put("b_gu", inp["b_gu"][l, e], e * 16)
    put("fng", inp["final_norm_g"])
    return pv


def pack_bd(inp, l):
    bd = np.zeros((128, 12, 128), np.float32)

    def blk(idx, w4, cg):
        for j in range(2):
            bd[64 * j:64 * j + 64, idx, 64 * j:64 * j + 64] = w4[2 * cg + j]

    for d in range(2):
        for cg in range(2):
            blk(d * 2 + cg, inp["w_rg_r"][l, d], cg)
            blk(4 + d * 2 + cg, inp["w_rg_i"][l, d], cg)
    for cg in range(2):
        blk(8 + cg, inp["w_pool"][l], cg)
        blk(10 + cg, inp["w_four"][l], cg)
    return bd


def make_consts():
    cst = np.zeros((128, 6, 128), np.float32)
    c = np.arange(64)
    ang = 2 * np.pi * np.outer(c, c) / 64.0
    for j in range(2):
        cst[64 * j:64 * j + 64, 0, 64 * j:64 * j + 64] = np.cos(ang) / 8.0
        cst[64 * j:64 * j + 64, 1, 64 * j:64 * j + 64] = -np.sin(ang) / 8.0
        cst[64 * j:64 * j + 64, 2, 64 * j:64 * j + 64] = 1.0 / 64.0
    cst[:, 3, :] = 1.0
    cst[:, 4, :] = np.eye(128)
    cst[:, 5, :] = np.triu(np.ones((128, 128), np.float32), 1)
    iot = np.zeros((128, 33 + NBLK), np.float32)
    iot[:, 32 + NBLK] = np.arange(128)
    iot[:, :32] = np.arange(32)[None, :]
    iot[:, 32:32 + NBLK] = (np.arange(NBLK) * BLK)[None, :]

    def dft(L):
        k = np.arange(L, dtype=np.int64)
        a = 2 * np.pi * ((np.outer(k, k) % L).astype(np.float64)) / L
        return np.stack([np.cos(a), np.sin(a)]).astype(np.float32) / np.sqrt(L)

    def tile_tab(t, L):
        nlt, nk = L // 128, L // 256
        return np.ascontiguousarray(t.reshape(2, nlt, 128, nk, 256).transpose(0, 3, 2, 1, 4))

    dftx = tile_tab(dft(LX), LX).astype(ml_dtypes.bfloat16)
    dftc = tile_tab(dft(LC), LC).astype(ml_dtypes.bfloat16)
    dalt = np.zeros((128, LX // 128, 2), np.float32)
    dalt[:, :, 0] = (((-1.0) ** np.arange(128)) / np.sqrt(LX))[:, None]
    dalt = dalt.astype(ml_dtypes.bfloat16)
    invc = np.ones((2, 128, SP), np.float32)
    for g, w in enumerate((2, 4, 8, 16)):
        for (c0, L) in ((PADW, LC), (2 * PADW + LC, LX)):
            t = np.arange(L)
            lo = np.clip(t - w // 2, 0, L)
            hi = np.clip(t + w - w // 2, 0, L)
            invc[g // 2, 64 * (g % 2):64 * (g % 2) + 64, c0:c0 + L] = 1.0 / (hi - lo)
    rows_n = LX // 64
    row = np.repeat(np.arange(rows_n), 64).astype(np.float32)
    col = np.tile(np.arange(64), rows_n).astype(np.float32)
    q = D // 4
    omega = (1.0 / (10000.0 ** (np.arange(q, dtype=np.float32) / q))).astype(np.float32)

    def emb(p):
        a = p[:, None] * omega[None, :]
        return np.concatenate([np.sin(a), np.cos(a)], axis=-1)

    pos = np.concatenate([emb(row), emb(col)], axis=-1).astype(np.float32)
    return dict(cst=cst, dftx=dftx, dftc=dftc, invc=invc, iot=iot, dalt=dalt, pos=np.ascontiguousarray(pos.T))


class Prog:
    ENG = ("pe", "dve", "act", "pool", "sp")
    KD = 8

    def __init__(self, nc, stack):
        self.nc = nc
        self.ops = {e: [] for e in self.ENG}
        self.cnt = {e: 0 for e in self.ENG}
        self.sems = {}
        for e in ("pe", "dve", "act", "pool"):
            self.sems[("e", e)] = stack.enter_context(nc.semaphore("s_" + e))
        for q in ("sp", "pool", "act"):
            for i in range(self.KD):
                self.sems[("d", q, i)] = stack.enter_context(nc.semaphore(f"d_{q}{i}"))
        self.dcnt = {q: 0 for q in ("sp", "pool", "act")}
        self.waited = {e: {} for e in self.ENG}
        self.state = {}
        self.bg = None
        self.bg_every = 12
        self._bgk = 0
        self._in_bg = False

    def _bg_step(self):
        self._in_bg = True
        try:
            next(self.bg)
        except StopIteration:
            self.bg = None
        self._in_bg = False

    def _tick(self):
        if self.bg is None or self._in_bg:
            return
        self._bgk += 1
        if self._bgk % self.bg_every == 0:
            self._bg_step()

    def drain(self):
        while self.bg is not None:
            self._bg_step()

    def _st(self, name, reg):
        d = self.state.setdefault(name, {})
        if reg not in d:
            d[reg] = {"w": None, "r": {}}
        return d[reg]

    def _states(self, name, reg):
        d = self.state.get(name, {})
        if reg is None:
            return list(d.values())
        out = []
        if reg in d:
            out.append(d[reg])
        if None in d:
            out.append(d[None])
        return out

    def _deps(self, reads, writes):
        evs = []
        for (name, reg) in reads:
            for st in self._states(name, reg):
                if st["w"] is not None:
                    evs.append(st["w"])
        for (name, reg) in writes:
            for st in self._states(name, reg):
                if st["w"] is not None:
                    evs.append(st["w"])
                evs.extend(st["r"].items())
        return evs

    def _commit(self, ev, reads, writes):
        for (name, reg) in reads:
            r = self._st(name, reg)["r"]
            if r.get(ev[0], 0) < ev[1]:
                r[ev[0]] = ev[1]
        for (name, reg) in writes:
            if reg is None:
                self.state[name] = {None: {"w": ev, "r": {}}}
            else:
                st = self._st(name, reg)
                st["w"] = ev
                st["r"] = {}

    def _waits(self, eng, evs):
        out = {}
        w = self.waited[eng]
        for (sk, v) in evs:
            if sk == ("e", "pe") and eng == "pe":
                continue
            if w.get(sk, 0) >= v:
                continue
            if out.get(sk, 0) < v:
                out[sk] = v
        for sk, v in out.items():
            w[sk] = v
        return list(out.items())

    @staticmethod
    def _keys(ks):
        return [(k, None) if isinstance(k, str) else (k[0], k[1]) for k in ks]

    def op(self, eng, fn, reads=(), writes=()):
        reads = self._keys(reads)
        writes = self._keys(writes)
        waits = self._waits(eng, self._deps(reads, writes))
        self.cnt[eng] += 1
        ev = (("e", eng), self.cnt[eng])
        self.ops[eng].append((waits, fn, ev, 1))
        self._commit(ev, reads, writes)
        self._tick()

    def dma(self, q, fn, reads=(), writes=()):
        reads = self._keys(reads)
        writes = self._keys(writes)
        n = self.dcnt[q]
        self.dcnt[q] += 1
        i = n % self.KD
        val = 16 * (n // self.KD + 1)
        evs = self._deps(reads, writes)
        if n >= self.KD:
            evs.append((("d", q, i), val - 16))
        waits = self._waits(q, evs)
        ev = (("d", q, i), val)
        self.ops[q].append((waits, fn, ev, 16))
        self._commit(ev, reads, writes)
        self._tick()

    def _all_events(self):
        evs = []
        for q in ("sp", "pool", "act"):
            n = self.dcnt[q]
            for i in range(self.KD):
                if n > i:
                    evs.append((("d", q, i), 16 * ((n - i + self.KD - 1) // self.KD)))
        for e in ("pe", "dve", "act", "pool"):
            if self.cnt[e]:
                evs.append((("e", e), self.cnt[e]))
        return evs

    def barrier(self):
        evs = self._all_events()
        for eng in self.ENG:
            waits = self._waits(eng, [ev for ev in evs if ev[0] != ("e", eng)])
            if waits:
                self.ops[eng].append((waits, None, None, 0))
        self.state = {}

    def emit(self):
        nc = self.nc
        sems = self.sems
        ops = self.ops
        self.ops = {e: [] for e in self.ENG}

        def run(name, e):
            for waits, fn, ev, inc in ops[name]:
                for (sk, v) in waits:
                    e.wait_ge(sems[sk], v)
                if fn is None:
                    continue
                fn(e).then_inc(sems[ev[0]], inc)

        with nc.Block() as block:
            @block.tensor
            def _(e):
                run("pe", e)

            @block.vector
            def _(e):
                run("dve", e)

            @block.scalar
            def _(e):
                run("act", e)

            @block.gpsimd
            def _(e):
                run("pool", e)

            @block.sync
            def _(e):
                run("sp", e)


def build_nc(debug=False, stop_after=None, depth=DEPTH, n_exp=NE, sparse=True):
    nc = bass.Bass("TRN2", target_bir_lowering=False)
    I = lambda n, s, d=F32: nc.dram_tensor(n, s, d, kind="ExternalInput").ap()
    xin = I("xin", [D, S]); cvec = I("cvec", [128, 8, 2]); pos = I("pos", [D, LX])
    w_mod = I("w_mod", [DEPTH, D, 6 * D]); pvd = I("pv", [DEPTH, 128, NPV]); bdd = I("bd", [DEPTH, 128, 12, 128])
    w_in = I("w_in", [DEPTH, D, 1536]); w_pw = I("w_pw", [DEPTH, 256, 256]); w_out = I("w_out", [DEPTH, D, D])
    w_router = I("w_router", [DEPTH, D, NE]); b_router = I("b_router", [DEPTH, NE])
    w_gu = I("w_gu", [DEPTH, n_exp, D, 2 * D]); w_down = I("w_down", [DEPTH, n_exp, D, D]); b_down = I("b_down", [DEPTH, NE, D])
    cstd = I("cst", [128, 6, 128]); dftx = I("dftx", [2, LX // 256, 128, LX // 128, 256], BF16); dftc = I("dftc", [2, LC // 256, 128, LC // 128, 256], BF16)
    invcd = I("invc", [2, 128, SP]); iotd = I("iot", [128, 33 + NBLK]); daltd = I("dalt", [128, LX // 128, 2], BF16); b_gu_d = I("b_gu", [DEPTH, NE, 2 * D])
    outT = nc.dram_tensor("outT", [D, LX], F32, kind="ExternalOutput").ap()
    sk = "ExternalOutput" if debug else "Internal"
    res = nc.dram_tensor("res", [D, S], F32, kind=sk).ap()
    proj = nc.dram_tensor("proj", [1536, S], F32, kind=sk).ap()
    ycat = nc.dram_tensor("ycat", [D, S], BF16, kind=sk).ap()
    hT = nc.dram_tensor("hT", [D, S], BF16, kind=sk).ap()
    gTd = nc.dram_tensor("gTd", [NE, S], F32, kind=sk).ap()
    WGbs = [nc.dram_tensor(f"WGb{i}", [NE, 128, 9, 2048], BF16).ap() for i in range(DEPTH)]
    WDbs = [nc.dram_tensor(f"WDb{i}", [NE, 128, 8, 1024], BF16).ap() for i in range(DEPTH)]
    htm = nc.dram_tensor("htm", [S, D], BF16, kind=sk).ap(); xs_d = nc.dram_tensor("xs_d", [NSLOT, D], BF16, kind=sk).ap()
    ys_d = nc.dram_tensor("ys_d", [NSLOT, D], F32, kind=sk).ap()

    with contextlib.ExitStack() as top:
        P = Prog(nc, top)
        PS = [top.enter_context(nc.psum_tensor(f"ps{i}", [128, 512], F32)) for i in range(8)]
        psn = [f"ps{i}" for i in range(8)]

        _uid = [0]

        def SB(st, n, s, d=F32):
            _uid[0] += 1
            return st.enter_context(nc.sbuf_tensor(f"sb{_uid[0]}_{n}", s, d))

        cst = SB(top, "cst", [128, 6, 128]); cstb = SB(top, "cstb", [128, 6, 128], BF16)
        iot = SB(top, "iot", [128, 33 + NBLK])
        P.dma("sp", lambda e: e.dma_start(out=iot[:], in_=iotd), writes=["iot"])
        def ind_dma(e, **kw):
            return e.indirect_dma_start(**kw)

        rB = top.enter_context(nc.gpsimd.register("rB"))
        vbox = {}
        dmy = SB(top, "dmy", [128, 8])

        def init_pool(e):
            e.reg_mov(rB, NE * 128 - 1)
            vbox["v"] = e.snap(rB, donate=True)
            return e.memset(dmy[:], 0.0)

        P.op("pool", init_pool, writes=["dmy"])

        def precast_gen(l):
            WGb, WDb = WGbs[l], WDbs[l]
            P.dma("pool", lambda e: e.dma_start(out=WGb[:, 0, 8, :], in_=b_gu_d[l]))
            yield
            for ex_ in range(n_exp):
                P.dma("pool", lambda e, ex_=ex_: e.dma_start(out=WGb[ex_, :, 0:8, :], in_=w_gu[l, ex_].rearrange("(k p) f -> p k f", p=128)))
                yield
                P.dma("pool", lambda e, ex_=ex_: e.dma_start(out=WDb[ex_, :, :, :], in_=w_down[l, ex_].rearrange("(k p) f -> p k f", p=128)))
                yield

        if sparse and stop_after is None:
            P.bg = precast_gen(0)
            P.bg_every = 100
        P.dma("sp", lambda e: e.dma_start(out=cst[:], in_=cstd), writes=["cst"])
        P.op("dve", lambda e: e.tensor_copy(out=cstb[:], in_=cst[:]), reads=["cst"], writes=["cstb"])
        C64b, S64b, MAVb, ONEb, IDb, UTb = (cstb[:, i, :] for i in range(6))
        ID32 = cst[:, 4, :]
        cv = SB(top, "cv", [128, 8, 2])
        P.dma("sp", lambda e: e.dma_start(out=cv[:], in_=cvec), writes=["cv"])
        P.op("act", lambda e: e.activation(out=cv[:], in_=cv[:], func=AF.Silu), reads=["cv"], writes=["cv"])
        P.barrier(); P.emit()

        def rms_rstd(st_r, rk, sqb, sqk, n, psi, rstd, rstdk):
            P.op("act", lambda e: e.activation(out=sqb[:, :, :n], in_=st_r[:, :, :n], func=AF.Square), reads=[rk], writes=[sqk])
            for k in range(8):
                P.op("pe", lambda e, k=k: e.matmul(PS[psi][:, :n], lhsT=ONEb, rhs=sqb[:, k, :n], start=(k == 0), stop=(k == 7)),
                     reads=[sqk, "cstb"], writes=[psn[psi]])
            P.op("dve", lambda e: e.tensor_scalar(out=rstd[:, :n], in0=PS[psi][:, :n], scalar1=1.0 / D, scalar2=EPS, op0=ALU.mult, op1=ALU.add),
                 reads=[psn[psi]], writes=[rstdk])
            P.op("act", lambda e: e.activation(out=rstd[:, :n], in_=rstd[:, :n], func=AF.Sqrt), reads=[rstdk], writes=[rstdk])
            P.op("dve", lambda e: e.reciprocal(out=rstd[:, :n], in_=rstd[:, :n]), reads=[rstdk], writes=[rstdk])

        for l in range(depth):
            last = (l == DEPTH - 1)
            with contextlib.ExitStack() as lay:
                pv = SB(lay, "pvt", [128, NPV]); modT = SB(lay, "modT", [128, 48, 2]); A1 = SB(lay, "A1", [128, 8, 2]); A2 = SB(lay, "A2", [128, 8, 2])
                P.dma("sp", lambda e: e.dma_start(out=pv[:], in_=pvd[l]), writes=["pvt"])
                pc = lambda name, i=0: pv[:, PV[name] + i:PV[name] + i + 1]
                NT = S // 128
                idxf = SB(lay, "idxf", [128, NT, 4]); g4 = SB(lay, "g4", [128, NT, 4]); rank4 = SB(lay, "rank4", [128, NT, 4]); carry = SB(lay, "carry", [128, NE])
                destu = SB(lay, "destu", [128, NT * 4], U32); widx = SB(lay, "widx", [128, NBLK], U32)
                WGb, WDb = WGbs[l], WDbs[l]
                with contextlib.ExitStack() as ph:
                    wm = [SB(ph, f"wm{i}", [128, 8, 768]) for i in range(2)]
                    mrow = SB(ph, "mrow", [2, 6 * D])
                    for q in range(8):
                        b = q % 2
                        P.dma("sp", lambda e, b=b, q=q: e.dma_start(out=wm[b][:], in_=w_mod[l].rearrange("(k p) n -> p k n", p=128)[:, :, q * 768:(q + 1) * 768]), writes=[f"wm{b}"])
                        for hh in range(2):
                            pi = 1 + hh
                            for k in range(8):
                                P.op("pe", lambda e, b=b, hh=hh, pi=pi, k=k: e.matmul(PS[pi][0:2, 0:384], lhsT=cv[:, k, :], rhs=wm[b][:, k, hh * 384:(hh + 1) * 384], start=(k == 0), stop=(k == 7)),
                                     reads=[f"wm{b}", "cv"], writes=[psn[pi]])
                            P.op("act", lambda e, q=q, hh=hh, pi=pi: e.activation(out=mrow[:, q * 768 + hh * 384:q * 768 + (hh + 1) * 384], in_=PS[pi][0:2, 0:384], func=AF.Copy), reads=[psn[pi]], writes=["mrow"])
                    for j in range(48):
                        P.op("pe", lambda e, j=j: e.transpose(out=PS[0][:, 2 * j:2 * j + 2], in_=mrow[0:2, j * 128:(j + 1) * 128], identity=cst[0:2, 4, 0:2]), reads=["mrow", "cst"], writes=["ps0"])
                    psm = PS[0][:, 0:96].rearrange("p (j w) -> p j w", w=2)
                    for w in range(2):
                        P.op("dve", lambda e, w=w: e.tensor_tensor(out=modT[:, :, w], in0=psm[:, :, w], in1=pv[:, PV["b_mod"]:PV["b_mod"] + 48], op=ALU.add), reads=["ps0", "pvt"], writes=["modT"])
                        for (A, sc0, gn) in ((A1, 8, "n1g"), (A2, 32, "n2g")):
                            P.op("dve", lambda e, w=w, A=A, sc0=sc0: e.tensor_scalar(out=A[:, :, w], in0=modT[:, sc0:sc0 + 8, w], scalar1=1.0, scalar2=None, op0=ALU.add), reads=["modT"], writes=["A"])
                            P.op("dve", lambda e, w=w, A=A, gn=gn: e.tensor_tensor(out=A[:, :, w], in0=A[:, :, w], in1=pv[:, PV[gn]:PV[gn] + 8], op=ALU.mult), reads=["A", "pvt"], writes=["A"])
                    P.barrier(); P.emit()
                SH1 = lambda k, w: modT[:, k, w:w + 1]
                G1 = lambda k, w: modT[:, 16 + k, w:w + 1]
                SH2 = lambda k, w: modT[:, 24 + k, w:w + 1]
                G2 = lambda k, w: modT[:, 40 + k, w:w + 1]

                with contextlib.ExitStack() as ph:
                    wst = SB(ph, "wst", [128, 8, 1536]); wib = SB(ph, "wib", [128, 8, 1536], BF16)
                    P.dma("sp", lambda e: e.dma_start(out=wst[:], in_=w_in[l].rearrange("(k p) n -> p k n", p=128)), writes=["wst"])
                    for k in range(8):
                        P.op("pool", lambda e, k=k: e.tensor_copy(out=wib[:, k, :], in_=wst[:, k, :]), reads=["wst"], writes=[("wib", k)])
                    rb = [SB(ph, f"r{i}", [128, 8, 512]) for i in range(2)]
                    posb = [SB(ph, f"posb{i}", [128, 8, 512]) for i in range(2)] if l == 0 else None
                    sqb = SB(ph, "sqb", [128, 8, 512], BF16); tmp = SB(ph, "tmp", [128, 8, 512])
                    ub = [SB(ph, f"u{i}", [128, 8, 512], BF16) for i in range(2)]
                    rstd = SB(ph, "rstd", [128, 512]); ot = [SB(ph, f"ot{i}", [128, 512]) for i in range(4)]
                    oc = 0
                    for ci, (s0, n) in enumerate(CH):
                        b = ci % 2; w = 1 if ci == 0 else 0
                        if l == 0:
                            P.dma("sp", lambda e, b=b, s0=s0, n=n: e.dma_start(out=rb[b][:, :, :n], in_=xin.rearrange("(k p) s -> p k s", p=128)[:, :, s0:s0 + n]), writes=[f"r{b}"])
                            if ci > 0:
                                P.dma("act", lambda e, b=b, s0=s0, n=n: e.dma_start(out=posb[b][:, :, :n], in_=pos.rearrange("(k p) s -> p k s", p=128)[:, :, s0 - LC:s0 - LC + n]), writes=[f"posb{b}"])
                                P.op("pool", lambda e, b=b, n=n: e.tensor_tensor(out=rb[b][:, :, :n], in0=rb[b][:, :, :n], in1=posb[b][:, :, :n], op=ALU.add), reads=[f"r{b}", f"posb{b}"], writes=[f"r{b}"])
                            P.dma("act", lambda e, b=b, s0=s0, n=n: e.dma_start(out=res.rearrange("(k p) s -> p k s", p=128)[:, :, s0:s0 + n], in_=rb[b][:, :, :n]), reads=[f"r{b}"])
                        else:
                            P.dma("sp", lambda e, b=b, s0=s0, n=n: e.dma_start(out=rb[b][:, :, :n], in_=res.rearrange("(k p) s -> p k s", p=128)[:, :, s0:s0 + n]), writes=[f"r{b}"])
                        rms_rstd(rb[b], f"r{b}", sqb, "sqb", n, 7, rstd, "rstd")
                        for k in range(8):
                            P.op("dve", lambda e, b=b, k=k, n=n: e.tensor_tensor(out=tmp[:, k, :n], in0=rb[b][:, k, :n], in1=rstd[:, :n], op=ALU.mult), reads=[f"r{b}", "rstd"], writes=[("tmp", k)])
                            P.op("act", lambda e, b=b, k=k, n=n, w=w: e.activation(out=ub[b][:, k, :n], in_=tmp[:, k, :n], func=AF.Identity, scale=A1[:, k, w:w + 1], bias=SH1(k, w)), reads=[("tmp", k), "A", "modT"], writes=[(f"u{b}", k)])
                        for fc in range(12):
                            pi = fc % 4
                            for k in range(8):
                                P.op("pe", lambda e, b=b, k=k, fc=fc, pi=pi, n=n: e.matmul(PS[pi][:, :n], lhsT=wib[:, k, fc * 128:(fc + 1) * 128], rhs=ub[b][:, k, :n], start=(k == 0), stop=(k == 7)),
                                     reads=[(f"u{b}", k), ("wib", k)], writes=[psn[pi]])
                            o = oc % 4; oc += 1
                            P.op("act", lambda e, o=o, pi=pi, fc=fc, n=n: e.activation(out=ot[o][:, :n], in_=PS[pi][:, :n], func=AF.Identity, bias=pc("b_in", fc)), reads=[psn[pi], "pvt"], writes=[f"ot{o}"])
                            P.dma("sp", lambda e, o=o, fc=fc, s0=s0, n=n: e.dma_start(out=proj[fc * 128:(fc + 1) * 128, s0:s0 + n], in_=ot[o][:, :n]), reads=[f"ot{o}"])
                    P.barrier(); P.emit()
                if stop_after == "inproj":
                    break

                PCH = [(pcol(s0), n) for (s0, n) in CH]
                mixs = contextlib.ExitStack()
                bdst = SB(mixs, "bdst", [128, 12, 128]); bdb = SB(mixs, "bdb", [128, 12, 128], BF16)
                P.dma("sp", lambda e: e.dma_start(out=bdst[:], in_=bdd[l]), writes=["bdst"])
                P.op("dve", lambda e: e.tensor_copy(out=bdb[:], in_=bdst[:]), reads=["bdst"], writes=["bdb"])

                def load_pad(t, tk, row0, eng="sp"):
                    P.op("pool", lambda e: e.memset(t[:], 0.0), writes=[tk])
                    P.dma(eng, lambda e: e.dma_start(out=t[:, PADW:PADW + LC], in_=proj[row0:row0 + 128, 0:LC]), writes=[tk])
                    P.dma(eng, lambda e: e.dma_start(out=t[:, 2 * PADW + LC:2 * PADW + S], in_=proj[row0:row0 + 128, LC:S]), writes=[tk])

                def store_seg(t, tk, row0):
                    P.dma("sp", lambda e: e.dma_start(out=ycat[row0:row0 + 128, 0:LC], in_=t[:, PADW:PADW + LC]), reads=[tk])
                    P.dma("sp", lambda e: e.dma_start(out=ycat[row0:row0 + 128, LC:S], in_=t[:, 2 * PADW + LC:2 * PADW + S]), reads=[tk])

                with contextlib.ExitStack() as ph:
                    xa = SB(ph, "xa", [128, SP]); xc = SB(ph, "xc", [128, SP]); xcb = SB(ph, "xcb", [128, SP], BF16)
                    rg = SB(ph, "rg", [128, SP]); ig = SB(ph, "ig", [128, SP]); aa = SB(ph, "aa", [128, SP]); bt = SB(ph, "bt", [128, SP])
                    hh = [SB(ph, f"hh{i}", [128, SP]) for i in range(2)]; ga = rg; yab = SB(ph, "yab", [128, SP], BF16)
                    sm = SB(ph, "sm", [128, 4])
                    c0, c1 = PADW, SP - PADW
                    for cg in range(2):
                        load_pad(xa, "xa", cg * 128)
                        for d in range(2):
                            for j in range(4):
                                o = (j - 3) if d == 0 else (3 - j)
                                wj = pc("caw", (d * 4 + j) * 2 + cg)
                                if j == 0:
                                    P.op("dve", lambda e, o=o, wj=wj, d=d, cg=cg: e.tensor_scalar(out=xc[:, c0:c1], in0=xa[:, c0 + o:c1 + o], scalar1=wj, scalar2=pc("cab", d * 2 + cg), op0=ALU.mult, op1=ALU.add), reads=["xa", "pvt"], writes=["xc"])
                                else:
                                    P.op("dve", lambda e, o=o, wj=wj: e.scalar_tensor_tensor(out=xc[:, c0:c1], in0=xa[:, c0 + o:c1 + o], scalar=wj, in1=xc[:, c0:c1], op0=ALU.mult, op1=ALU.add), reads=["xa", "xc", "pvt"], writes=["xc"])
                            P.op("pool", lambda e: e.tensor_copy(out=xcb[:, c0:c1], in_=xc[:, c0:c1]), reads=["xc"], writes=["xcb"])
                            for gi, (gt_, gk, bn) in enumerate(((rg, "rg", "brr"), (ig, "ig", "bri"))):
                                for qi, (p0, n) in enumerate(PCH):
                                    pi = (gi * 9 + qi) % 4
                                    P.op("pe", lambda e, pi=pi, gi=gi, d=d, cg=cg, p0=p0, n=n: e.matmul(PS[pi][:, :n], lhsT=bdb[:, gi * 4 + d * 2 + cg, :], rhs=xcb[:, p0:p0 + n], start=True, stop=True), reads=["xcb", "bdb"], writes=[psn[pi]])
                                    P.op("act", lambda e, pi=pi, gt_=gt_, bn=bn, d=d, cg=cg, p0=p0, n=n: e.activation(out=gt_[:, p0:p0 + n], in_=PS[pi][:, :n], func=AF.Sigmoid, bias=pc(bn, d * 2 + cg)), reads=[psn[pi], "pvt"], writes=[(gk, qi)])
                            P.op("act", lambda e, d=d, cg=cg: e.activation(out=sm[:, 0:1], in_=pc("lam", d * 2 + cg), func=AF.Exp, scale=-1.0), reads=["pvt"], writes=["sm"])
                            P.op("dve", lambda e: e.tensor_scalar(out=sm[:, 0:1], in0=sm[:, 0:1], scalar1=1.0, scalar2=None, op0=ALU.add), reads=["sm"], writes=["sm"])
                            P.op("act", lambda e: e.activation(out=sm[:, 1:2], in_=sm[:, 0:1], func=AF.Ln), reads=["sm"], writes=["sm"])
                            P.op("dve", lambda e: e.tensor_scalar(out=sm[:, 2:3], in0=sm[:, 1:2], scalar1=-8.0, scalar2=None, op0=ALU.mult), reads=["sm"], writes=["sm"])
                            P.op("act", lambda e: e.activation(out=aa[:, c0:c1], in_=rg[:, c0:c1], func=AF.Exp, scale=sm[:, 2:3]), reads=["rg", "sm"], writes=["aa"])
                            P.op("pool", lambda e: e.tensor_tensor(out=bt[:, c0:c1], in0=aa[:, c0:c1], in1=aa[:, c0:c1], op=ALU.mult), reads=["aa"], writes=["bt"])
                            P.op("dve", lambda e: e.tensor_scalar(out=bt[:, c0:c1], in0=bt[:, c0:c1], scalar1=-1.0, scalar2=1.0, op0=ALU.mult, op1=ALU.add), reads=["bt"], writes=["bt"])
                            P.op("act", lambda e: e.activation(out=bt[:, c0:c1], in_=bt[:, c0:c1], func=AF.Sqrt), reads=["bt"], writes=["bt"])
                            P.op("pool", lambda e: e.tensor_tensor(out=ig[:, c0:c1], in0=ig[:, c0:c1], in1=xc[:, c0:c1], op=ALU.mult), reads=["ig", "xc"], writes=["ig"])
                            P.op("dve", lambda e: e.tensor_tensor(out=bt[:, c0:c1], in0=bt[:, c0:c1], in1=ig[:, c0:c1], op=ALU.mult), reads=["bt", "ig"], writes=["bt"])
                            h = hh[d]; hk = f"hh{d}"
                            sc_, sx_ = slice(PADW, PADW + LC), slice(2 * PADW + LC, 2 * PADW + S)
                            if d == 0:
                                P.op("dve", lambda e, h=h: e.tensor_tensor_scan(out=h[:, sc_], data0=aa[:, sc_], data1=bt[:, sc_], initial=0.0, op0=ALU.mult, op1=ALU.add), reads=["aa", "bt"], writes=[hk])
                                P.op("dve", lambda e, h=h: e.tensor_tensor_scan(out=h[:, sx_], data0=aa[:, sx_], data1=bt[:, sx_], initial=h[:, PADW + LC - 1:PADW + LC], op0=ALU.mult, op1=ALU.add), reads=["aa", "bt", hk], writes=[hk])
                            else:
                                P.op("dve", lambda e, h=h: e.tensor_tensor_scan(out=h[:, sc_][:, ::-1], data0=aa[:, sc_][:, ::-1], data1=bt[:, sc_][:, ::-1], initial=0.0, op0=ALU.mult, op1=ALU.add), reads=["aa", "bt"], writes=[hk])
                                P.op("dve", lambda e, h=h: e.tensor_tensor_scan(out=h[:, sx_][:, ::-1], data0=aa[:, sx_][:, ::-1], data1=bt[:, sx_][:, ::-1], initial=h[:, PADW:PADW + 1], op0=ALU.mult, op1=ALU.add), reads=["aa", "bt", hk], writes=[hk])
                        load_pad(ga, "rg", 256 + cg * 128)
                        P.op("pool", lambda e: e.tensor_tensor(out=hh[0][:, c0:c1], in0=hh[0][:, c0:c1], in1=hh[1][:, c0:c1], op=ALU.add), reads=["hh0", "hh1"], writes=["hh0"])
                        P.op("pool", lambda e: e.tensor_tensor(out=aa[:, c0:c1], in0=ga[:, c0:c1], in1=ga[:, c0:c1], op=ALU.mult), reads=["rg"], writes=["aa"])
                        P.op("dve", lambda e: e.tensor_scalar(out=aa[:, c0:c1], in0=aa[:, c0:c1], scalar1=0.044715, scalar2=1.0, op0=ALU.mult, op1=ALU.add), reads=["aa"], writes=["aa"])
                        P.op("pool", lambda e: e.tensor_tensor(out=aa[:, c0:c1], in0=aa[:, c0:c1], in1=ga[:, c0:c1], op=ALU.mult), reads=["aa", "rg"], writes=["aa"])
                        P.op("act", lambda e: e.activation(out=aa[:, c0:c1], in_=aa[:, c0:c1], func=AF.Sigmoid, scale=1.5957691216057308), reads=["aa"], writes=["aa"])
                        P.op("dve", lambda e: e.tensor_tensor(out=aa[:, c0:c1], in0=aa[:, c0:c1], in1=ga[:, c0:c1], op=ALU.mult), reads=["aa", "rg"], writes=["aa"])
                        P.op("dve", lambda e: e.tensor_tensor(out=yab[:, c0:c1], in0=aa[:, c0:c1], in1=hh[0][:, c0:c1], op=ALU.mult), reads=["aa", "hh0"], writes=["yab"])
                        store_seg(yab, "yab", cg * 128)
                    P.barrier(); P.emit()

                with contextlib.ExitStack() as ph:
                    xb = SB(ph, "xb", [128, SP]); wa = SB(ph, "wa", [128, SP]); wb = SB(ph, "wb", [128, SP]); ivc = SB(ph, "ivc", [128, SP])
                    pbf = SB(ph, "pbf", [128, SP], BF16); ob = [SB(ph, f"ob{i}", [128, 512], BF16) for i in range(2)]; sm = SB(ph, "smb", [128, 2])
                    for cg in range(2):
                        load_pad(xb, "xb", 512 + cg * 128)
                        P.dma("sp", lambda e, cg=cg: e.dma_start(out=ivc[:], in_=invcd[cg]), writes=["ivc"])
                        P.op("pool", lambda e: e.memset(wa[:], 0.0), writes=["wa"])
                        P.op("pool", lambda e: e.memset(wb[:], 0.0), writes=["wb"])
                        P.op("dve", lambda e: e.tensor_tensor(out=wa[:, 1:SP], in0=xb[:, 0:SP - 1], in1=xb[:, 1:SP], op=ALU.add), reads=["xb"], writes=["wa"])
                        P.op("dve", lambda e: e.tensor_tensor(out=wb[:, 1:SP - 1], in0=wa[:, 0:SP - 2], in1=wa[:, 2:SP], op=ALU.add), reads=["wa"], writes=["wb"])
                        if cg == 1:
                            P.op("dve", lambda e: e.tensor_tensor(out=wa[:, 3:SP - 3], in0=wb[:, 1:SP - 5], in1=wb[:, 5:SP - 1], op=ALU.add), reads=["wb"], writes=["wa"])
                            P.op("dve", lambda e: e.tensor_tensor(out=wb[:, 7:SP - 7], in0=wa[:, 3:SP - 11], in1=wa[:, 11:SP - 3], op=ALU.add), reads=["wa"], writes=["wb"])
                        c0, c1 = PADW, SP - PADW
                        P.op("dve", lambda e: e.tensor_tensor(out=wa[0:64, c0:c1], in0=wa[0:64, c0:c1], in1=ivc[0:64, c0:c1], op=ALU.mult), reads=["wa", "ivc"], writes=["wa"])
                        P.op("dve", lambda e: e.tensor_tensor(out=wa[64:128, c0:c1], in0=wb[64:128, c0:c1], in1=ivc[64:128, c0:c1], op=ALU.mult), reads=["wb", "wa", "ivc"], writes=["wa"])
                        P.op("dve", lambda e: e.tensor_tensor(out=pbf[:, c0:c1], in0=wa[:, c0:c1], in1=xb[:, c0:c1], op=ALU.subtract), reads=["wa", "xb"], writes=["pbf"])
                        P.op("dve", lambda e, cg=cg: e.tensor_tensor(out=sm[:, 0:1], in0=pc("bpool", cg), in1=pc("pscale", cg), op=ALU.mult), reads=["pvt"], writes=["smb"])
                        for qi, (p0, n) in enumerate(PCH):
                            pi = qi % 4; o = qi % 2; s0 = CH[qi][0]
                            P.op("pe", lambda e, pi=pi, cg=cg, p0=p0, n=n: e.matmul(PS[pi][:, :n], lhsT=bdb[:, 8 + cg, :], rhs=pbf[:, p0:p0 + n], start=True, stop=True), reads=["pbf", "bdb"], writes=[psn[pi]])
                            P.op("act", lambda e, pi=pi, o=o, cg=cg, n=n: e.activation(out=ob[o][:, :n], in_=PS[pi][:, :n], func=AF.Identity, scale=pc("pscale", cg), bias=sm[:, 0:1]), reads=[psn[pi], "pvt", "smb"], writes=[f"ob{o}"])
                            P.dma("sp", lambda e, o=o, cg=cg, s0=s0, n=n: e.dma_start(out=ycat[256 + cg * 128:256 + (cg + 1) * 128, s0:s0 + n], in_=ob[o][:, :n]), reads=[f"ob{o}"])
                    P.barrier(); P.emit()

                with contextlib.ExitStack() as ph:
                    xs = SB(ph, "xs", [128, 1, LX]); xsb = SB(ph, "xsb", [128, 2, LX], BF16)
                    XCS = SB(ph, "XCS", [128, 32, 512], BF16)
                    TB = [[SB(ph, f"tb{i}{j}", [128, 32, 256], BF16) for j in range(2)] for i in range(2)]
                    fb = [SB(ph, f"fb{i}", [128, 512], BF16) for i in range(2)]; ob = [SB(ph, f"oc{i}", [128, 512], BF16) for i in range(2)]
                    bsb = [SB(ph, f"bsb{i}", [128, 256]) for i in range(2)]; fbp = [SB(ph, f"fbp{i}", [128, 256], BF16) for i in range(2)]; fbm = [SB(ph, f"fbm{i}", [128, 256], BF16) for i in range(2)]
                    obp = [SB(ph, f"obp{i}", [128, 256], BF16) for i in range(2)]; obm = [SB(ph, f"obm{i}", [128, 256], BF16) for i in range(2)]
                    dal = SB(ph, "dal", [128, LX // 128, 2], BF16)
                    it = 0
                    for (s0, L, tab) in ((0, LC, dftc), (LC, LX, dftx)):
                        nlt = L // 128
                        for cg in range(2):
                            P.dma("sp", lambda e, cg=cg, s0=s0, L=L: e.dma_start(out=xs[:, 0, :L], in_=proj[768 + cg * 128:768 + (cg + 1) * 128, s0:s0 + L]), writes=["xs"])
                            P.op("pool", lambda e, cg=cg, L=L: e.tensor_copy(out=xsb[:, cg, :L], in_=xs[:, 0, :L]), reads=["xs"], writes=[("xsb", cg)])
                        for lt in range(nlt):
                            pi = lt % 2
                            for q, (cg, M) in enumerate(((0, C64b), (1, C64b), (0, S64b), (1, S64b))):
                                P.op("pe", lambda e, pi=pi, q=q, cg=cg, M=M, lt=lt: e.matmul(PS[pi][:, q * 128:(q + 1) * 128], lhsT=xsb[:, cg, lt * 128:(lt + 1) * 128], rhs=M, start=True, stop=True), reads=[("xsb", cg), "cstb"], writes=[psn[pi]])
                            eng = "act" if lt % 2 == 0 else "dve"
                            if eng == "act":
                                P.op("act", lambda e, pi=pi, lt=lt: e.activation(out=XCS[:, lt, :], in_=PS[pi][:, :], func=AF.Copy), reads=[psn[pi]], writes=[("XCS", lt)])
                            else:
                                P.op("dve", lambda e, pi=pi, lt=lt: e.tensor_copy(out=XCS[:, lt, :], in_=PS[pi][:, :]), reads=[psn[pi]], writes=[("XCS", lt)])
                        n = 256
                        nk = L // n
                        if L == LX:
                            for kc in range(nk // 2):
                                tb = TB[it % 2]; tk = f"tb{it % 2}"; it += 1
                                for j in range(2):
                                    P.dma("sp" if j == 0 else "act", lambda e, tb=tb, j=j, kc=kc: e.dma_start(out=tb[j][:, :, :], in_=tab[j, kc]), writes=[tk + str(j)])
                                for cg in range(2):
                                    for j in range(2):
                                        pi = 2 + 2 * j + cg
                                        for lt in range(nlt):
                                            P.op("pe", lambda e, pi=pi, tb=tb, j=j, lt=lt, cg=cg: e.matmul(PS[pi][:, :n], lhsT=XCS[:, lt, j * 256 + cg * 128:j * 256 + (cg + 1) * 128], rhs=tb[j][:, lt, :n], start=(lt == 0), stop=(lt == nlt - 1)),
                                                 reads=[("XCS", lt), tk + str(j)], writes=[psn[pi]])
                                    P.op("act", lambda e, cg=cg: e.activation(out=bsb[cg][:], in_=PS[4 + cg][:, :n], func=AF.Copy), reads=[psn[4 + cg]], writes=[f"bsb{cg}"])
                                    P.op("dve", lambda e, cg=cg: e.tensor_tensor(out=fbp[cg][:], in0=PS[2 + cg][:, :n], in1=bsb[cg][:], op=ALU.add), reads=[psn[2 + cg], f"bsb{cg}"], writes=[f"fbp{cg}"])
                                    P.op("dve", lambda e, cg=cg: e.tensor_tensor(out=fbm[cg][:], in0=PS[2 + cg][:, :n], in1=bsb[cg][:], op=ALU.subtract), reads=[psn[2 + cg], f"bsb{cg}"], writes=[f"fbm{cg}"])
                                    P.op("pe", lambda e, cg=cg: e.matmul(PS[6][:, :n], lhsT=bdb[:, 10 + cg, :], rhs=fbp[cg][:], start=True, stop=True), reads=[f"fbp{cg}", "bdb"], writes=["ps6"])
                                    P.op("pe", lambda e, cg=cg: e.matmul(PS[7][:, :n], lhsT=bdb[:, 10 + cg, :], rhs=fbm[cg][:], start=True, stop=True), reads=[f"fbm{cg}", "bdb"], writes=["ps7"])
                                    P.op("act", lambda e, cg=cg: e.activation(out=obp[cg][:], in_=PS[6][:, :n], func=AF.Identity, bias=pc("bfour", cg)), reads=["ps6", "pvt"], writes=[f"obp{cg}"])
                                    P.dma("sp", lambda e, cg=cg, kc=kc: e.dma_start(out=ycat[512 + cg * 128:512 + (cg + 1) * 128, s0 + kc * n:s0 + (kc + 1) * n], in_=obp[cg][:]), reads=[f"obp{cg}"])
                                    j0 = 1 if kc == 0 else 0
                                    m = n - j0
                                    P.op("dve", lambda e, cg=cg, j0=j0, m=m: e.tensor_scalar(out=obm[cg][:, 0:m], in0=PS[7][:, j0:n][:, ::-1], scalar1=pc("bfour", cg), scalar2=None, op0=ALU.add), reads=["ps7", "pvt"], writes=[f"obm{cg}"])
                                    c_lo = L - n * kc - (n - 1)
                                    P.dma("act", lambda e, cg=cg, c_lo=c_lo, m=m: e.dma_start(out=ycat[512 + cg * 128:512 + (cg + 1) * 128, s0 + c_lo:s0 + c_lo + m], in_=obm[cg][:, 0:m]), reads=[f"obm{cg}"])
                            P.dma("sp", lambda e: e.dma_start(out=dal[:], in_=daltd), writes=["dal"])
                            for cg in range(2):
                                for lt in range(nlt):
                                    P.op("pe", lambda e, lt=lt, cg=cg: e.matmul(PS[2 + cg][:, 0:2], lhsT=XCS[:, lt, cg * 128:(cg + 1) * 128], rhs=dal[:, lt, :], start=(lt == 0), stop=(lt == nlt - 1)), reads=[("XCS", lt), "dal"], writes=[psn[2 + cg]])
                                P.op("dve", lambda e, cg=cg: e.tensor_copy(out=fbp[cg][:, 0:2], in_=PS[2 + cg][:, 0:2]), reads=[psn[2 + cg]], writes=[f"fbp{cg}"])
                                P.op("pe", lambda e, cg=cg: e.matmul(PS[6][:, 0:2], lhsT=bdb[:, 10 + cg, :], rhs=fbp[cg][:, 0:2], start=True, stop=True), reads=[f"fbp{cg}", "bdb"], writes=["ps6"])
                                P.op("act", lambda e, cg=cg: e.activation(out=obp[cg][:, 0:2], in_=PS[6][:, 0:2], func=AF.Identity, bias=pc("bfour", cg)), reads=["ps6", "pvt"], writes=[f"obp{cg}"])
                                P.dma("sp", lambda e, cg=cg: e.dma_start(out=ycat[512 + cg * 128:512 + (cg + 1) * 128, s0 + L // 2:s0 + L // 2 + 1], in_=obp[cg][:, 0:1], allow_slow_non_contiguous=True), reads=[f"obp{cg}"])
                            continue
                        for kc in range(nk):
                            tb = TB[it % 2]; tk = f"tb{it % 2}"; it += 1
                            for j in range(2):
                                P.dma("sp" if j == 0 else "act", lambda e, tb=tb, j=j, kc=kc, nlt=nlt, n=n, tab=tab: e.dma_start(out=tb[j][:, :nlt, :n], in_=tab[j, kc]), writes=[tk + str(j)])
                            for cg in range(2):
                                pi = 2 + (kc * 2 + cg) % 2
                                for j in range(2):
                                    for lt in range(nlt):
                                        P.op("pe", lambda e, pi=pi, tb=tb, j=j, lt=lt, cg=cg, n=n, nlt=nlt: e.matmul(PS[pi][:, :n], lhsT=XCS[:, lt, j * 256 + cg * 128:j * 256 + (cg + 1) * 128], rhs=tb[j][:, lt, :n], start=(j == 0 and lt == 0), stop=(j == 1 and lt == nlt - 1)),
                                             reads=[("XCS", lt), tk + str(j)], writes=[psn[pi]])
                                o = (kc * 2 + cg) % 2
                                P.op("dve", lambda e, pi=pi, o=o, n=n: e.tensor_copy(out=fb[o][:, :n], in_=PS[pi][:, :n]), reads=[psn[pi]], writes=[f"fb{o}"])
                                P.op("pe", lambda e, o=o, cg=cg, n=n: e.matmul(PS[4 + o][:, :n], lhsT=bdb[:, 10 + cg, :], rhs=fb[o][:, :n], start=True, stop=True), reads=[f"fb{o}", "bdb"], writes=[psn[4 + o]])
                                P.op("act", lambda e, o=o, cg=cg, n=n: e.activation(out=ob[o][:, :n], in_=PS[4 + o][:, :n], func=AF.Identity, bias=pc("bfour", cg)), reads=[psn[4 + o], "pvt"], writes=[f"oc{o}"])
                                P.dma("sp", lambda e, o=o, cg=cg, s0=s0, kc=kc, n=n: e.dma_start(out=ycat[512 + cg * 128:512 + (cg + 1) * 128, s0 + kc * n:s0 + (kc + 1) * n], in_=ob[o][:, :n]), reads=[f"oc{o}"])
                    P.barrier(); P.emit()

                with contextlib.ExitStack() as ph:
                    xv = SB(ph, "xv", [128, SP]); xg = SB(ph, "xg", [128, SP]); vb = [SB(ph, f"vb{i}", [128, SP], BF16) for i in range(2)]
                    dg = [SB(ph, f"dg{i}", [128, 31, 128], BF16) for i in range(2)]
                    wpst = SB(ph, "wpst", [128, 2, 256]); wpb = SB(ph, "wpb", [128, 2, 256], BF16)
                    vc = SB(ph, "vc", [128, 512]); vcb = SB(ph, "vcb", [128, 512], BF16); cen = SB(ph, "cen", [128, 512]); sq2 = SB(ph, "sq2", [128, 512], BF16)
                    rs2 = SB(ph, "rs2", [128, 512]); sg2 = SB(ph, "sg2", [128, 512]); svb = [SB(ph, f"svb{i}", [128, 512], BF16) for i in range(2)]
                    ob = [SB(ph, f"od{i}", [128, 512], BF16) for i in range(2)]
                    P.dma("sp", lambda e: e.dma_start(out=wpst[:], in_=w_pw[l].rearrange("(k p) n -> p k n", p=128)), writes=["wpst"])
                    P.op("dve", lambda e: e.tensor_copy(out=wpb[:], in_=wpst[:]), reads=["wpst"], writes=["wpb"])
                    for cg in range(2):
                        load_pad(xv, "xv", 1024 + cg * 128)
                        load_pad(xg, "xg", 1280 + cg * 128)
                        P.op("act", lambda e: e.activation(out=xg[:], in_=xg[:], func=AF.Sigmoid), reads=["xg"], writes=["xg"])
                        P.op("dve", lambda e, cg=cg: e.tensor_tensor(out=vb[cg][:], in0=xv[:], in1=xg[:], op=ALU.mult), reads=["xv", "xg"], writes=[f"vb{cg}"])
                        for j in range(31):
                            P.op("dve", lambda e, cg=cg, j=j: e.tensor_scalar(out=dg[cg][:, j, :], in0=ID32, scalar1=pc("cdw", j * 2 + cg), scalar2=None, op0=ALU.mult), reads=["cst", "pvt"], writes=[f"dg{cg}"])
                    for qi, (p0, n) in enumerate(PCH):
                        s0 = CH[qi][0]
                        for cg in range(2):
                            for j in range(31):
                                P.op("pe", lambda e, cg=cg, j=j, p0=p0, n=n: e.matmul(PS[cg][:, :n], lhsT=dg[cg][:, j, :], rhs=vb[cg][:, p0 + j - 15:p0 + j - 15 + n], start=(j == 0), stop=(j == 30)), reads=[f"vb{cg}", f"dg{cg}"], writes=[psn[cg]])
                            P.op("act", lambda e, cg=cg, n=n: e.activation(out=vc[:, :n], in_=PS[cg][:, :n], func=AF.Identity, bias=pc("cdb", cg)), reads=[psn[cg], "pvt"], writes=["vc"])
                            P.op("pool", lambda e, n=n: e.tensor_copy(out=vcb[:, :n], in_=vc[:, :n]), reads=["vc"], writes=["vcb"])
                            P.op("pe", lambda e, n=n: e.matmul(PS[2][:, :n], lhsT=MAVb, rhs=vcb[:, :n], start=True, stop=True), reads=["vcb", "cstb"], writes=["ps2"])
                            P.op("dve", lambda e, n=n: e.tensor_tensor(out=cen[:, :n], in0=vc[:, :n], in1=PS[2][:, :n], op=ALU.subtract), reads=["vc", "ps2"], writes=["cen"])
                            P.op("act", lambda e, n=n: e.activation(out=sq2[:, :n], in_=cen[:, :n], func=AF.Square), reads=["cen"], writes=["sq2"])
                            P.op("pe", lambda e, n=n: e.matmul(PS[3][:, :n], lhsT=MAVb, rhs=sq2[:, :n], start=True, stop=True), reads=["sq2", "cstb"], writes=["ps3"])
                            P.op("dve", lambda e, n=n: e.tensor_scalar(out=rs2[:, :n], in0=PS[3][:, :n], scalar1=EPS, scalar2=None, op0=ALU.add), reads=["ps3"], writes=["rs2"])
                            P.op("act", lambda e, n=n: e.activation(out=rs2[:, :n], in_=rs2[:, :n], func=AF.Sqrt), reads=["rs2"], writes=["rs2"])
                            P.op("dve", lambda e, n=n: e.reciprocal(out=rs2[:, :n], in_=rs2[:, :n]), reads=["rs2"], writes=["rs2"])
                            P.op("dve", lambda e, n=n: e.tensor_tensor(out=cen[:, :n], in0=cen[:, :n], in1=rs2[:, :n], op=ALU.mult), reads=["cen", "rs2"], writes=["cen"])
                            P.op("act", lambda e, n=n, cg=cg: e.activation(out=cen[:, :n], in_=cen[:, :n], func=AF.Identity, scale=pc("lng", cg), bias=pc("lnb", cg)), reads=["cen", "pvt"], writes=["cen"])
                            P.op("act", lambda e, n=n: e.activation(out=sg2[:, :n], in_=cen[:, :n], func=AF.Sigmoid), reads=["cen"], writes=["sg2"])
                            P.op("dve", lambda e, n=n, cg=cg: e.tensor_tensor(out=svb[cg][:, :n], in0=cen[:, :n], in1=sg2[:, :n], op=ALU.mult), reads=["cen", "sg2"], writes=[f"svb{cg}"])
                        for oc_ in range(2):
                            for cg in range(2):
                                P.op("pe", lambda e, oc_=oc_, cg=cg, n=n: e.matmul(PS[4 + oc_][:, :n], lhsT=wpb[:, cg, oc_ * 128:(oc_ + 1) * 128], rhs=svb[cg][:, :n], start=(cg == 0), stop=(cg == 1)), reads=[f"svb{cg}", "wpb"], writes=[psn[4 + oc_]])
                            P.op("act", lambda e, oc_=oc_, n=n: e.activation(out=ob[oc_][:, :n], in_=PS[4 + oc_][:, :n], func=AF.Identity, bias=pc("bpw", oc_)), reads=[psn[4 + oc_], "pvt"], writes=[f"od{oc_}"])
                            P.dma("sp", lambda e, oc_=oc_, s0=s0, n=n: e.dma_start(out=ycat[768 + oc_ * 128:768 + (oc_ + 1) * 128, s0:s0 + n], in_=ob[oc_][:, :n]), reads=[f"od{oc_}"])
                    P.barrier(); P.emit()
                mixs.close()
                if stop_after == "mix":
                    break

                chs = list(enumerate(CH))
                if last:
                    chs = chs[1:]
                with contextlib.ExitStack() as ph:
                    wob = SB(ph, "wob", [128, 8, 1024], BF16)
                    GT = SB(ph, "GT", [NE, S])
                    wr = SB(ph, "wr", [128, 8, NE]); brb = SB(ph, "brb", [128, NE]); bdn = SB(ph, "bdn", [NE, D])
                    P.dma("sp", lambda e: e.dma_start(out=wr[:], in_=w_router[l].rearrange("(k p) n -> p k n", p=128)), writes=["wr"])
                    P.dma("sp", lambda e: e.dma_start(out=brb[:], in_=b_router[l:l + 1, :].partition_broadcast(128)), writes=["brb"])
                    P.dma("sp", lambda e: e.dma_start(out=bdn[:], in_=b_down[l]), writes=["bdn"])
                    yc = [SB(ph, f"yc{i}", [128, 8, 512], BF16) for i in range(2)]
                    rb = [SB(ph, f"r{i}", [128, 8, 512]) for i in range(2)]
                    yt = SB(ph, "yt", [128, 512]); sqb = SB(ph, "sqb", [128, 8, 512], BF16); rstd = SB(ph, "rstd", [128, 512])
                    tmp = SB(ph, "tmp", [128, 8, 512]); h32 = SB(ph, "h32", [128, 8, 512]); hb = SB(ph, "hbw", [128, 8, 512], BF16)
                    for hf_ in range(2):
                        P.dma("sp", lambda e, hf_=hf_: e.dma_start(out=tmp[:], in_=w_out[l].rearrange("(k p) n -> p k n", p=128)[:, :, hf_ * 512:(hf_ + 1) * 512]), writes=["tmp"])
                        P.op("pool", lambda e, hf_=hf_: e.tensor_copy(out=wob[:, :, hf_ * 512:(hf_ + 1) * 512], in_=tmp[:]), reads=["tmp"], writes=["wob"])
                    i8 = SB(ph, "i8", [128, 8], U32); mkb = SB(ph, "mkb", [128, NE], BF16); Rk = SB(ph, "Rk", [128, NE]); oh = SB(ph, "oh", [128, NE])
                    e4 = SB(ph, "e4", [128, 4]); htb = [SB(ph, f"htb{i}", [128, D], BF16) for i in range(2)]
                    P.op("pool", lambda e: e.memset(carry[:], 0.0), writes=["carry"])
                    lg = SB(ph, "lg", [128, NE]); t8 = SB(ph, "t8", [128, 8]); ex = SB(ph, "ex", [128, NE]); mk = SB(ph, "mk", [128, NE]); s1 = SB(ph, "s1", [128, 4])
                    for ci, (s0, n) in chs:
                        b = ci % 2; w = 1 if ci == 0 else 0
                        P.dma("sp", lambda e, b=b, s0=s0, n=n: e.dma_start(out=yc[b][:, :, :n], in_=ycat.rearrange("(k p) s -> p k s", p=128)[:, :, s0:s0 + n]), writes=[f"yc{b}"])
                        P.dma("act", lambda e, b=b, s0=s0, n=n: e.dma_start(out=rb[b][:, :, :n], in_=res.rearrange("(k p) s -> p k s", p=128)[:, :, s0:s0 + n]), writes=[f"r{b}"])
                        for dc in range(8):
                            pi = dc % 4
                            for k in range(8):
                                P.op("pe", lambda e, b=b, pi=pi, dc=dc, k=k, n=n: e.matmul(PS[pi][:, :n], lhsT=wob[:, k, dc * 128:(dc + 1) * 128], rhs=yc[b][:, k, :n], start=(k == 0), stop=(k == 7)), reads=[f"yc{b}", "wob"], writes=[psn[pi]])
                            P.op("act", lambda e, pi=pi, dc=dc, n=n: e.activation(out=yt[:, :n], in_=PS[pi][:, :n], func=AF.Identity, bias=pc("b_out", dc)), reads=[psn[pi], "pvt"], writes=["yt"])
                            P.op("dve", lambda e, b=b, dc=dc, n=n, w=w: e.scalar_tensor_tensor(out=rb[b][:, dc, :n], in0=yt[:, :n], scalar=G1(dc, w), in1=rb[b][:, dc, :n], op0=ALU.mult, op1=ALU.add), reads=["yt", "modT", f"r{b}"], writes=[f"r{b}"])
                        rms_rstd(rb[b], f"r{b}", sqb, "sqb", n, 7, rstd, "rstd")
                        for k in range(8):
                            P.op("dve", lambda e, b=b, k=k, n=n: e.tensor_tensor(out=tmp[:, k, :n], in0=rb[b][:, k, :n], in1=rstd[:, :n], op=ALU.mult), reads=[f"r{b}", "rstd"], writes=[("tmp", k)])
                            P.op("act", lambda e, k=k, n=n, w=w: e.activation(out=h32[:, k, :n], in_=tmp[:, k, :n], func=AF.Identity, scale=A2[:, k, w:w + 1], bias=SH2(k, w)), reads=[("tmp", k), "A", "modT"], writes=[("h32", k)])
                            P.op("pool", lambda e, k=k, n=n: e.tensor_copy(out=hb[:, k, :n], in_=h32[:, k, :n]), reads=[("h32", k)], writes=[("hbw", k)])
                        P.dma("sp", lambda e, s0=s0, n=n: e.dma_start(out=hT.rearrange("(k p) s -> p k s", p=128)[:, :, s0:s0 + n], in_=hb[:, :, :n]), reads=["hbw"])
                        for t0 in range(0, n, 128):
                            for k in range(8):
                                P.op("pe", lambda e, k=k, t0=t0: e.matmul(PS[4][:, 0:NE], lhsT=h32[:, k, t0:t0 + 128], rhs=wr[:, k, :], start=(k == 0), stop=(k == 7)), reads=[("h32", k), "wr"], writes=["ps4"])
                            P.op("dve", lambda e: e.tensor_tensor(out=lg[:], in0=PS[4][:, 0:NE], in1=brb[:], op=ALU.add), reads=["ps4", "brb"], writes=["lg"])
                            P.op("dve", lambda e: e.max(out=t8[:], in_=lg[:]), reads=["lg"], writes=["t8"])
                            P.op("dve", lambda e: e.tensor_scalar(out=s1[:, 0:1], in0=t8[:, 0:1], scalar1=-1.0, scalar2=None, op0=ALU.mult), reads=["t8"], writes=["s1"])
                            P.op("act", lambda e: e.activation(out=ex[:], in_=lg[:], func=AF.Exp, bias=s1[:, 0:1]), reads=["lg", "s1"], writes=["ex"])
                            P.op("dve", lambda e: e.tensor_scalar(out=mk[:], in0=lg[:], scalar1=t8[:, 3:4], scalar2=None, op0=ALU.is_ge), reads=["lg", "t8"], writes=["mk"])
                            P.op("dve", lambda e: e.tensor_tensor(out=ex[:], in0=ex[:], in1=mk[:], op=ALU.mult), reads=["ex", "mk"], writes=["ex"])
                            P.op("dve", lambda e: e.tensor_reduce(out=s1[:, 1:2], in_=ex[:], axis=AX.X, op=ALU.add), reads=["ex"], writes=["s1"])
                            P.op("dve", lambda e: e.reciprocal(out=s1[:, 2:3], in_=s1[:, 1:2]), reads=["s1"], writes=["s1"])
                            P.op("dve", lambda e: e.tensor_scalar(out=ex[:], in0=ex[:], scalar1=s1[:, 2:3], scalar2=None, op0=ALU.mult), reads=["ex", "s1"], writes=["ex"])
                            if sparse:
                                tg = (s0 + t0) // 128
                                P.op("dve", lambda e: e.max_index(out=i8[:], in_max=t8[:], in_values=lg[:]), reads=["lg", "t8"], writes=["i8"])
                                P.op("dve", lambda e, tg=tg: e.tensor_copy(out=idxf[:, tg, :], in_=i8[:, 0:4]), reads=["i8"], writes=["idxf"])
                                P.op("act", lambda e: e.activation(out=e4[:], in_=t8[:, 0:4], func=AF.Exp, bias=s1[:, 0:1]), reads=["t8", "s1"], writes=["e4"])
                                P.op("dve", lambda e: e.tensor_reduce(out=s1[:, 3:4], in_=e4[:], axis=AX.X, op=ALU.add), reads=["e4"], writes=["s1"])
                                P.op("dve", lambda e: e.reciprocal(out=s1[:, 3:4], in_=s1[:, 3:4]), reads=["s1"], writes=["s1"])
                                P.op("dve", lambda e, tg=tg: e.tensor_scalar(out=g4[:, tg, :], in0=e4[:], scalar1=s1[:, 3:4], scalar2=None, op0=ALU.mult), reads=["e4", "s1"], writes=["g4"])
                                P.op("pool", lambda e: e.tensor_copy(out=mkb[:], in_=mk[:]), reads=["mk"], writes=["mkb"])
                                P.op("pe", lambda e: e.matmul(PS[6][:, 0:NE], lhsT=UTb, rhs=mkb[:], start=True, stop=True), reads=["mkb", "cstb"], writes=["ps6"])
                                P.op("pe", lambda e: e.matmul(PS[6][:, 64:64 + NE], lhsT=ONEb, rhs=mkb[:], start=True, stop=True), reads=["mkb", "cstb"], writes=["ps6"])
                                P.op("dve", lambda e: e.tensor_tensor(out=Rk[:], in0=PS[6][:, 0:NE], in1=carry[:], op=ALU.add), reads=["ps6", "carry"], writes=["Rk"])
                                P.op("dve", lambda e: e.tensor_tensor(out=carry[:], in0=PS[6][:, 64:64 + NE], in1=carry[:], op=ALU.add), reads=["ps6", "carry"], writes=["carry"])
                                for k4 in range(4):
                                    P.op("dve", lambda e, tg=tg, k4=k4: e.tensor_scalar(out=oh[:], in0=iot[:, 0:NE], scalar1=idxf[:, tg, k4:k4 + 1], scalar2=None, op0=ALU.is_equal), reads=["iot", "idxf"], writes=["oh"])
                                    P.op("dve", lambda e: e.tensor_tensor(out=oh[:], in0=oh[:], in1=Rk[:], op=ALU.mult), reads=["oh", "Rk"], writes=["oh"])
                                    P.op("dve", lambda e, tg=tg, k4=k4: e.tensor_reduce(out=rank4[:, tg, k4:k4 + 1], in_=oh[:], axis=AX.X, op=ALU.add), reads=["oh"], writes=["rank4"])
                                hq = tg % 2
                                psb = PS[6][:].bitcast(BF16)
                                for k in range(8):
                                    P.op("pe", lambda e, k=k, t0=t0, psb=psb: e.transpose(out=psb[:, 256 + k * 96:256 + k * 96 + 128] if False else PS[3][:].bitcast(BF16)[:, k * 128:(k + 1) * 128], in_=hb[:, k, t0:t0 + 128], identity=IDb), reads=[("hbw", k), "cstb"], writes=["ps3"])
                                P.op("act", lambda e, hq=hq: e.activation(out=htb[hq][:], in_=PS[3][:].bitcast(BF16), func=AF.Copy), reads=["ps3"], writes=[f"htb{hq}"])
                                P.dma("act", lambda e, hq=hq, s0=s0, t0=t0: e.dma_start(out=htm[s0 + t0:s0 + t0 + 128, :], in_=htb[hq][:]), reads=[f"htb{hq}"])
                            P.op("pe", lambda e: e.transpose(out=PS[5][0:NE, 0:128], in_=ex[:], identity=ID32), reads=["ex", "cst"], writes=["ps5"])
                            P.op("act", lambda e, s0=s0, t0=t0: e.activation(out=GT[:, s0 + t0:s0 + t0 + 128], in_=PS[5][0:NE, 0:128], func=AF.Copy), reads=["ps5"], writes=[("GT", s0 + t0)])
                        for dc in range(8):
                            pi = 2 + dc % 2
                            P.op("pe", lambda e, pi=pi, dc=dc, s0=s0, n=n: e.matmul(PS[pi][:, :n], lhsT=bdn[:, dc * 128:(dc + 1) * 128], rhs=GT[:, s0:s0 + n], start=True, stop=True), reads=["GT", "bdn"], writes=[psn[pi]])
                            P.op("dve", lambda e, pi=pi, b=b, dc=dc, n=n, w=w: e.scalar_tensor_tensor(out=rb[b][:, dc, :n], in0=PS[pi][:, :n], scalar=G2(dc, w), in1=rb[b][:, dc, :n], op0=ALU.mult, op1=ALU.add), reads=[psn[pi], "modT", f"r{b}"], writes=[f"r{b}"])
                        P.dma("sp", lambda e, b=b, s0=s0, n=n: e.dma_start(out=res.rearrange("(k p) s -> p k s", p=128)[:, :, s0:s0 + n], in_=rb[b][:, :, :n]), reads=[f"r{b}"])
                        P.dma("sp", lambda e, s0=s0, n=n: e.dma_start(out=gTd[:, s0:s0 + n], in_=GT[:, s0:s0 + n]), reads=["GT"])
                    P.barrier(); P.emit()
                if stop_after == "outproj":
                    break

                if sparse:
                    nblk = (4 * 128 * len([1 for ci_, (s0_, n_) in chs for _ in range(n_ // 128)])) // BLK + NE
                    tiles = [(s0_ + t0_) // 128 for ci_, (s0_, n_) in chs for t0_ in range(0, n_, 128)]
                    P.drain()
                    with contextlib.ExitStack() as ph:
                        ci_t = SB(ph, "ci_t", [128, NE], I32); padf = SB(ph, "padf", [128, NE]); pend = SB(ph, "pend", [128, NE]); pst = SB(ph, "pst", [128, NE])
                        one32 = SB(ph, "one32", [128, NE]); bef = SB(ph, "bef", [128, NBLK]); oh = SB(ph, "oh2", [128, NE]); pst4 = SB(ph, "pst4", [128, NT, 4])
                        hl = [SB(ph, f"hl{i}", [128, D], BF16) for i in range(3)]
                        P.op("pool", lambda e: e.memset(one32[:], 1.0), writes=["one32"])
                        P.op("pool", lambda e: e.memset(pst4[:], 0.0), writes=["pst4"])
                        P.op("dve", lambda e: e.tensor_scalar(out=padf[:], in0=carry[:], scalar1=float(BLK - 1), scalar2=None, op0=ALU.add), reads=["carry"], writes=["padf"])
                        P.op("dve", lambda e: e.tensor_copy(out=ci_t[:], in_=padf[:]), reads=["padf"], writes=["ci_t"])
                        P.op("dve", lambda e: e.tensor_scalar(out=ci_t[:], in0=ci_t[:], scalar1=8, scalar2=8, op0=ALU.arith_shift_right, op1=ALU.logical_shift_left), reads=["ci_t"], writes=["ci_t"])
                        P.op("dve", lambda e: e.tensor_copy(out=padf[:], in_=ci_t[:]), reads=["ci_t"], writes=["padf"])
                        P.op("dve", lambda e: e.tensor_tensor_scan(out=pend[:], data0=one32[:], data1=padf[:], initial=0.0, op0=ALU.mult, op1=ALU.add), reads=["one32", "padf"], writes=["pend"])
                        P.op("dve", lambda e: e.tensor_tensor(out=pst[:], in0=pend[:], in1=padf[:], op=ALU.subtract), reads=["pend", "padf"], writes=["pst"])
                        P.op("pool", lambda e: e.memset(bef[:], 0.0), writes=["bef"])
                        for ex_ in range(NE):
                            P.op("dve", lambda e, ex_=ex_: e.scalar_tensor_tensor(out=bef[:], in0=iot[:, 32:32 + NBLK], scalar=pend[:, ex_:ex_ + 1], in1=bef[:], op0=ALU.is_ge, op1=ALU.add), reads=["iot", "pend", "bef"], writes=["bef"])
                        P.op("dve", lambda e: e.tensor_scalar(out=bef[:], in0=bef[:], scalar1=float(NE - 1), scalar2=None, op0=ALU.min), reads=["bef"], writes=["bef"])
                        skp = SB(ph, "skp", [128, NBLK])
                        P.op("dve", lambda e: e.tensor_tensor(out=skp[:, 2:NBLK], in0=bef[:, 2:NBLK], in1=bef[:, 0:NBLK - 2], op=ALU.is_equal), reads=["bef"], writes=["skp"])
                        P.op("dve", lambda e: e.tensor_scalar(out=bef[:], in0=bef[:], scalar1=128.0, scalar2=iot[:, 32 + NBLK:33 + NBLK], op0=ALU.mult, op1=ALU.add), reads=["bef", "iot"], writes=["bef"])
                        P.op("dve", lambda e: e.scalar_tensor_tensor(out=bef[:, 2:NBLK], in0=skp[:, 2:NBLK], scalar=1048576.0, in1=bef[:, 2:NBLK], op0=ALU.mult, op1=ALU.add), reads=["bef", "skp"], writes=["bef"])
                        P.op("dve", lambda e: e.tensor_copy(out=widx[:], in_=bef[:]), reads=["bef"], writes=["widx"])
                        for tg in tiles:
                            for k4 in range(4):
                                P.op("dve", lambda e, tg=tg, k4=k4: e.tensor_scalar(out=oh[:], in0=iot[:, 0:NE], scalar1=idxf[:, tg, k4:k4 + 1], scalar2=None, op0=ALU.is_equal), reads=["iot", "idxf"], writes=["oh2"])
                                P.op("dve", lambda e: e.tensor_tensor(out=oh[:], in0=oh[:], in1=pst[:], op=ALU.mult), reads=["oh2", "pst"], writes=["oh2"])
                                P.op("dve", lambda e, tg=tg, k4=k4: e.tensor_reduce(out=pst4[:, tg, k4:k4 + 1], in_=oh[:], axis=AX.X, op=ALU.add), reads=["oh2"], writes=["pst4"])
                        P.op("dve", lambda e: e.tensor_tensor(out=pst4[:], in0=pst4[:], in1=rank4[:], op=ALU.add), reads=["pst4", "rank4"], writes=["pst4"])
                        P.op("dve", lambda e: e.tensor_copy(out=destu[:], in_=pst4[:].rearrange("p t k -> p (t k)")), reads=["pst4"], writes=["destu"])
                        for i_, tg in enumerate(tiles):
                            hq = i_ % 3
                            P.dma("sp", lambda e, hq=hq, tg=tg: e.dma_start(out=hl[hq][:], in_=htm[tg * 128:(tg + 1) * 128, :]), writes=[f"hl{hq}"])
                            for k4 in range(4):
                                P.dma("pool", lambda e, hq=hq, tg=tg, k4=k4: ind_dma(e, out=xs_d, out_offset=bass.IndirectOffsetOnAxis(ap=destu[:, tg * 4 + k4:tg * 4 + k4 + 1], axis=0), in_=hl[hq][:, :], in_offset=None), reads=[f"hl{hq}", "destu"])
                        P.barrier(); P.emit()

                    with contextlib.ExitStack() as ph:
                        wg = [SB(ph, f"wg{i}", [128, 9, 2048], BF16) for i in range(2)]
                        wd = [SB(ph, f"wd{i}", [128, 8, 1024], BF16) for i in range(2)]
                        xsb = [SB(ph, f"xsb{i}", [128, 2, D], BF16) for i in range(2)]
                        xT = [SB(ph, f"xT{i}", [128, 8, BLK], BF16) for i in range(2)]
                        aT = [SB(ph, f"aT{i}", [128, 8, BLK], BF16) for i in range(2)]
                        ysb = [SB(ph, f"ysb{i}", [128, 2, D]) for i in range(2)]
                        gt = [SB(ph, f"gt{i}", [128, BLK]) for i in range(2)]; sg = [SB(ph, f"sg{i}", [128, BLK]) for i in range(2)]; up = [SB(ph, f"up{i}", [128, BLK]) for i in range(2)]
                        onr = SB(ph, "onr", [1, BLK], BF16)
                        P.op("pool", lambda e: e.memset(onr[:], 1.0), writes=["onr"])

                        def wload(b):
                            p_ = b % 2
                            P.dma("pool", lambda e: ind_dma(e, out=wg[p_][:].rearrange("p k f -> p (k f)"), out_offset=None, in_=WGb.rearrange("e p k f -> (e p) (k f)"), in_offset=bass.IndirectOffsetOnAxis(ap=widx[:, b:b + 1], axis=0), bounds_check=vbox["v"], oob_is_err=False), reads=["widx"], writes=[f"wg{p_}"])
                            P.dma("pool", lambda e: ind_dma(e, out=wd[p_][:].rearrange("p k f -> p (k f)"), out_offset=None, in_=WDb.rearrange("e p k f -> (e p) (k f)"), in_offset=bass.IndirectOffsetOnAxis(ap=widx[:, b:b + 1], axis=0), bounds_check=vbox["v"], oob_is_err=False), reads=["widx"], writes=[f"wd{p_}"])
                            P.dma("sp", lambda e: e.dma_start(out=xsb[p_][:], in_=xs_d[b * BLK:(b + 1) * BLK, :].rearrange("(j p) d -> p j d", p=128)), writes=[f"xsb{p_}"])

                        def block(b):
                            p_ = b % 2
                            if b + 1 < nblk:
                                wload(b + 1)
                            for j in range(2):
                                psb = PS[6 + j][:].bitcast(BF16)
                                for k in range(8):
                                    P.op("pe", lambda e, j=j, k=k, psb=psb: e.transpose(out=psb[:, k * 128:(k + 1) * 128], in_=xsb[p_][:, j, k * 128:(k + 1) * 128], identity=IDb), reads=[f"xsb{p_}", "cstb"], writes=[psn[6 + j]])
                                if j == 0:
                                    P.op("act", lambda e, psb=psb: e.activation(out=xT[p_][:, :, 0:128], in_=psb.rearrange("p (k t) -> p k t", t=128), func=AF.Copy), reads=[psn[6]], writes=[(f"xT{p_}", 0)])
                                else:
                                    P.op("dve", lambda e, psb=psb: e.tensor_copy(out=xT[p_][:, :, 128:256], in_=psb.rearrange("p (k t) -> p k t", t=128)), reads=[psn[7]], writes=[(f"xT{p_}", 1)])
                            for fc in range(8):
                                q = fc % 2
                                for hf in range(2):
                                    c0_ = hf * 1024 + fc * 128
                                    for k in range(8):
                                        P.op("pe", lambda e, q=q, hf=hf, c0_=c0_, k=k: e.matmul(PS[q][:, hf * BLK:(hf + 1) * BLK], lhsT=wg[p_][:, k, c0_:c0_ + 128], rhs=xT[p_][:, k, :], start=(k == 0), stop=False), reads=[f"wg{p_}", f"xT{p_}"], writes=[psn[q]])
                                    P.op("pe", lambda e, q=q, hf=hf, c0_=c0_: e.matmul(PS[q][:, hf * BLK:(hf + 1) * BLK], lhsT=wg[p_][0:1, 8, c0_:c0_ + 128], rhs=onr[0:1, :], start=False, stop=True), reads=[f"wg{p_}", "onr"], writes=[psn[q]])
                                P.op("dve", lambda e, q=q: e.tensor_scalar(out=gt[q][:], in0=PS[q][:, 0:BLK], scalar1=7.0, scalar2=None, op0=ALU.min), reads=[psn[q]], writes=[f"gt{q}"])
                                P.op("act", lambda e, q=q: e.activation(out=sg[q][:], in_=gt[q][:], func=AF.Sigmoid, scale=1.702), reads=[f"gt{q}"], writes=[f"sg{q}"])
                                P.op("dve", lambda e, q=q: e.tensor_scalar(out=up[q][:], in0=PS[q][:, BLK:2 * BLK], scalar1=7.0, scalar2=-7.0, op0=ALU.min, op1=ALU.max), reads=[psn[q]], writes=[f"up{q}"])
                                P.op("pool", lambda e, q=q: e.tensor_tensor(out=gt[q][:], in0=gt[q][:], in1=sg[q][:], op=ALU.mult), reads=[f"gt{q}", f"sg{q}"], writes=[f"gt{q}"])
                                P.op("dve", lambda e, q=q, fc=fc: e.scalar_tensor_tensor(out=aT[p_][:, fc, :], in0=up[q][:], scalar=1.0, in1=gt[q][:], op0=ALU.add, op1=ALU.mult), reads=[f"up{q}", f"gt{q}"], writes=[(f"aT{p_}", fc)])
                            for j in range(2):
                                for dh in range(2):
                                    q = 2 + (j * 2 + dh) % 4
                                    for fc in range(8):
                                        P.op("pe", lambda e, q=q, j=j, dh=dh, fc=fc: e.matmul(PS[q][:, :], lhsT=aT[p_][:, fc, j * 128:(j + 1) * 128], rhs=wd[p_][:, fc, dh * 512:(dh + 1) * 512], start=(fc == 0), stop=(fc == 7)), reads=[(f"aT{p_}", fc), f"wd{p_}"], writes=[psn[q]])
                                    if dh == 0:
                                        P.op("act", lambda e, q=q, j=j, dh=dh: e.activation(out=ysb[p_][:, j, dh * 512:(dh + 1) * 512], in_=PS[q][:, :], func=AF.Copy), reads=[psn[q]], writes=[(f"ysb{p_}", j * 2 + dh)])
                                    else:
                                        P.op("pool" if False else "dve", lambda e, q=q, j=j, dh=dh: e.tensor_copy(out=ysb[p_][:, j, dh * 512:(dh + 1) * 512], in_=PS[q][:, :]), reads=[psn[q]], writes=[(f"ysb{p_}", j * 2 + dh)])
                            P.dma("sp", lambda e: e.dma_start(out=ys_d[b * BLK:(b + 1) * BLK, :].rearrange("(j p) d -> p j d", p=128), in_=ysb[p_][:]), reads=[f"ysb{p_}"])

                        if l + 1 < depth and stop_after is None:
                            P.bg = precast_gen(l + 1)
                            P.bg_every = 500
                        wload(0)
                        for b in range(nblk):
                            block(b)
                        P.bg_every = 100
                        P.barrier(); P.emit()

                    with contextlib.ExitStack() as ph:
                        gb = [[SB(ph, f"gb{i}{k}", [128, D]) for k in range(4)] for i in range(2)]
                        acc = [SB(ph, f"acc{i}", [128, D]) for i in range(4)]
                        sqbc = SB(ph, "sqbc", [128, 8, 512], BF16); rstdc = SB(ph, "rstdc", [128, 512])
                        rb = [SB(ph, f"r{i}", [128, 8, 512]) for i in range(2)]
                        it_ = 0
                        for ci, (s0, n) in chs:
                            b = ci % 2; w = 1 if ci == 0 else 0
                            P.dma("sp", lambda e, b=b, s0=s0, n=n: e.dma_start(out=rb[b][:, :, :n], in_=res.rearrange("(k p) s -> p k s", p=128)[:, :, s0:s0 + n]), writes=[f"r{b}"])
                            for ti, t0 in enumerate(range(0, n, 128)):
                                tg = (s0 + t0) // 128
                                gq = it_ % 2; it_ += 1
                                for k4 in range(4):
                                    P.dma("pool", lambda e, gq=gq, k4=k4, tg=tg: ind_dma(e, out=gb[gq][k4][:, :], out_offset=None, in_=ys_d, in_offset=bass.IndirectOffsetOnAxis(ap=destu[:, tg * 4 + k4:tg * 4 + k4 + 1], axis=0)), reads=["destu"], writes=[f"gb{gq}{k4}"])
                                P.op("dve", lambda e, gq=gq, ti=ti, tg=tg: e.tensor_scalar(out=acc[ti][:], in0=gb[gq][0][:], scalar1=g4[:, tg, 0:1], scalar2=None, op0=ALU.mult), reads=[f"gb{gq}0", "g4"], writes=[f"acc{ti}"])
                                for k4 in range(1, 4):
                                    P.op("dve", lambda e, gq=gq, ti=ti, tg=tg, k4=k4: e.scalar_tensor_tensor(out=acc[ti][:], in0=gb[gq][k4][:], scalar=g4[:, tg, k4:k4 + 1], in1=acc[ti][:], op0=ALU.mult, op1=ALU.add), reads=[f"gb{gq}{k4}", "g4", f"acc{ti}"], writes=[f"acc{ti}"])
                            for k in range(8):
                                q = k % 4
                                for ti, t0 in enumerate(range(0, n, 128)):
                                    P.op("pe", lambda e, q=q, k=k, ti=ti, t0=t0: e.transpose(out=PS[q][:, t0:t0 + 128], in_=acc[ti][:, k * 128:(k + 1) * 128], identity=ID32), reads=[f"acc{ti}", "cst"], writes=[psn[q]])
                                P.op("dve", lambda e, q=q, k=k, b=b, n=n, w=w: e.scalar_tensor_tensor(out=rb[b][:, k, :n], in0=PS[q][:, :n], scalar=G2(k, w), in1=rb[b][:, k, :n], op0=ALU.mult, op1=ALU.add), reads=[psn[q], "modT", f"r{b}"], writes=[f"r{b}"])
                            if last and stop_after is None:
                                rms_rstd(rb[b], f"r{b}", sqbc, "sqbc", n, 7, rstdc, "rstdc")
                                for k in range(8):
                                    P.op("dve", lambda e, b=b, k=k, n=n: e.scalar_tensor_tensor(out=rb[b][:, k, :n], in0=rb[b][:, k, :n], scalar=pc("fng", k), in1=rstdc[:, :n], op0=ALU.mult, op1=ALU.mult), reads=[f"r{b}", "rstdc", "pvt"], writes=[f"r{b}"])
                                P.dma("sp", lambda e, b=b, s0=s0, n=n: e.dma_start(out=outT.rearrange("(k p) s -> p k s", p=128)[:, :, s0 - LC:s0 - LC + n], in_=rb[b][:, :, :n]), reads=[f"r{b}"])
                            else:
                                P.dma("sp", lambda e, b=b, s0=s0, n=n: e.dma_start(out=res.rearrange("(k p) s -> p k s", p=128)[:, :, s0:s0 + n], in_=rb[b][:, :, :n]), reads=[f"r{b}"])
                        P.barrier(); P.emit()
                    continue

                with contextlib.ExitStack() as ph:
                    wg = [SB(ph, f"wg{i}", [128, 8, 2048], BF16) for i in range(2)]
                    wd = [SB(ph, f"wd{i}", [128, 8, 1024], BF16) for i in range(2)]
                    stg = [SB(ph, f"stg{i}", [128, 2048]) for i in range(3)]
                    hbuf = [SB(ph, f"hb{i}", [128, 8, 512], BF16) for i in range(2)]
                    act_ = SB(ph, "actT", [128, 8, 512], BF16)
                    gbc = [SB(ph, f"gbc{i}", [128, 512]) for i in range(2)]
                    gt = [SB(ph, f"gt{i}", [128, 512]) for i in range(2)]; sg = [SB(ph, f"sg{i}", [128, 512]) for i in range(2)]; up = [SB(ph, f"up{i}", [128, 512]) for i in range(2)]
                    rtb = [SB(ph, f"rt{i}", [128, 8, 512]) for i in range(2)]; ty = [SB(ph, f"ty{i}", [128, 512]) for i in range(2)]
                    scn = [0]

                    def wstep(ex_, k):
                        wb_ = ex_ % 2
                        s_ = scn[0] % 3; scn[0] += 1
                        if k < 8:
                            P.dma("act", lambda e: e.dma_start(out=stg[s_][:, :], in_=w_gu[l, ex_, k * 128:(k + 1) * 128, :]), writes=[f"stg{s_}"])
                            P.op("pool", lambda e: e.tensor_copy(out=wg[wb_][:, k, :], in_=stg[s_][:, :]), reads=[f"stg{s_}"], writes=[(f"wg{wb_}", k)])
                        else:
                            k2 = k - 8
                            P.dma("act", lambda e: e.dma_start(out=stg[s_][:, 0:1024], in_=w_down[l, ex_, k2 * 128:(k2 + 1) * 128, :]), writes=[f"stg{s_}"])
                            P.op("pool", lambda e: e.tensor_copy(out=wd[wb_][:, k2, :], in_=stg[s_][:, 0:1024]), reads=[f"stg{s_}"], writes=[(f"wd{wb_}", k2)])

                    work = [(ex_, j, ci, s0, n) for ex_ in range(n_exp) for j, (ci, (s0, n)) in enumerate(chs)]

                    def loads(i):
                        ex_, j, ci, s0, n = work[i]
                        b_ = i % 2
                        P.dma("sp", lambda e: e.dma_start(out=hbuf[b_][:, :, :n], in_=hT.rearrange("(k p) s -> p k s", p=128)[:, :, s0:s0 + n]), writes=[f"hb{b_}"])
                        P.dma("sp", lambda e: e.dma_start(out=gbc[b_][:, :n], in_=gTd[ex_:ex_ + 1, s0:s0 + n].partition_broadcast(128)), writes=[f"gbc{b_}"])
                        P.dma("sp", lambda e: e.dma_start(out=rtb[b_][:, :, :n], in_=res.rearrange("(k p) s -> p k s", p=128)[:, :, s0:s0 + n]), reads=[("res", ci)], writes=[f"rt{b_}"])

                    for k in range(16):
                        wstep(0, k)
                    loads(0)
                    nch = len(chs)
                    def compute(i):
                        ex_, j, ci, s0, n = work[i]
                        wb_ = ex_ % 2; b_ = i % 2
                        w = 1 if ci == 0 else 0
                        if ex_ + 1 < n_exp:
                            for k in range(16):
                                if k * nch // 16 == j:
                                    wstep(ex_ + 1, k)
                        if i + 1 < len(work):
                            loads(i + 1)
                        for fc in range(8):
                            q = fc % 2
                            for k in range(8):
                                P.op("pe", lambda e, q=q, fc=fc, k=k: e.matmul(PS[q][:, :n], lhsT=wg[wb_][:, k, fc * 128:(fc + 1) * 128], rhs=hbuf[b_][:, k, :n], start=(k == 0), stop=(k == 7)), reads=[(f"wg{wb_}", k), f"hb{b_}"], writes=[psn[q]])
                            for k in range(8):
                                P.op("pe", lambda e, q=q, fc=fc, k=k: e.matmul(PS[2 + q][:, :n], lhsT=wg[wb_][:, k, 1024 + fc * 128:1024 + (fc + 1) * 128], rhs=hbuf[b_][:, k, :n], start=(k == 0), stop=(k == 7)), reads=[(f"wg{wb_}", k), f"hb{b_}"], writes=[psn[2 + q]])
                            bg = pc("b_gu", ex_ * 16 + fc); bu = pc("b_gu", ex_ * 16 + 8 + fc)
                            P.op("dve", lambda e, q=q, bg=bg: e.tensor_scalar(out=gt[q][:, :n], in0=PS[q][:, :n], scalar1=bg, scalar2=7.0, op0=ALU.add, op1=ALU.min), reads=[psn[q], "pvt"], writes=[f"gt{q}"])
                            P.op("act", lambda e, q=q: e.activation(out=sg[q][:, :n], in_=gt[q][:, :n], func=AF.Sigmoid, scale=1.702), reads=[f"gt{q}"], writes=[f"sg{q}"])
                            P.op("act", lambda e, q=q, bu=bu: e.activation(out=up[q][:, :n], in_=PS[2 + q][:, :n], func=AF.Identity, bias=bu), reads=[psn[2 + q], "pvt"], writes=[f"up{q}"])
                            P.op("pool", lambda e, q=q: e.tensor_scalar(out=up[q][:, :n], in0=up[q][:, :n], scalar1=7.0, scalar2=-7.0, op0=ALU.min, op1=ALU.max), reads=[f"up{q}"], writes=[f"up{q}"])
                            P.op("pool", lambda e, q=q: e.tensor_tensor(out=gt[q][:, :n], in0=gt[q][:, :n], in1=sg[q][:, :n], op=ALU.mult), reads=[f"gt{q}", f"sg{q}"], writes=[f"gt{q}"])
                            P.op("dve", lambda e, q=q, fc=fc: e.scalar_tensor_tensor(out=act_[:, fc, :n], in0=up[q][:, :n], scalar=1.0, in1=gt[q][:, :n], op0=ALU.add, op1=ALU.mult), reads=[f"up{q}", f"gt{q}"], writes=[("actT", fc)])
                        for dc in range(8):
                            q = dc % 2
                            for fc in range(8):
                                P.op("pe", lambda e, q=q, dc=dc, fc=fc: e.matmul(PS[4 + q][:, :n], lhsT=wd[wb_][:, fc, dc * 128:(dc + 1) * 128], rhs=act_[:, fc, :n], start=(fc == 0), stop=(fc == 7)), reads=[(f"wd{wb_}", fc), ("actT", fc)], writes=[psn[4 + q]])
                            P.op("dve", lambda e, q=q, dc=dc: e.scalar_tensor_tensor(out=ty[q][:, :n], in0=PS[4 + q][:, :n], scalar=G2(dc, w), in1=gbc[b_][:, :n], op0=ALU.mult, op1=ALU.mult), reads=[psn[4 + q], "modT", f"gbc{b_}"], writes=[f"ty{q}"])
                            P.op("pool", lambda e, q=q, dc=dc: e.tensor_tensor(out=rtb[b_][:, dc, :n], in0=rtb[b_][:, dc, :n], in1=ty[q][:, :n], op=ALU.add), reads=[f"rt{b_}", f"ty{q}"], writes=[f"rt{b_}"])
                        P.dma("sp", lambda e: e.dma_start(out=res.rearrange("(k p) s -> p k s", p=128)[:, :, s0:s0 + n], in_=rtb[b_][:, :, :n]), reads=[f"rt{b_}"], writes=[("res", ci)])
                    for i in range(len(work)):
                        compute(i)
                    P.barrier(); P.emit()

        if stop_after is None and not (sparse and depth == DEPTH):
            with contextlib.ExitStack() as ph:
                pv = SB(ph, "pvf", [128, NPV])
                P.dma("sp", lambda e: e.dma_start(out=pv[:], in_=pvd[0]), writes=["pvf"])
                rb = [SB(ph, f"r{i}", [128, 8, 512]) for i in range(2)]; sqb = SB(ph, "sqb", [128, 8, 512], BF16); rstd = SB(ph, "rstd", [128, 512])
                ob = [SB(ph, f"of{i}", [128, 8, 512]) for i in range(2)]
                for ci, (s0, n) in list(enumerate(CH))[1:]:
                    b = ci % 2
                    P.dma("sp", lambda e, b=b, s0=s0, n=n: e.dma_start(out=rb[b][:, :, :n], in_=res.rearrange("(k p) s -> p k s", p=128)[:, :, s0:s0 + n]), writes=[f"r{b}"])
                    rms_rstd(rb[b], f"r{b}", sqb, "sqb", n, 7, rstd, "rstd")
                    for k in range(8):
                        P.op("dve", lambda e, b=b, k=k, n=n: e.scalar_tensor_tensor(out=ob[b][:, k, :n], in0=rb[b][:, k, :n], scalar=pv[:, PV["fng"] + k:PV["fng"] + k + 1], in1=rstd[:, :n], op0=ALU.mult, op1=ALU.mult), reads=[f"r{b}", "rstd", "pvf"], writes=[f"of{b}"])
                    P.dma("sp", lambda e, b=b, s0=s0, n=n: e.dma_start(out=outT.rearrange("(k p) s -> p k s", p=128)[:, :, s0 - LC:s0 - LC + n], in_=ob[b][:, :, :n]), reads=[f"of{b}"])
                P.barrier(); P.emit()
    return nc


_CONSTS = None


def make_in_maps(inputs, cores=range(8)):
    global _CONSTS
    if _CONSTS is None:
        _CONSTS = make_consts()
    c = _CONSTS
    f = lambda a: np.ascontiguousarray(np.asarray(a, np.float32))
    shared = dict(pos=c["pos"], w_mod=f(inputs["w_mod"]), pv=np.stack([pack_pv(inputs, l) for l in range(DEPTH)]),
                  bd=np.stack([pack_bd(inputs, l) for l in range(DEPTH)]), w_in=f(inputs["w_in"]), w_pw=f(inputs["w_pw"]),
                  w_out=f(inputs["w_out"]), w_router=f(inputs["w_router"]), b_router=f(inputs["b_router"]), w_gu=f(inputs["w_gu"]),
                  w_down=f(inputs["w_down"]), b_down=f(inputs["b_down"]), b_gu=f(inputs["b_gu"]), cst=c["cst"], dftx=c["dftx"], dftc=c["dftc"],
                  invc=c["invc"], iot=c["iot"], dalt=c["dalt"])
    maps = []
    for b in cores:
        xin = np.ascontiguousarray(np.concatenate([inputs["ctx"][b], inputs["x"][b]], axis=0).T.astype(np.float32))
        cvec = np.stack([_cols(inputs["c"][b]), _cols(inputs["c_ctx"])], axis=-1)
        m = dict(shared)
        m["xin"] = xin
        m["cvec"] = np.ascontiguousarray(cvec.astype(np.float32))
        maps.append(m)
    return maps


def kernel(**inputs):
    inputs = {k: np.asarray(v) for k, v in inputs.items()}
    nc = build_nc()
    maps = make_in_maps(inputs)
    res = run_bass_kernel_spmd(nc, maps, core_ids=list(range(8)))
    out = np.stack([np.ascontiguousarray(r["outT"].T) for r in res.results], axis=0)
    return out.astype(np.float32)
```

```python
import contextlib
import numpy as np
import ml_dtypes
import concourse.bass as bass
import concourse.mybir as mybir
from concourse.bass_utils import run_bass_kernel_spmd

F32 = mybir.dt.float32
BF16 = mybir.dt.bfloat16
AF = mybir.ActivationFunctionType
ALU = mybir.AluOpType
AX = mybir.AxisListType

D = 1024
LC = 256
LX = 4096
S = LC + LX
PADW = 16
SP = S + 3 * PADW
NE = 32
DEPTH = 2
EPS = 1e-6
BLK = 256
NBLK = 100
NSLOT = NBLK * BLK
U32 = mybir.dt.uint32
I32 = mybir.dt.int32
CH = [(0, 256)] + [(256 + 512 * i, 512) for i in range(8)]


def pcol(s):
    return s + PADW if s < LC else s + 2 * PADW


PV = {}
_o = 0
for _n, _k in [("b_mod", 48), ("n1g", 8), ("n2g", 8), ("b_in", 12), ("caw", 16), ("cab", 4), ("brr", 4), ("bri", 4),
               ("lam", 4), ("bpool", 2), ("pscale", 2), ("bfour", 2), ("cdw", 62), ("cdb", 2), ("lng", 2), ("lnb", 2),
               ("bpw", 2), ("b_out", 8), ("b_gu", 512), ("fng", 8)]:
    PV[_n] = _o
    _o += _k
NPV = _o


def _cols(v):
    v = np.asarray(v, np.float32).reshape(-1)
    return v.reshape(-1, 128).T


def pack_pv(inp, l):
    pv = np.zeros((128, NPV), np.float32)

    def put(name, v, off=0):
        c = _cols(v)
        pv[:, PV[name] + off:PV[name] + off + c.shape[1]] = c

    put("b_mod", inp["b_mod"][l]); put("n1g", inp["norm1_g"][l]); put("n2g", inp["norm2_g"][l]); put("b_in", inp["b_in"][l])
    for d in range(2):
        for j in range(4):
            put("caw", inp["conv_a_w"][l, d, j], (d * 4 + j) * 2)
        put("cab", inp["conv_a_b"][l, d], d * 2)
        put("brr", inp["b_rg_r"][l, d], d * 2)
        put("bri", inp["b_rg_i"][l, d], d * 2)
        put("lam", inp["rg_lambda"][l, d], d * 2)
    put("bpool", inp["b_pool"][l]); put("pscale", inp["pool_scale"][l]); put("bfour", inp["b_four"][l])
    for j in range(31):
        put("cdw", inp["conv_d_w"][l, j], j * 2)
    put("cdb", inp["conv_d_b"][l]); put("lng", inp["ln_d_g"][l]); put("lnb", inp["ln_d_b"][l]); put("bpw", inp["b_pw"][l])
    put("b_out", inp["b_out"][l])
    for e in range(NE):
        put("b_gu", inp["b_gu"][l, e], e * 16)
    put("fng", inp["final_norm_g"])
    return pv


def pack_bd(inp, l):
    bd = np.zeros((128, 12, 128), np.float32)

    def blk(idx, w4, cg):
        for j in range(2):
            bd[64 * j:64 * j + 64, idx, 64 * j:64 * j + 64] = w4[2 * cg + j]

    for d in range(2):
        for cg in range(2):
            blk(d * 2 + cg, inp["w_rg_r"][l, d], cg)
            blk(4 + d * 2 + cg, inp["w_rg_i"][l, d], cg)
    for cg in range(2):
        blk(8 + cg, inp["w_pool"][l], cg)
        blk(10 + cg, inp["w_four"][l], cg)
    return bd


def make_consts():
    cst = np.zeros((128, 6, 128), np.float32)
    c = np.arange(64)
    ang = 2 * np.pi * np.outer(c, c) / 64.0
    for j in range(2):
        cst[64 * j:64 * j + 64, 0, 64 * j:64 * j + 64] = np.cos(ang) / 8.0
        cst[64 * j:64 * j + 64, 1, 64 * j:64 * j + 64] = -np.sin(ang) / 8.0
        cst[64 * j:64 * j + 64, 2, 64 * j:64 * j + 64] = 1.0 / 64.0
    cst[:, 3, :] = 1.0
    cst[:, 4, :] = np.eye(128)
    cst[:, 5, :] = np.triu(np.ones((128, 128), np.float32), 1)
    iot = np.zeros((128, 33 + NBLK), np.float32)
    iot[:, 32 + NBLK] = np.arange(128)
    iot[:, :32] = np.arange(32)[None, :]
    iot[:, 32:32 + NBLK] = (np.arange(NBLK) * BLK)[None, :]

    def dft(L):
        k = np.arange(L, dtype=np.int64)
        a = 2 * np.pi * ((np.outer(k, k) % L).astype(np.float64)) / L
        return np.stack([np.cos(a), np.sin(a)]).astype(np.float32) / np.sqrt(L)

    def tile_tab(t, L):
        nlt, nk = L // 128, L // 256
        return np.ascontiguousarray(t.reshape(2, nlt, 128, nk, 256).transpose(0, 3, 2, 1, 4))

    dftx = tile_tab(dft(LX), LX).astype(ml_dtypes.bfloat16)
    dftc = tile_tab(dft(LC), LC).astype(ml_dtypes.bfloat16)
    dalt = np.zeros((128, LX // 128, 2), np.float32)
    dalt[:, :, 0] = (((-1.0) ** np.arange(128)) / np.sqrt(LX))[:, None]
    dalt = dalt.astype(ml_dtypes.bfloat16)
    invc = np.ones((2, 128, SP), np.float32)
    for g, w in enumerate((2, 4, 8, 16)):
        for (c0, L) in ((PADW, LC), (2 * PADW + LC, LX)):
            t = np.arange(L)
            lo = np.clip(t - w // 2, 0, L)
            hi = np.clip(t + w - w // 2, 0, L)
            invc[g // 2, 64 * (g % 2):64 * (g % 2) + 64, c0:c0 + L] = 1.0 / (hi - lo)
    rows_n = LX // 64
    row = np.repeat(np.arange(rows_n), 64).astype(np.float32)
    col = np.tile(np.arange(64), rows_n).astype(np.float32)
    q = D // 4
    omega = (1.0 / (10000.0 ** (np.arange(q, dtype=np.float32) / q))).astype(np.float32)

    def emb(p):
        a = p[:, None] * omega[None, :]
        return np.concatenate([np.sin(a), np.cos(a)], axis=-1)

    pos = np.concatenate([emb(row), emb(col)], axis=-1).astype(np.float32)
    return dict(cst=cst, dftx=dftx, dftc=dftc, invc=invc, iot=iot, dalt=dalt, pos=np.ascontiguousarray(pos.T))


class Prog:
    ENG = ("pe", "dve", "act", "pool", "sp")
    KD = 8

    def __init__(self, nc, stack):
        self.nc = nc
        self.ops = {e: [] for e in self.ENG}
        self.cnt = {e: 0 for e in self.ENG}
        self.sems = {}
        for e in ("pe", "dve", "act", "pool"):
            self.sems[("e", e)] = stack.enter_context(nc.semaphore("s_" + e))
        for q in ("sp", "pool", "act"):
            for i in range(self.KD):
                self.sems[("d", q, i)] = stack.enter_context(nc.semaphore(f"d_{q}{i}"))
        self.dcnt = {q: 0 for q in ("sp", "pool", "act")}
        self.waited = {e: {} for e in self.ENG}
        self.state = {}
        self.bg = None
        self.bg_every = 12
        self._bgk = 0
        self._in_bg = False

    def _bg_step(self):
        self._in_bg = True
        try:
            next(self.bg)
        except StopIteration:
            self.bg = None
        self._in_bg = False

    def _tick(self):
        if self.bg is None or self._in_bg:
            return
        self._bgk += 1
        if self._bgk % self.bg_every == 0:
            self._bg_step()

    def drain(self):
        while self.bg is not None:
            self._bg_step()

    def _st(self, name, reg):
        d = self.state.setdefault(name, {})
        if reg not in d:
            d[reg] = {"w": None, "r": {}}
        return d[reg]

    def _states(self, name, reg):
        d = self.state.get(name, {})
        if reg is None:
            return list(d.values())
        out = []
        if reg in d:
            out.append(d[reg])
        if None in d:
            out.append(d[None])
        return out

    def _deps(self, reads, writes):
        evs = []
        for (name, reg) in reads:
            for st in self._states(name, reg):
                if st["w"] is not None:
                    evs.append(st["w"])
        for (name, reg) in writes:
            for st in self._states(name, reg):
                if st["w"] is not None:
                    evs.append(st["w"])
                evs.extend(st["r"].items())
        return evs

    def _commit(self, ev, reads, writes):
        for (name, reg) in reads:
            r = self._st(name, reg)["r"]
            if r.get(ev[0], 0) < ev[1]:
                r[ev[0]] = ev[1]
        for (name, reg) in writes:
            if reg is None:
                self.state[name] = {None: {"w": ev, "r": {}}}
            else:
                st = self._st(name, reg)
                st["w"] = ev
                st["r"] = {}

    def _waits(self, eng, evs):
        out = {}
        w = self.waited[eng]
        for (sk, v) in evs:
            if sk == ("e", "pe") and eng == "pe":
                continue
            if w.get(sk, 0) >= v:
                continue
            if out.get(sk, 0) < v:
                out[sk] = v
        for sk, v in out.items():
            w[sk] = v
        return list(out.items())

    @staticmethod
    def _keys(ks):
        return [(k, None) if isinstance(k, str) else (k[0], k[1]) for k in ks]

    def op(self, eng, fn, reads=(), writes=()):
        reads = self._keys(reads)
        writes = self._keys(writes)
        waits = self._waits(eng, self._deps(reads, writes))
        self.cnt[eng] += 1
        ev = (("e", eng), self.cnt[eng])
        self.ops[eng].append((waits, fn, ev, 1))
        self._commit(ev, reads, writes)
        self._tick()

    def dma(self, q, fn, reads=(), writes=()):
        reads = self._keys(reads)
        writes = self._keys(writes)
        n = self.dcnt[q]
        self.dcnt[q] += 1
        i = n % self.KD
        val = 16 * (n // self.KD + 1)
        evs = self._deps(reads, writes)
        if n >= self.KD:
            evs.append((("d", q, i), val - 16))
        waits = self._waits(q, evs)
        ev = (("d", q, i), val)
        self.ops[q].append((waits, fn, ev, 16))
        self._commit(ev, reads, writes)
        self._tick()

    def _all_events(self):
        evs = []
        for q in ("sp", "pool", "act"):
            n = self.dcnt[q]
            for i in range(self.KD):
                if n > i:
                    evs.append((("d", q, i), 16 * ((n - i + self.KD - 1) // self.KD)))
        for e in ("pe", "dve", "act", "pool"):
            if self.cnt[e]:
                evs.append((("e", e), self.cnt[e]))
        return evs

    def barrier(self):
        evs = self._all_events()
        for eng in self.ENG:
            waits = self._waits(eng, [ev for ev in evs if ev[0] != ("e", eng)])
            if waits:
                self.ops[eng].append((waits, None, None, 0))
        self.state = {}

    def emit(self):
        nc = self.nc
        sems = self.sems
        ops = self.ops
        self.ops = {e: [] for e in self.ENG}

        def run(name, e):
            for waits, fn, ev, inc in ops[name]:
                for (sk, v) in waits:
                    e.wait_ge(sems[sk], v)
                if fn is None:
                    continue
                fn(e).then_inc(sems[ev[0]], inc)

        with nc.Block() as block:
            @block.tensor
            def _(e):
                run("pe", e)

            @block.vector
            def _(e):
                run("dve", e)

            @block.scalar
            def _(e):
                run("act", e)

            @block.gpsimd
            def _(e):
                run("pool", e)

            @block.sync
            def _(e):
                run("sp", e)


def build_nc(debug=False, stop_after=None, depth=DEPTH, n_exp=NE, sparse=True):
    nc = bass.Bass("TRN2", target_bir_lowering=False)
    I = lambda n, s, d=F32: nc.dram_tensor(n, s, d, kind="ExternalInput").ap()
    xin = I("xin", [D, S]); cvec = I("cvec", [128, 8, 2]); pos = I("pos", [D, LX])
    w_mod = I("w_mod", [DEPTH, D, 6 * D]); pvd = I("pv", [DEPTH, 128, NPV]); bdd = I("bd", [DEPTH, 128, 12, 128])
    w_in = I("w_in", [DEPTH, D, 1536]); w_pw = I("w_pw", [DEPTH, 256, 256]); w_out = I("w_out", [DEPTH, D, D])
    w_router = I("w_router", [DEPTH, D, NE]); b_router = I("b_router", [DEPTH, NE])
    w_gu = I("w_gu", [DEPTH, n_exp, D, 2 * D]); w_down = I("w_down", [DEPTH, n_exp, D, D]); b_down = I("b_down", [DEPTH, NE, D])
    cstd = I("cst", [128, 6, 128]); dftx = I("dftx", [2, LX // 256, 128, LX // 128, 256], BF16); dftc = I("dftc", [2, LC // 256, 128, LC // 128, 256], BF16)
    invcd = I("invc", [2, 128, SP]); iotd = I("iot", [128, 33 + NBLK]); daltd = I("dalt", [128, LX // 128, 2], BF16); b_gu_d = I("b_gu", [DEPTH, NE, 2 * D])
    outT = nc.dram_tensor("outT", [D, LX], F32, kind="ExternalOutput").ap()
    sk = "ExternalOutput" if debug else "Internal"
    res = nc.dram_tensor("res", [D, S], F32, kind=sk).ap()
    proj = nc.dram_tensor("proj", [1536, S], F32, kind=sk).ap()
    ycat = nc.dram_tensor("ycat", [D, S], BF16, kind=sk).ap()
    hT = nc.dram_tensor("hT", [D, S], BF16, kind=sk).ap()
    gTd = nc.dram_tensor("gTd", [NE, S], F32, kind=sk).ap()
    WGbs = [nc.dram_tensor(f"WGb{i}", [NE, 128, 9, 2048], BF16).ap() for i in range(DEPTH)]
    WDbs = [nc.dram_tensor(f"WDb{i}", [NE, 128, 8, 1024], BF16).ap() for i in range(DEPTH)]
    htm = nc.dram_tensor("htm", [S, D], BF16, kind=sk).ap(); xs_d = nc.dram_tensor("xs_d", [NSLOT, D], BF16, kind=sk).ap()
    ys_d = nc.dram_tensor("ys_d", [NSLOT, D], F32, kind=sk).ap()

    with contextlib.ExitStack() as top:
        P = Prog(nc, top)
        PS = [top.enter_context(nc.psum_tensor(f"ps{i}", [128, 512], F32)) for i in range(8)]
        psn = [f"ps{i}" for i in range(8)]

        _uid = [0]

        def SB(st, n, s, d=F32):
            _uid[0] += 1
            return st.enter_context(nc.sbuf_tensor(f"sb{_uid[0]}_{n}", s, d))

        cst = SB(top, "cst", [128, 6, 128]); cstb = SB(top, "cstb", [128, 6, 128], BF16)
        iot = SB(top, "iot", [128, 33 + NBLK])
        P.dma("sp", lambda e: e.dma_start(out=iot[:], in_=iotd), writes=["iot"])
        def ind_dma(e, **kw):
            return e.indirect_dma_start(**kw)

        rB = top.enter_context(nc.gpsimd.register("rB"))
        vbox = {}
        dmy = SB(top, "dmy", [128, 8])

        def init_pool(e):
            e.reg_mov(rB, NE * 128 - 1)
            vbox["v"] = e.snap(rB, donate=True)
            return e.memset(dmy[:], 0.0)

        P.op("pool", init_pool, writes=["dmy"])

        def precast_gen(l):
            WGb, WDb = WGbs[l], WDbs[l]
            P.dma("pool", lambda e: e.dma_start(out=WGb[:, 0, 8, :], in_=b_gu_d[l]))
            yield
            for ex_ in range(n_exp):
                P.dma("pool", lambda e, ex_=ex_: e.dma_start(out=WGb[ex_, :, 0:8, :], in_=w_gu[l, ex_].rearrange("(k p) f -> p k f", p=128)))
                yield
                P.dma("pool", lambda e, ex_=ex_: e.dma_start(out=WDb[ex_, :, :, :], in_=w_down[l, ex_].rearrange("(k p) f -> p k f", p=128)))
                yield

        if sparse and stop_after is None:
            P.bg = precast_gen(0)
            P.bg_every = 100
        P.dma("sp", lambda e: e.dma_start(out=cst[:], in_=cstd), writes=["cst"])
        P.op("dve", lambda e: e.tensor_copy(out=cstb[:], in_=cst[:]), reads=["cst"], writes=["cstb"])
        C64b, S64b, MAVb, ONEb, IDb, UTb = (cstb[:, i, :] for i in range(6))
        ID32 = cst[:, 4, :]
        cv = SB(top, "cv", [128, 8, 2])
        P.dma("sp", lambda e: e.dma_start(out=cv[:], in_=cvec), writes=["cv"])
        P.op("act", lambda e: e.activation(out=cv[:], in_=cv[:], func=AF.Silu), reads=["cv"], writes=["cv"])
        P.barrier(); P.emit()

        def rms_rstd(st_r, rk, sqb, sqk, n, psi, rstd, rstdk):
            P.op("act", lambda e: e.activation(out=sqb[:, :, :n], in_=st_r[:, :, :n], func=AF.Square), reads=[rk], writes=[sqk])
            for k in range(8):
                P.op("pe", lambda e, k=k: e.matmul(PS[psi][:, :n], lhsT=ONEb, rhs=sqb[:, k, :n], start=(k == 0), stop=(k == 7)),
                     reads=[sqk, "cstb"], writes=[psn[psi]])
            P.op("dve", lambda e: e.tensor_scalar(out=rstd[:, :n], in0=PS[psi][:, :n], scalar1=1.0 / D, scalar2=EPS, op0=ALU.mult, op1=ALU.add),
                 reads=[psn[psi]], writes=[rstdk])
            P.op("act", lambda e: e.activation(out=rstd[:, :n], in_=rstd[:, :n], func=AF.Sqrt), reads=[rstdk], writes=[rstdk])
            P.op("dve", lambda e: e.reciprocal(out=rstd[:, :n], in_=rstd[:, :n]), reads=[rstdk], writes=[rstdk])

        for l in range(depth):
            last = (l == DEPTH - 1)
            with contextlib.ExitStack() as lay:
                pv = SB(lay, "pvt", [128, NPV]); modT = SB(lay, "modT", [128, 48, 2]); A1 = SB(lay, "A1", [128, 8, 2]); A2 = SB(lay, "A2", [128, 8, 2])
                P.dma("sp", lambda e: e.dma_start(out=pv[:], in_=pvd[l]), writes=["pvt"])
                pc = lambda name, i=0: pv[:, PV[name] + i:PV[name] + i + 1]
                NT = S // 128
                idxf = SB(lay, "idxf", [128, NT, 4]); g4 = SB(lay, "g4", [128, NT, 4]); rank4 = SB(lay, "rank4", [128, NT, 4]); carry = SB(lay, "carry", [128, NE])
                destu = SB(lay, "destu", [128, NT * 4], U32); widx = SB(lay, "widx", [128, NBLK], U32)
                WGb, WDb = WGbs[l], WDbs[l]
                with contextlib.ExitStack() as ph:
                    wm = [SB(ph, f"wm{i}", [128, 8, 768]) for i in range(2)]
                    mrow = SB(ph, "mrow", [2, 6 * D])
                    for q in range(8):
                        b = q % 2
                        P.dma("sp", lambda e, b=b, q=q: e.dma_start(out=wm[b][:], in_=w_mod[l].rearrange("(k p) n -> p k n", p=128)[:, :, q * 768:(q + 1) * 768]), writes=[f"wm{b}"])
                        for hh in range(2):
                            pi = 1 + hh
                            for k in range(8):
                                P.op("pe", lambda e, b=b, hh=hh, pi=pi, k=k: e.matmul(PS[pi][0:2, 0:384], lhsT=cv[:, k, :], rhs=wm[b][:, k, hh * 384:(hh + 1) * 384], start=(k == 0), stop=(k == 7)),
                                     reads=[f"wm{b}", "cv"], writes=[psn[pi]])
                            P.op("act", lambda e, q=q, hh=hh, pi=pi: e.activation(out=mrow[:, q * 768 + hh * 384:q * 768 + (hh + 1) * 384], in_=PS[pi][0:2, 0:384], func=AF.Copy), reads=[psn[pi]], writes=["mrow"])
                    for j in range(48):
                        P.op("pe", lambda e, j=j: e.transpose(out=PS[0][:, 2 * j:2 * j + 2], in_=mrow[0:2, j * 128:(j + 1) * 128], identity=cst[0:2, 4, 0:2]), reads=["mrow", "cst"], writes=["ps0"])
                    psm = PS[0][:, 0:96].rearrange("p (j w) -> p j w", w=2)
                    for w in range(2):
                        P.op("dve", lambda e, w=w: e.tensor_tensor(out=modT[:, :, w], in0=psm[:, :, w], in1=pv[:, PV["b_mod"]:PV["b_mod"] + 48], op=ALU.add), reads=["ps0", "pvt"], writes=["modT"])
                        for (A, sc0, gn) in ((A1, 8, "n1g"), (A2, 32, "n2g")):
                            P.op("dve", lambda e, w=w, A=A, sc0=sc0: e.tensor_scalar(out=A[:, :, w], in0=modT[:, sc0:sc0 + 8, w], scalar1=1.0, scalar2=None, op0=ALU.add), reads=["modT"], writes=["A"])
                            P.op("dve", lambda e, w=w, A=A, gn=gn: e.tensor_tensor(out=A[:, :, w], in0=A[:, :, w], in1=pv[:, PV[gn]:PV[gn] + 8], op=ALU.mult), reads=["A", "pvt"], writes=["A"])
                    P.barrier(); P.emit()
                SH1 = lambda k, w: modT[:, k, w:w + 1]
                G1 = lambda k, w: modT[:, 16 + k, w:w + 1]
                SH2 = lambda k, w: modT[:, 24 + k, w:w + 1]
                G2 = lambda k, w: modT[:, 40 + k, w:w + 1]

                with contextlib.ExitStack() as ph:
                    wst = SB(ph, "wst", [128, 8, 1536]); wib = SB(ph, "wib", [128, 8, 1536], BF16)
                    P.dma("sp", lambda e: e.dma_start(out=wst[:], in_=w_in[l].rearrange("(k p) n -> p k n", p=128)), writes=["wst"])
                    for k in range(8):
                        P.op("pool", lambda e, k=k: e.tensor_copy(out=wib[:, k, :], in_=wst[:, k, :]), reads=["wst"], writes=[("wib", k)])
                    rb = [SB(ph, f"r{i}", [128, 8, 512]) for i in range(2)]
                    posb = [SB(ph, f"posb{i}", [128, 8, 512]) for i in range(2)] if l == 0 else None
                    sqb = SB(ph, "sqb", [128, 8, 512], BF16); tmp = SB(ph, "tmp", [128, 8, 512])
                    ub = [SB(ph, f"u{i}", [128, 8, 512], BF16) for i in range(2)]
                    rstd = SB(ph, "rstd", [128, 512]); ot = [SB(ph, f"ot{i}", [128, 512]) for i in range(4)]
                    oc = 0
                    for ci, (s0, n) in enumerate(CH):
                        b = ci % 2; w = 1 if ci == 0 else 0
                        if l == 0:
                            P.dma("sp", lambda e, b=b, s0=s0, n=n: e.dma_start(out=rb[b][:, :, :n], in_=xin.rearrange("(k p) s -> p k s", p=128)[:, :, s0:s0 + n]), writes=[f"r{b}"])
                            if ci > 0:
                                P.dma("act", lambda e, b=b, s0=s0, n=n: e.dma_start(out=posb[b][:, :, :n], in_=pos.rearrange("(k p) s -> p k s", p=128)[:, :, s0 - LC:s0 - LC + n]), writes=[f"posb{b}"])
                                P.op("pool", lambda e, b=b, n=n: e.tensor_tensor(out=rb[b][:, :, :n], in0=rb[b][:, :, :n], in1=posb[b][:, :, :n], op=ALU.add), reads=[f"r{b}", f"posb{b}"], writes=[f"r{b}"])
                            P.dma("act", lambda e, b=b, s0=s0, n=n: e.dma_start(out=res.rearrange("(k p) s -> p k s", p=128)[:, :, s0:s0 + n], in_=rb[b][:, :, :n]), reads=[f"r{b}"])
                        else:
                            P.dma("sp", lambda e, b=b, s0=s0, n=n: e.dma_start(out=rb[b][:, :, :n], in_=res.rearrange("(k p) s -> p k s", p=128)[:, :, s0:s0 + n]), writes=[f"r{b}"])
                        rms_rstd(rb[b], f"r{b}", sqb, "sqb", n, 7, rstd, "rstd")
                        for k in range(8):
                            P.op("dve", lambda e, b=b, k=k, n=n: e.tensor_tensor(out=tmp[:, k, :n], in0=rb[b][:, k, :n], in1=rstd[:, :n], op=ALU.mult), reads=[f"r{b}", "rstd"], writes=[("tmp", k)])
                            P.op("act", lambda e, b=b, k=k, n=n, w=w: e.activation(out=ub[b][:, k, :n], in_=tmp[:, k, :n], func=AF.Identity, scale=A1[:, k, w:w + 1], bias=SH1(k, w)), reads=[("tmp", k), "A", "modT"], writes=[(f"u{b}", k)])
                        for fc in range(12):
                            pi = fc % 4
                            for k in range(8):
                                P.op("pe", lambda e, b=b, k=k, fc=fc, pi=pi, n=n: e.matmul(PS[pi][:, :n], lhsT=wib[:, k, fc * 128:(fc + 1) * 128], rhs=ub[b][:, k, :n], start=(k == 0), stop=(k == 7)),
                                     reads=[(f"u{b}", k), ("wib", k)], writes=[psn[pi]])
                            o = oc % 4; oc += 1
                            P.op("act", lambda e, o=o, pi=pi, fc=fc, n=n: e.activation(out=ot[o][:, :n], in_=PS[pi][:, :n], func=AF.Identity, bias=pc("b_in", fc)), reads=[psn[pi], "pvt"], writes=[f"ot{o}"])
                            P.dma("sp", lambda e, o=o, fc=fc, s0=s0, n=n: e.dma_start(out=proj[fc * 128:(fc + 1) * 128, s0:s0 + n], in_=ot[o][:, :n]), reads=[f"ot{o}"])
                    P.barrier(); P.emit()
                if stop_after == "inproj":
                    break

                PCH = [(pcol(s0), n) for (s0, n) in CH]
                mixs = contextlib.ExitStack()
                bdst = SB(mixs, "bdst", [128, 12, 128]); bdb = SB(mixs, "bdb", [128, 12, 128], BF16)
                P.dma("sp", lambda e: e.dma_start(out=bdst[:], in_=bdd[l]), writes=["bdst"])
                P.op("dve", lambda e: e.tensor_copy(out=bdb[:], in_=bdst[:]), reads=["bdst"], writes=["bdb"])

                def load_pad(t, tk, row0, eng="sp"):
                    P.op("pool", lambda e: e.memset(t[:], 0.0), writes=[tk])
                    P.dma(eng, lambda e: e.dma_start(out=t[:, PADW:PADW + LC], in_=proj[row0:row0 + 128, 0:LC]), writes=[tk])
                    P.dma(eng, lambda e: e.dma_start(out=t[:, 2 * PADW + LC:2 * PADW + S], in_=proj[row0:row0 + 128, LC:S]), writes=[tk])

                def store_seg(t, tk, row0):
                    P.dma("sp", lambda e: e.dma_start(out=ycat[row0:row0 + 128, 0:LC], in_=t[:, PADW:PADW + LC]), reads=[tk])
                    P.dma("sp", lambda e: e.dma_start(out=ycat[row0:row0 + 128, LC:S], in_=t[:, 2 * PADW + LC:2 * PADW + S]), reads=[tk])

                with contextlib.ExitStack() as ph:
                    xa = SB(ph, "xa", [128, SP]); xc = SB(ph, "xc", [128, SP]); xcb = SB(ph, "xcb", [128, SP], BF16)
                    rg = SB(ph, "rg", [128, SP]); ig = SB(ph, "ig", [128, SP]); aa = SB(ph, "aa", [128, SP]); bt = SB(ph, "bt", [128, SP])
                    hh = [SB(ph, f"hh{i}", [128, SP]) for i in range(2)]; ga = rg; yab = SB(ph, "yab", [128, SP], BF16)
                    sm = SB(ph, "sm", [128, 4])
                    c0, c1 = PADW, SP - PADW
                    for cg in range(2):
                        load_pad(xa, "xa", cg * 128)
                        for d in range(2):
                            for j in range(4):
                                o = (j - 3) if d == 0 else (3 - j)
                                wj = pc("caw", (d * 4 + j) * 2 + cg)
                                if j == 0:
                                    P.op("dve", lambda e, o=o, wj=wj, d=d, cg=cg: e.tensor_scalar(out=xc[:, c0:c1], in0=xa[:, c0 + o:c1 + o], scalar1=wj, scalar2=pc("cab", d * 2 + cg), op0=ALU.mult, op1=ALU.add), reads=["xa", "pvt"], writes=["xc"])
                                else:
                                    P.op("dve", lambda e, o=o, wj=wj: e.scalar_tensor_tensor(out=xc[:, c0:c1], in0=xa[:, c0 + o:c1 + o], scalar=wj, in1=xc[:, c0:c1], op0=ALU.mult, op1=ALU.add), reads=["xa", "xc", "pvt"], writes=["xc"])
                            P.op("pool", lambda e: e.tensor_copy(out=xcb[:, c0:c1], in_=xc[:, c0:c1]), reads=["xc"], writes=["xcb"])
                            for gi, (gt_, gk, bn) in enumerate(((rg, "rg", "brr"), (ig, "ig", "bri"))):
                                for qi, (p0, n) in enumerate(PCH):
                                    pi = (gi * 9 + qi) % 4
                                    P.op("pe", lambda e, pi=pi, gi=gi, d=d, cg=cg, p0=p0, n=n: e.matmul(PS[pi][:, :n], lhsT=bdb[:, gi * 4 + d * 2 + cg, :], rhs=xcb[:, p0:p0 + n], start=True, stop=True), reads=["xcb", "bdb"], writes=[psn[pi]])
                                    P.op("act", lambda e, pi=pi, gt_=gt_, bn=bn, d=d, cg=cg, p0=p0, n=n: e.activation(out=gt_[:, p0:p0 + n], in_=PS[pi][:, :n], func=AF.Sigmoid, bias=pc(bn, d * 2 + cg)), reads=[psn[pi], "pvt"], writes=[(gk, qi)])
                            P.op("act", lambda e, d=d, cg=cg: e.activation(out=sm[:, 0:1], in_=pc("lam", d * 2 + cg), func=AF.Exp, scale=-1.0), reads=["pvt"], writes=["sm"])
                            P.op("dve", lambda e: e.tensor_scalar(out=sm[:, 0:1], in0=sm[:, 0:1], scalar1=1.0, scalar2=None, op0=ALU.add), reads=["sm"], writes=["sm"])
                            P.op("act", lambda e: e.activation(out=sm[:, 1:2], in_=sm[:, 0:1], func=AF.Ln), reads=["sm"], writes=["sm"])
                            P.op("dve", lambda e: e.tensor_scalar(out=sm[:, 2:3], in0=sm[:, 1:2], scalar1=-8.0, scalar2=None, op0=ALU.mult), reads=["sm"], writes=["sm"])
                            P.op("act", lambda e: e.activation(out=aa[:, c0:c1], in_=rg[:, c0:c1], func=AF.Exp, scale=sm[:, 2:3]), reads=["rg", "sm"], writes=["aa"])
                            P.op("pool", lambda e: e.tensor_tensor(out=bt[:, c0:c1], in0=aa[:, c0:c1], in1=aa[:, c0:c1], op=ALU.mult), reads=["aa"], writes=["bt"])
                            P.op("dve", lambda e: e.tensor_scalar(out=bt[:, c0:c1], in0=bt[:, c0:c1], scalar1=-1.0, scalar2=1.0, op0=ALU.mult, op1=ALU.add), reads=["bt"], writes=["bt"])
                            P.op("act", lambda e: e.activation(out=bt[:, c0:c1], in_=bt[:, c0:c1], func=AF.Sqrt), reads=["bt"], writes=["bt"])
                            P.op("pool", lambda e: e.tensor_tensor(out=ig[:, c0:c1], in0=ig[:, c0:c1], in1=xc[:, c0:c1], op=ALU.mult), reads=["ig", "xc"], writes=["ig"])
                            P.op("dve", lambda e: e.tensor_tensor(out=bt[:, c0:c1], in0=bt[:, c0:c1], in1=ig[:, c0:c1], op=ALU.mult), reads=["bt", "ig"], writes=["bt"])
                            h = hh[d]; hk = f"hh{d}"
                            sc_, sx_ = slice(PADW, PADW + LC), slice(2 * PADW + LC, 2 * PADW + S)
                            if d == 0:
                                P.op("dve", lambda e, h=h: e.tensor_tensor_scan(out=h[:, sc_], data0=aa[:, sc_], data1=bt[:, sc_], initial=0.0, op0=ALU.mult, op1=ALU.add), reads=["aa", "bt"], writes=[hk])
                                P.op("dve", lambda e, h=h: e.tensor_tensor_scan(out=h[:, sx_], data0=aa[:, sx_], data1=bt[:, sx_], initial=h[:, PADW + LC - 1:PADW + LC], op0=ALU.mult, op1=ALU.add), reads=["aa", "bt", hk], writes=[hk])
                            else:
                                P.op("dve", lambda e, h=h: e.tensor_tensor_scan(out=h[:, sc_][:, ::-1], data0=aa[:, sc_][:, ::-1], data1=bt[:, sc_][:, ::-1], initial=0.0, op0=ALU.mult, op1=ALU.add), reads=["aa", "bt"], writes=[hk])
                                P.op("dve", lambda e, h=h: e.tensor_tensor_scan(out=h[:, sx_][:, ::-1], data0=aa[:, sx_][:, ::-1], data1=bt[:, sx_][:, ::-1], initial=h[:, PADW:PADW + 1], op0=ALU.mult, op1=ALU.add), reads=["aa", "bt", hk], writes=[hk])
                        load_pad(ga, "rg", 256 + cg * 128)
                        P.op("pool", lambda e: e.tensor_tensor(out=hh[0][:, c0:c1], in0=hh[0][:, c0:c1], in1=hh[1][:, c0:c1], op=ALU.add), reads=["hh0", "hh1"], writes=["hh0"])
                        P.op("pool", lambda e: e.tensor_tensor(out=aa[:, c0:c1], in0=ga[:, c0:c1], in1=ga[:, c0:c1], op=ALU.mult), reads=["rg"], writes=["aa"])
                        P.op("dve", lambda e: e.tensor_scalar(out=aa[:, c0:c1], in0=aa[:, c0:c1], scalar1=0.044715, scalar2=1.0, op0=ALU.mult, op1=ALU.add), reads=["aa"], writes=["aa"])
                        P.op("pool", lambda e: e.tensor_tensor(out=aa[:, c0:c1], in0=aa[:, c0:c1], in1=ga[:, c0:c1], op=ALU.mult), reads=["aa", "rg"], writes=["aa"])
                        P.op("act", lambda e: e.activation(out=aa[:, c0:c1], in_=aa[:, c0:c1], func=AF.Sigmoid, scale=1.5957691216057308), reads=["aa"], writes=["aa"])
                        P.op("dve", lambda e: e.tensor_tensor(out=aa[:, c0:c1], in0=aa[:, c0:c1], in1=ga[:, c0:c1], op=ALU.mult), reads=["aa", "rg"], writes=["aa"])
                        P.op("dve", lambda e: e.tensor_tensor(out=yab[:, c0:c1], in0=aa[:, c0:c1], in1=hh[0][:, c0:c1], op=ALU.mult), reads=["aa", "hh0"], writes=["yab"])
                        store_seg(yab, "yab", cg * 128)
                    P.barrier(); P.emit()

                with contextlib.ExitStack() as ph:
                    xb = SB(ph, "xb", [128, SP]); wa = SB(ph, "wa", [128, SP]); wb = SB(ph, "wb", [128, SP]); ivc = SB(ph, "ivc", [128, SP])
                    pbf = SB(ph, "pbf", [128, SP], BF16); ob = [SB(ph, f"ob{i}", [128, 512], BF16) for i in range(2)]; sm = SB(ph, "smb", [128, 2])
                    for cg in range(2):
                        load_pad(xb, "xb", 512 + cg * 128)
                        P.dma("sp", lambda e, cg=cg: e.dma_start(out=ivc[:], in_=invcd[cg]), writes=["ivc"])
                        P.op("pool", lambda e: e.memset(wa[:], 0.0), writes=["wa"])
                        P.op("pool", lambda e: e.memset(wb[:], 0.0), writes=["wb"])
                        P.op("dve", lambda e: e.tensor_tensor(out=wa[:, 1:SP], in0=xb[:, 0:SP - 1], in1=xb[:, 1:SP], op=ALU.add), reads=["xb"], writes=["wa"])
                        P.op("dve", lambda e: e.tensor_tensor(out=wb[:, 1:SP - 1], in0=wa[:, 0:SP - 2], in1=wa[:, 2:SP], op=ALU.add), reads=["wa"], writes=["wb"])
                        if cg == 1:
                            P.op("dve", lambda e: e.tensor_tensor(out=wa[:, 3:SP - 3], in0=wb[:, 1:SP - 5], in1=wb[:, 5:SP - 1], op=ALU.add), reads=["wb"], writes=["wa"])
                            P.op("dve", lambda e: e.tensor_tensor(out=wb[:, 7:SP - 7], in0=wa[:, 3:SP - 11], in1=wa[:, 11:SP - 3], op=ALU.add), reads=["wa"], writes=["wb"])
                        c0, c1 = PADW, SP - PADW
                        P.op("dve", lambda e: e.tensor_tensor(out=wa[0:64, c0:c1], in0=wa[0:64, c0:c1], in1=ivc[0:64, c0:c1], op=ALU.mult), reads=["wa", "ivc"], writes=["wa"])
                        P.op("dve", lambda e: e.tensor_tensor(out=wa[64:128, c0:c1], in0=wb[64:128, c0:c1], in1=ivc[64:128, c0:c1], op=ALU.mult), reads=["wb", "wa", "ivc"], writes=["wa"])
                        P.op("dve", lambda e: e.tensor_tensor(out=pbf[:, c0:c1], in0=wa[:, c0:c1], in1=xb[:, c0:c1], op=ALU.subtract), reads=["wa", "xb"], writes=["pbf"])
                        P.op("dve", lambda e, cg=cg: e.tensor_tensor(out=sm[:, 0:1], in0=pc("bpool", cg), in1=pc("pscale", cg), op=ALU.mult), reads=["pvt"], writes=["smb"])
                        for qi, (p0, n) in enumerate(PCH):
                            pi = qi % 4; o = qi % 2; s0 = CH[qi][0]
                            P.op("pe", lambda e, pi=pi, cg=cg, p0=p0, n=n: e.matmul(PS[pi][:, :n], lhsT=bdb[:, 8 + cg, :], rhs=pbf[:, p0:p0 + n], start=True, stop=True), reads=["pbf", "bdb"], writes=[psn[pi]])
                            P.op("act", lambda e, pi=pi, o=o, cg=cg, n=n: e.activation(out=ob[o][:, :n], in_=PS[pi][:, :n], func=AF.Identity, scale=pc("pscale", cg), bias=sm[:, 0:1]), reads=[psn[pi], "pvt", "smb"], writes=[f"ob{o}"])
                            P.dma("sp", lambda e, o=o, cg=cg, s0=s0, n=n: e.dma_start(out=ycat[256 + cg * 128:256 + (cg + 1) * 128, s0:s0 + n], in_=ob[o][:, :n]), reads=[f"ob{o}"])
                    P.barrier(); P.emit()

                with contextlib.ExitStack() as ph:
                    xs = SB(ph, "xs", [128, 1, LX]); xsb = SB(ph, "xsb", [128, 2, LX], BF16)
                    XCS = SB(ph, "XCS", [128, 32, 512], BF16)
                    TB = [[SB(ph, f"tb{i}{j}", [128, 32, 256], BF16) for j in range(2)] for i in range(2)]
                    fb = [SB(ph, f"fb{i}", [128, 512], BF16) for i in range(2)]; ob = [SB(ph, f"oc{i}", [128, 512], BF16) for i in range(2)]
                    bsb = [SB(ph, f"bsb{i}", [128, 256]) for i in range(2)]; fbp = [SB(ph, f"fbp{i}", [128, 256], BF16) for i in range(2)]; fbm = [SB(ph, f"fbm{i}", [128, 256], BF16) for i in range(2)]
                    obp = [SB(ph, f"obp{i}", [128, 256], BF16) for i in range(2)]; obm = [SB(ph, f"obm{i}", [128, 256], BF16) for i in range(2)]
                    dal = SB(ph, "dal", [128, LX // 128, 2], BF16)
                    it = 0
                    for (s0, L, tab) in ((0, LC, dftc), (LC, LX, dftx)):
                        nlt = L // 128
                        for cg in range(2):
                            P.dma("sp", lambda e, cg=cg, s0=s0, L=L: e.dma_start(out=xs[:, 0, :L], in_=proj[768 + cg * 128:768 + (cg + 1) * 128, s0:s0 + L]), writes=["xs"])
                            P.op("pool", lambda e, cg=cg, L=L: e.tensor_copy(out=xsb[:, cg, :L], in_=xs[:, 0, :L]), reads=["xs"], writes=[("xsb", cg)])
                        for lt in range(nlt):
                            pi = lt % 2
                            for q, (cg, M) in enumerate(((0, C64b), (1, C64b), (0, S64b), (1, S64b))):
                                P.op("pe", lambda e, pi=pi, q=q, cg=cg, M=M, lt=lt: e.matmul(PS[pi][:, q * 128:(q + 1) * 128], lhsT=xsb[:, cg, lt * 128:(lt + 1) * 128], rhs=M, start=True, stop=True), reads=[("xsb", cg), "cstb"], writes=[psn[pi]])
                            eng = "act" if lt % 2 == 0 else "dve"
                            if eng == "act":
                                P.op("act", lambda e, pi=pi, lt=lt: e.activation(out=XCS[:, lt, :], in_=PS[pi][:, :], func=AF.Copy), reads=[psn[pi]], writes=[("XCS", lt)])
                            else:
                                P.op("dve", lambda e, pi=pi, lt=lt: e.tensor_copy(out=XCS[:, lt, :], in_=PS[pi][:, :]), reads=[psn[pi]], writes=[("XCS", lt)])
                        n = 256
                        nk = L // n
                        if L == LX:
                            for kc in range(nk // 2):
                                tb = TB[it % 2]; tk = f"tb{it % 2}"; it += 1
                                for j in range(2):
                                    P.dma("sp" if j == 0 else "act", lambda e, tb=tb, j=j, kc=kc: e.dma_start(out=tb[j][:, :, :], in_=tab[j, kc]), writes=[tk + str(j)])
                                for cg in range(2):
                                    for j in range(2):
                                        pi = 2 + 2 * j + cg
                                        for lt in range(nlt):
                                            P.op("pe", lambda e, pi=pi, tb=tb, j=j, lt=lt, cg=cg: e.matmul(PS[pi][:, :n], lhsT=XCS[:, lt, j * 256 + cg * 128:j * 256 + (cg + 1) * 128], rhs=tb[j][:, lt, :n], start=(lt == 0), stop=(lt == nlt - 1)),
                                                 reads=[("XCS", lt), tk + str(j)], writes=[psn[pi]])
                                    P.op("act", lambda e, cg=cg: e.activation(out=bsb[cg][:], in_=PS[4 + cg][:, :n], func=AF.Copy), reads=[psn[4 + cg]], writes=[f"bsb{cg}"])
                                    P.op("dve", lambda e, cg=cg: e.tensor_tensor(out=fbp[cg][:], in0=PS[2 + cg][:, :n], in1=bsb[cg][:], op=ALU.add), reads=[psn[2 + cg], f"bsb{cg}"], writes=[f"fbp{cg}"])
                                    P.op("dve", lambda e, cg=cg: e.tensor_tensor(out=fbm[cg][:], in0=PS[2 + cg][:, :n], in1=bsb[cg][:], op=ALU.subtract), reads=[psn[2 + cg], f"bsb{cg}"], writes=[f"fbm{cg}"])
                                    P.op("pe", lambda e, cg=cg: e.matmul(PS[6][:, :n], lhsT=bdb[:, 10 + cg, :], rhs=fbp[cg][:], start=True, stop=True), reads=[f"fbp{cg}", "bdb"], writes=["ps6"])
                                    P.op("pe", lambda e, cg=cg: e.matmul(PS[7][:, :n], lhsT=bdb[:, 10 + cg, :], rhs=fbm[cg][:], start=True, stop=True), reads=[f"fbm{cg}", "bdb"], writes=["ps7"])
                                    P.op("act", lambda e, cg=cg: e.activation(out=obp[cg][:], in_=PS[6][:, :n], func=AF.Identity, bias=pc("bfour", cg)), reads=["ps6", "pvt"], writes=[f"obp{cg}"])
                                    P.dma("sp", lambda e, cg=cg, kc=kc: e.dma_start(out=ycat[512 + cg * 128:512 + (cg + 1) * 128, s0 + kc * n:s0 + (kc + 1) * n], in_=obp[cg][:]), reads=[f"obp{cg}"])
                                    j0 = 1 if kc == 0 else 0
                                    m = n - j0
                                    P.op("dve", lambda e, cg=cg, j0=j0, m=m: e.tensor_scalar(out=obm[cg][:, 0:m], in0=PS[7][:, j0:n][:, ::-1], scalar1=pc("bfour", cg), scalar2=None, op0=ALU.add), reads=["ps7", "pvt"], writes=[f"obm{cg}"])
                                    c_lo = L - n * kc - (n - 1)
                                    P.dma("act", lambda e, cg=cg, c_lo=c_lo, m=m: e.dma_start(out=ycat[512 + cg * 128:512 + (cg + 1) * 128, s0 + c_lo:s0 + c_lo + m], in_=obm[cg][:, 0:m]), reads=[f"obm{cg}"])
                            P.dma("sp", lambda e: e.dma_start(out=dal[:], in_=daltd), writes=["dal"])
                            for cg in range(2):
                                for lt in range(nlt):
                                    P.op("pe", lambda e, lt=lt, cg=cg: e.matmul(PS[2 + cg][:, 0:2], lhsT=XCS[:, lt, cg * 128:(cg + 1) * 128], rhs=dal[:, lt, :], start=(lt == 0), stop=(lt == nlt - 1)), reads=[("XCS", lt), "dal"], writes=[psn[2 + cg]])
                                P.op("dve", lambda e, cg=cg: e.tensor_copy(out=fbp[cg][:, 0:2], in_=PS[2 + cg][:, 0:2]), reads=[psn[2 + cg]], writes=[f"fbp{cg}"])
                                P.op("pe", lambda e, cg=cg: e.matmul(PS[6][:, 0:2], lhsT=bdb[:, 10 + cg, :], rhs=fbp[cg][:, 0:2], start=True, stop=True), reads=[f"fbp{cg}", "bdb"], writes=["ps6"])
                                P.op("act", lambda e, cg=cg: e.activation(out=obp[cg][:, 0:2], in_=PS[6][:, 0:2], func=AF.Identity, bias=pc("bfour", cg)), reads=["ps6", "pvt"], writes=[f"obp{cg}"])
                                P.dma("sp", lambda e, cg=cg: e.dma_start(out=ycat[512 + cg * 128:512 + (cg + 1) * 128, s0 + L // 2:s0 + L // 2 + 1], in_=obp[cg][:, 0:1], allow_slow_non_contiguous=True), reads=[f"obp{cg}"])
                            continue
                        for kc in range(nk):
                            tb = TB[it % 2]; tk = f"tb{it % 2}"; it += 1
                            for j in range(2):
                                P.dma("sp" if j == 0 else "act", lambda e, tb=tb, j=j, kc=kc, nlt=nlt, n=n, tab=tab: e.dma_start(out=tb[j][:, :nlt, :n], in_=tab[j, kc]), writes=[tk + str(j)])
                            for cg in range(2):
                                pi = 2 + (kc * 2 + cg) % 2
                                for j in range(2):
                                    for lt in range(nlt):
                                        P.op("pe", lambda e, pi=pi, tb=tb, j=j, lt=lt, cg=cg, n=n, nlt=nlt: e.matmul(PS[pi][:, :n], lhsT=XCS[:, lt, j * 256 + cg * 128:j * 256 + (cg + 1) * 128], rhs=tb[j][:, lt, :n], start=(j == 0 and lt == 0), stop=(j == 1 and lt == nlt - 1)),
                                             reads=[("XCS", lt), tk + str(j)], writes=[psn[pi]])
                                o = (kc * 2 + cg) % 2
                                P.op("dve", lambda e, pi=pi, o=o, n=n: e.tensor_copy(out=fb[o][:, :n], in_=PS[pi][:, :n]), reads=[psn[pi]], writes=[f"fb{o}"])
                                P.op("pe", lambda e, o=o, cg=cg, n=n: e.matmul(PS[4 + o][:, :n], lhsT=bdb[:, 10 + cg, :], rhs=fb[o][:, :n], start=True, stop=True), reads=[f"fb{o}", "bdb"], writes=[psn[4 + o]])
                                P.op("act", lambda e, o=o, cg=cg, n=n: e.activation(out=ob[o][:, :n], in_=PS[4 + o][:, :n], func=AF.Identity, bias=pc("bfour", cg)), reads=[psn[4 + o], "pvt"], writes=[f"oc{o}"])
                                P.dma("sp", lambda e, o=o, cg=cg, s0=s0, kc=kc, n=n: e.dma_start(out=ycat[512 + cg * 128:512 + (cg + 1) * 128, s0 + kc * n:s0 + (kc + 1) * n], in_=ob[o][:, :n]), reads=[f"oc{o}"])
                    P.barrier(); P.emit()

                with contextlib.ExitStack() as ph:
                    xv = SB(ph, "xv", [128, SP]); xg = SB(ph, "xg", [128, SP]); vb = [SB(ph, f"vb{i}", [128, SP], BF16) for i in range(2)]
                    dg = [SB(ph, f"dg{i}", [128, 31, 128], BF16) for i in range(2)]
                    wpst = SB(ph, "wpst", [128, 2, 256]); wpb = SB(ph, "wpb", [128, 2, 256], BF16)
                    vc = SB(ph, "vc", [128, 512]); vcb = SB(ph, "vcb", [128, 512], BF16); cen = SB(ph, "cen", [128, 512]); sq2 = SB(ph, "sq2", [128, 512], BF16)
                    rs2 = SB(ph, "rs2", [128, 512]); sg2 = SB(ph, "sg2", [128, 512]); svb = [SB(ph, f"svb{i}", [128, 512], BF16) for i in range(2)]
                    ob = [SB(ph, f"od{i}", [128, 512], BF16) for i in range(2)]
                    P.dma("sp", lambda e: e.dma_start(out=wpst[:], in_=w_pw[l].rearrange("(k p) n -> p k n", p=128)), writes=["wpst"])
                    P.op("dve", lambda e: e.tensor_copy(out=wpb[:], in_=wpst[:]), reads=["wpst"], writes=["wpb"])
                    for cg in range(2):
                        load_pad(xv, "xv", 1024 + cg * 128)
                        load_pad(xg, "xg", 1280 + cg * 128)
                        P.op("act", lambda e: e.activation(out=xg[:], in_=xg[:], func=AF.Sigmoid), reads=["xg"], writes=["xg"])
                        P.op("dve", lambda e, cg=cg: e.tensor_tensor(out=vb[cg][:], in0=xv[:], in1=xg[:], op=ALU.mult), reads=["xv", "xg"], writes=[f"vb{cg}"])
                        for j in range(31):
                            P.op("dve", lambda e, cg=cg, j=j: e.tensor_scalar(out=dg[cg][:, j, :], in0=ID32, scalar1=pc("cdw", j * 2 + cg), scalar2=None, op0=ALU.mult), reads=["cst", "pvt"], writes=[f"dg{cg}"])
                    for qi, (p0, n) in enumerate(PCH):
                        s0 = CH[qi][0]
                        for cg in range(2):
                            for j in range(31):
                                P.op("pe", lambda e, cg=cg, j=j, p0=p0, n=n: e.matmul(PS[cg][:, :n], lhsT=dg[cg][:, j, :], rhs=vb[cg][:, p0 + j - 15:p0 + j - 15 + n], start=(j == 0), stop=(j == 30)), reads=[f"vb{cg}", f"dg{cg}"], writes=[psn[cg]])
                            P.op("act", lambda e, cg=cg, n=n: e.activation(out=vc[:, :n], in_=PS[cg][:, :n], func=AF.Identity, bias=pc("cdb", cg)), reads=[psn[cg], "pvt"], writes=["vc"])
                            P.op("pool", lambda e, n=n: e.tensor_copy(out=vcb[:, :n], in_=vc[:, :n]), reads=["vc"], writes=["vcb"])
                            P.op("pe", lambda e, n=n: e.matmul(PS[2][:, :n], lhsT=MAVb, rhs=vcb[:, :n], start=True, stop=True), reads=["vcb", "cstb"], writes=["ps2"])
                            P.op("dve", lambda e, n=n: e.tensor_tensor(out=cen[:, :n], in0=vc[:, :n], in1=PS[2][:, :n], op=ALU.subtract), reads=["vc", "ps2"], writes=["cen"])
                            P.op("act", lambda e, n=n: e.activation(out=sq2[:, :n], in_=cen[:, :n], func=AF.Square), reads=["cen"], writes=["sq2"])
                            P.op("pe", lambda e, n=n: e.matmul(PS[3][:, :n], lhsT=MAVb, rhs=sq2[:, :n], start=True, stop=True), reads=["sq2", "cstb"], writes=["ps3"])
                            P.op("dve", lambda e, n=n: e.tensor_scalar(out=rs2[:, :n], in0=PS[3][:, :n], scalar1=EPS, scalar2=None, op0=ALU.add), reads=["ps3"], writes=["rs2"])
                            P.op("act", lambda e, n=n: e.activation(out=rs2[:, :n], in_=rs2[:, :n], func=AF.Sqrt), reads=["rs2"], writes=["rs2"])
                            P.op("dve", lambda e, n=n: e.reciprocal(out=rs2[:, :n], in_=rs2[:, :n]), reads=["rs2"], writes=["rs2"])
                            P.op("dve", lambda e, n=n: e.tensor_tensor(out=cen[:, :n], in0=cen[:, :n], in1=rs2[:, :n], op=ALU.mult), reads=["cen", "rs2"], writes=["cen"])
                            P.op("act", lambda e, n=n, cg=cg: e.activation(out=cen[:, :n], in_=cen[:, :n], func=AF.Identity, scale=pc("lng", cg), bias=pc("lnb", cg)), reads=["cen", "pvt"], writes=["cen"])
                            P.op("act", lambda e, n=n: e.activation(out=sg2[:, :n], in_=cen[:, :n], func=AF.Sigmoid), reads=["cen"], writes=["sg2"])
                            P.op("dve", lambda e, n=n, cg=cg: e.tensor_tensor(out=svb[cg][:, :n], in0=cen[:, :n], in1=sg2[:, :n], op=ALU.mult), reads=["cen", "sg2"], writes=[f"svb{cg}"])
                        for oc_ in range(2):
                            for cg in range(2):
                                P.op("pe", lambda e, oc_=oc_, cg=cg, n=n: e.matmul(PS[4 + oc_][:, :n], lhsT=wpb[:, cg, oc_ * 128:(oc_ + 1) * 128], rhs=svb[cg][:, :n], start=(cg == 0), stop=(cg == 1)), reads=[f"svb{cg}", "wpb"], writes=[psn[4 + oc_]])
                            P.op("act", lambda e, oc_=oc_, n=n: e.activation(out=ob[oc_][:, :n], in_=PS[4 + oc_][:, :n], func=AF.Identity, bias=pc("bpw", oc_)), reads=[psn[4 + oc_], "pvt"], writes=[f"od{oc_}"])
                            P.dma("sp", lambda e, oc_=oc_, s0=s0, n=n: e.dma_start(out=ycat[768 + oc_ * 128:768 + (oc_ + 1) * 128, s0:s0 + n], in_=ob[oc_][:, :n]), reads=[f"od{oc_}"])
                    P.barrier(); P.emit()
                mixs.close()
                if stop_after == "mix":
                    break

                chs = list(enumerate(CH))
                if last:
                    chs = chs[1:]
                with contextlib.ExitStack() as ph:
                    wob = SB(ph, "wob", [128, 8, 1024], BF16)
                    GT = SB(ph, "GT", [NE, S])
                    wr = SB(ph, "wr", [128, 8, NE]); brb = SB(ph, "brb", [128, NE]); bdn = SB(ph, "bdn", [NE, D])
                    P.dma("sp", lambda e: e.dma_start(out=wr[:], in_=w_router[l].rearrange("(k p) n -> p k n", p=128)), writes=["wr"])
                    P.dma("sp", lambda e: e.dma_start(out=brb[:], in_=b_router[l:l + 1, :].partition_broadcast(128)), writes=["brb"])
                    P.dma("sp", lambda e: e.dma_start(out=bdn[:], in_=b_down[l]), writes=["bdn"])
                    yc = [SB(ph, f"yc{i}", [128, 8, 512], BF16) for i in range(2)]
                    rb = [SB(ph, f"r{i}", [128, 8, 512]) for i in range(2)]
                    yt = SB(ph, "yt", [128, 512]); sqb = SB(ph, "sqb", [128, 8, 512], BF16); rstd = SB(ph, "rstd", [128, 512])
                    tmp = SB(ph, "tmp", [128, 8, 512]); h32 = SB(ph, "h32", [128, 8, 512]); hb = SB(ph, "hbw", [128, 8, 512], BF16)
                    for hf_ in range(2):
                        P.dma("sp", lambda e, hf_=hf_: e.dma_start(out=tmp[:], in_=w_out[l].rearrange("(k p) n -> p k n", p=128)[:, :, hf_ * 512:(hf_ + 1) * 512]), writes=["tmp"])
                        P.op("pool", lambda e, hf_=hf_: e.tensor_copy(out=wob[:, :, hf_ * 512:(hf_ + 1) * 512], in_=tmp[:]), reads=["tmp"], writes=["wob"])
                    i8 = SB(ph, "i8", [128, 8], U32); mkb = SB(ph, "mkb", [128, NE], BF16); Rk = SB(ph, "Rk", [128, NE]); oh = SB(ph, "oh", [128, NE])
                    e4 = SB(ph, "e4", [128, 4]); htb = [SB(ph, f"htb{i}", [128, D], BF16) for i in range(4)]
                    P.op("pool", lambda e: e.memset(carry[:], 0.0), writes=["carry"])
                    lgs = [SB(ph, f"lg{i}", [128, NE]) for i in range(4)]; t8s = [SB(ph, f"t8{i}", [128, 8]) for i in range(4)]; exs = [SB(ph, f"ex{i}", [128, NE]) for i in range(4)]
                    mks = [SB(ph, f"mk{i}", [128, NE]) for i in range(4)]; s1s = [SB(ph, f"s1{i}", [128, 4]) for i in range(4)]
                    i8s = [SB(ph, f"i8{i}", [128, 8], U32) for i in range(4)]; e4s = [SB(ph, f"e4{i}", [128, 4]) for i in range(4)]; mkbs = [SB(ph, f"mkb{i}", [128, NE], BF16) for i in range(4)]
                    Rks = [SB(ph, f"Rk{i}", [128, NE]) for i in range(4)]; ohs = [SB(ph, f"oh{i}", [128, 4, NE]) for i in range(4)]
                    for ci, (s0, n) in chs:
                        b = ci % 2; w = 1 if ci == 0 else 0
                        P.dma("sp", lambda e, b=b, s0=s0, n=n: e.dma_start(out=yc[b][:, :, :n], in_=ycat.rearrange("(k p) s -> p k s", p=128)[:, :, s0:s0 + n]), writes=[f"yc{b}"])
                        P.dma("act", lambda e, b=b, s0=s0, n=n: e.dma_start(out=rb[b][:, :, :n], in_=res.rearrange("(k p) s -> p k s", p=128)[:, :, s0:s0 + n]), writes=[f"r{b}"])
                        for dc in range(8):
                            pi = dc % 4
                            for k in range(8):
                                P.op("pe", lambda e, b=b, pi=pi, dc=dc, k=k, n=n: e.matmul(PS[pi][:, :n], lhsT=wob[:, k, dc * 128:(dc + 1) * 128], rhs=yc[b][:, k, :n], start=(k == 0), stop=(k == 7)), reads=[f"yc{b}", "wob"], writes=[psn[pi]])
                            P.op("act", lambda e, pi=pi, dc=dc, n=n: e.activation(out=yt[:, :n], in_=PS[pi][:, :n], func=AF.Identity, bias=pc("b_out", dc)), reads=[psn[pi], "pvt"], writes=["yt"])
                            P.op("dve", lambda e, b=b, dc=dc, n=n, w=w: e.scalar_tensor_tensor(out=rb[b][:, dc, :n], in0=yt[:, :n], scalar=G1(dc, w), in1=rb[b][:, dc, :n], op0=ALU.mult, op1=ALU.add), reads=["yt", "modT", f"r{b}"], writes=[f"r{b}"])
                        rms_rstd(rb[b], f"r{b}", sqb, "sqb", n, 7, rstd, "rstd")
                        for k in range(8):
                            P.op("dve", lambda e, b=b, k=k, n=n: e.tensor_tensor(out=tmp[:, k, :n], in0=rb[b][:, k, :n], in1=rstd[:, :n], op=ALU.mult), reads=[f"r{b}", "rstd"], writes=[("tmp", k)])
                            P.op("act", lambda e, k=k, n=n, w=w: e.activation(out=h32[:, k, :n], in_=tmp[:, k, :n], func=AF.Identity, scale=A2[:, k, w:w + 1], bias=SH2(k, w)), reads=[("tmp", k), "A", "modT"], writes=[("h32", k)])
                            P.op("pool", lambda e, k=k, n=n: e.tensor_copy(out=hb[:, k, :n], in_=h32[:, k, :n]), reads=[("h32", k)], writes=[("hbw", k)])
                        P.dma("sp", lambda e, s0=s0, n=n: e.dma_start(out=hT.rearrange("(k p) s -> p k s", p=128)[:, :, s0:s0 + n], in_=hb[:, :, :n]), reads=["hbw"])
                        class Rec:
                            def __init__(self):
                                self.l = []

                            def op(self, *a_, **k_):
                                self.l.append([("op", a_, k_)])

                            def dma(self, *a_, **k_):
                                self.l.append([("dma", a_, k_)])

                            def group(self, items):
                                self.l.append([("op", a_, k_) for (a_, k_) in items])

                        def route_tile(R, ti, t0, s0=s0):
                            tg = (s0 + t0) // 128
                            lg_, t8_, s1_, ex_t, mk_, i8_, e4_, mkb_, Rk_, oh_ = (x[ti] for x in (lgs, t8s, s1s, exs, mks, i8s, e4s, mkbs, Rks, ohs))
                            kk = lambda nm: f"{nm}{ti}"
                            pl = PS[4][:, ti * NE:(ti + 1) * NE]
                            R.group([(("pe", lambda e, k=k: e.matmul(pl, lhsT=h32[:, k, t0:t0 + 128], rhs=wr[:, k, :], start=(k == 0), stop=(k == 7))), dict(reads=[("h32", k), "wr"], writes=[("ps4", ti)])) for k in range(8)])
                            R.op("dve", lambda e: e.tensor_tensor(out=lg_[:], in0=pl, in1=brb[:], op=ALU.add), reads=[("ps4", ti), "brb"], writes=[kk("lg")])
                            R.op("dve", lambda e: e.max(out=t8_[:], in_=lg_[:]), reads=[kk("lg")], writes=[kk("t8")])
                            R.op("dve", lambda e: e.tensor_scalar(out=s1_[:, 0:1], in0=t8_[:, 0:1], scalar1=-1.0, scalar2=None, op0=ALU.mult), reads=[kk("t8")], writes=[(kk("s1"), 0)])
                            R.op("act", lambda e: e.activation(out=ex_t[:], in_=lg_[:], func=AF.Exp, bias=s1_[:, 0:1]), reads=[kk("lg"), (kk("s1"), 0)], writes=[kk("ex")])
                            R.op("act", lambda e: e.activation(out=e4_[:], in_=t8_[:, 0:4], func=AF.Exp, bias=s1_[:, 0:1]), reads=[kk("t8"), (kk("s1"), 0)], writes=[kk("e4")])
                            R.op("dve", lambda e: e.tensor_scalar(out=mk_[:], in0=lg_[:], scalar1=t8_[:, 3:4], scalar2=None, op0=ALU.is_ge), reads=[kk("lg"), kk("t8")], writes=[kk("mk")])
                            R.op("dve", lambda e: e.max_index(out=i8_[:], in_max=t8_[:], in_values=lg_[:]), reads=[kk("lg"), kk("t8")], writes=[kk("i8")])
                            R.op("dve", lambda e: e.tensor_reduce(out=s1_[:, 3:4], in_=e4_[:], axis=AX.X, op=ALU.add), reads=[kk("e4")], writes=[(kk("s1"), 3)])
                            R.op("dve", lambda e: e.reciprocal(out=s1_[:, 3:4], in_=s1_[:, 3:4]), reads=[(kk("s1"), 3)], writes=[(kk("s1"), 3)])
                            R.op("dve", lambda e: e.tensor_scalar(out=g4[:, tg, :], in0=e4_[:], scalar1=s1_[:, 3:4], scalar2=None, op0=ALU.mult), reads=[kk("e4"), (kk("s1"), 3)], writes=[("g4", tg)])
                            R.op("dve", lambda e: e.scalar_tensor_tensor(out=ex_t[:], in0=ex_t[:], scalar=s1_[:, 3:4], in1=mk_[:], op0=ALU.mult, op1=ALU.mult), reads=[kk("ex"), kk("mk"), (kk("s1"), 3)], writes=[kk("ex")])
                            R.op("dve", lambda e: e.tensor_copy(out=idxf[:, tg, :], in_=i8_[:, 0:4]), reads=[kk("i8")], writes=[("idxf", tg)])
                            R.op("pool", lambda e: e.tensor_copy(out=mkb_[:], in_=mk_[:]), reads=[kk("mk")], writes=[kk("mkb")])
                            pr0 = PS[6][:, ti * 128:ti * 128 + NE]; pr1 = PS[6][:, ti * 128 + 64:ti * 128 + 64 + NE]
                            R.op("pe", lambda e: e.matmul(pr0, lhsT=UTb, rhs=mkb_[:], start=True, stop=True), reads=[kk("mkb"), "cstb"], writes=[("ps6", 2 * ti)])
                            R.op("pe", lambda e: e.matmul(pr1, lhsT=ONEb, rhs=mkb_[:], start=True, stop=True), reads=[kk("mkb"), "cstb"], writes=[("ps6", 2 * ti + 1)])
                            R.group([(("dve", lambda e: e.tensor_tensor(out=Rk_[:], in0=pr0, in1=carry[:], op=ALU.add)), dict(reads=[("ps6", 2 * ti), "carry"], writes=[kk("Rk")])),
                                     (("dve", lambda e: e.tensor_tensor(out=carry[:], in0=pr1, in1=carry[:], op=ALU.add)), dict(reads=[("ps6", 2 * ti + 1), "carry"], writes=["carry"]))])
                            for k4 in range(4):
                                R.op("dve", lambda e, k4=k4: e.tensor_scalar(out=oh_[:, k4, :], in0=iot[:, 0:NE], scalar1=idxf[:, tg, k4:k4 + 1], scalar2=None, op0=ALU.is_equal), reads=["iot", ("idxf", tg)], writes=[(kk("oh"), k4)])
                                R.op("dve", lambda e, k4=k4: e.tensor_tensor(out=oh_[:, k4, :], in0=oh_[:, k4, :], in1=Rk_[:], op=ALU.mult), reads=[(kk("oh"), k4), kk("Rk")], writes=[(kk("oh"), k4)])
                                R.op("dve", lambda e, k4=k4: e.tensor_reduce(out=rank4[:, tg, k4:k4 + 1], in_=oh_[:, k4, :], axis=AX.X, op=ALU.add), reads=[(kk("oh"), k4)], writes=[("rank4", tg * 4 + k4)])
                            hq = ti
                            pq = ti
                            R.group([(("pe", lambda e, k=k: e.transpose(out=PS[pq][:].bitcast(BF16)[:, k * 128:(k + 1) * 128], in_=hb[:, k, t0:t0 + 128], identity=IDb)), dict(reads=[("hbw", k), "cstb"], writes=[psn[pq]])) for k in range(8)])
                            R.op("act", lambda e: e.activation(out=htb[hq][:], in_=PS[pq][:].bitcast(BF16), func=AF.Copy), reads=[psn[pq]], writes=[f"htb{hq}"])
                            R.dma("act", lambda e: e.dma_start(out=htm[s0 + t0:s0 + t0 + 128, :], in_=htb[hq][:]), reads=[f"htb{hq}"])
                            pt = PS[5][0:NE, ti * 128:(ti + 1) * 128]
                            R.op("pe", lambda e: e.transpose(out=pt, in_=ex_t[:], identity=ID32), reads=[kk("ex"), "cst"], writes=[("ps5", ti)])
                            R.op("act", lambda e: e.activation(out=GT[:, s0 + t0:s0 + t0 + 128], in_=pt, func=AF.Copy), reads=[("ps5", ti)], writes=[("GT", s0 + t0)])

                        recs = []
                        for ti, t0 in enumerate(range(0, n, 128)):
                            R_ = Rec()
                            route_tile(R_, ti, t0)
                            recs.append(R_)
                        for step in range(max(len(r_.l) for r_ in recs)):
                            for r_ in recs:
                                if step < len(r_.l):
                                    for (kind, a_, k_) in r_.l[step]:
                                        getattr(P, kind)(*a_, **k_)
                        for dc in range(8):
                            pi = 2 + dc % 2
                            P.op("pe", lambda e, pi=pi, dc=dc, s0=s0, n=n: e.matmul(PS[pi][:, :n], lhsT=bdn[:, dc * 128:(dc + 1) * 128], rhs=GT[:, s0:s0 + n], start=True, stop=True), reads=["GT", "bdn"], writes=[psn[pi]])
                            P.op("dve", lambda e, pi=pi, b=b, dc=dc, n=n, w=w: e.scalar_tensor_tensor(out=rb[b][:, dc, :n], in0=PS[pi][:, :n], scalar=G2(dc, w), in1=rb[b][:, dc, :n], op0=ALU.mult, op1=ALU.add), reads=[psn[pi], "modT", f"r{b}"], writes=[f"r{b}"])
                        P.dma("sp", lambda e, b=b, s0=s0, n=n: e.dma_start(out=res.rearrange("(k p) s -> p k s", p=128)[:, :, s0:s0 + n], in_=rb[b][:, :, :n]), reads=[f"r{b}"])
                        P.dma("sp", lambda e, s0=s0, n=n: e.dma_start(out=gTd[:, s0:s0 + n], in_=GT[:, s0:s0 + n]), reads=["GT"])
                    P.barrier(); P.emit()
                if stop_after == "outproj":
                    break

                if sparse:
                    nblk = (4 * 128 * len([1 for ci_, (s0_, n_) in chs for _ in range(n_ // 128)])) // BLK + NE
                    tiles = [(s0_ + t0_) // 128 for ci_, (s0_, n_) in chs for t0_ in range(0, n_, 128)]
                    P.drain()
                    with contextlib.ExitStack() as ph:
                        ci_t = SB(ph, "ci_t", [128, NE], I32); padf = SB(ph, "padf", [128, NE]); pend = SB(ph, "pend", [128, NE]); pst = SB(ph, "pst", [128, NE])
                        one32 = SB(ph, "one32", [128, NE]); bef = SB(ph, "bef", [128, NBLK]); oh = SB(ph, "oh2", [128, NE]); pst4 = SB(ph, "pst4", [128, NT, 4])
                        hl = [SB(ph, f"hl{i}", [128, D], BF16) for i in range(3)]
                        P.op("pool", lambda e: e.memset(one32[:], 1.0), writes=["one32"])
                        P.op("pool", lambda e: e.memset(pst4[:], 0.0), writes=["pst4"])
                        P.op("dve", lambda e: e.tensor_scalar(out=padf[:], in0=carry[:], scalar1=float(BLK - 1), scalar2=None, op0=ALU.add), reads=["carry"], writes=["padf"])
                        P.op("dve", lambda e: e.tensor_copy(out=ci_t[:], in_=padf[:]), reads=["padf"], writes=["ci_t"])
                        P.op("dve", lambda e: e.tensor_scalar(out=ci_t[:], in0=ci_t[:], scalar1=8, scalar2=8, op0=ALU.arith_shift_right, op1=ALU.logical_shift_left), reads=["ci_t"], writes=["ci_t"])
                        P.op("dve", lambda e: e.tensor_copy(out=padf[:], in_=ci_t[:]), reads=["ci_t"], writes=["padf"])
                        P.op("dve", lambda e: e.tensor_tensor_scan(out=pend[:], data0=one32[:], data1=padf[:], initial=0.0, op0=ALU.mult, op1=ALU.add), reads=["one32", "padf"], writes=["pend"])
                        P.op("dve", lambda e: e.tensor_tensor(out=pst[:], in0=pend[:], in1=padf[:], op=ALU.subtract), reads=["pend", "padf"], writes=["pst"])
                        P.op("pool", lambda e: e.memset(bef[:], 0.0), writes=["bef"])
                        for ex_ in range(NE):
                            P.op("dve", lambda e, ex_=ex_: e.scalar_tensor_tensor(out=bef[:], in0=iot[:, 32:32 + NBLK], scalar=pend[:, ex_:ex_ + 1], in1=bef[:], op0=ALU.is_ge, op1=ALU.add), reads=["iot", "pend", "bef"], writes=["bef"])
                        P.op("dve", lambda e: e.tensor_scalar(out=bef[:], in0=bef[:], scalar1=float(NE - 1), scalar2=None, op0=ALU.min), reads=["bef"], writes=["bef"])
                        skp = SB(ph, "skp", [128, NBLK])
                        P.op("dve", lambda e: e.tensor_tensor(out=skp[:, 2:NBLK], in0=bef[:, 2:NBLK], in1=bef[:, 0:NBLK - 2], op=ALU.is_equal), reads=["bef"], writes=["skp"])
                        P.op("dve", lambda e: e.tensor_scalar(out=bef[:], in0=bef[:], scalar1=128.0, scalar2=iot[:, 32 + NBLK:33 + NBLK], op0=ALU.mult, op1=ALU.add), reads=["bef", "iot"], writes=["bef"])
                        P.op("dve", lambda e: e.scalar_tensor_tensor(out=bef[:, 2:NBLK], in0=skp[:, 2:NBLK], scalar=1048576.0, in1=bef[:, 2:NBLK], op0=ALU.mult, op1=ALU.add), reads=["bef", "skp"], writes=["bef"])
                        P.op("dve", lambda e: e.tensor_copy(out=widx[:], in_=bef[:]), reads=["bef"], writes=["widx"])
                        for tg in tiles:
                            for k4 in range(4):
                                P.op("dve", lambda e, tg=tg, k4=k4: e.tensor_scalar(out=oh[:], in0=iot[:, 0:NE], scalar1=idxf[:, tg, k4:k4 + 1], scalar2=None, op0=ALU.is_equal), reads=["iot", "idxf"], writes=["oh2"])
                                P.op("dve", lambda e: e.tensor_tensor(out=oh[:], in0=oh[:], in1=pst[:], op=ALU.mult), reads=["oh2", "pst"], writes=["oh2"])
                                P.op("dve", lambda e, tg=tg, k4=k4: e.tensor_reduce(out=pst4[:, tg, k4:k4 + 1], in_=oh[:], axis=AX.X, op=ALU.add), reads=["oh2"], writes=["pst4"])
                        P.op("dve", lambda e: e.tensor_tensor(out=pst4[:], in0=pst4[:], in1=rank4[:], op=ALU.add), reads=["pst4", "rank4"], writes=["pst4"])
                        P.op("dve", lambda e: e.tensor_copy(out=destu[:], in_=pst4[:].rearrange("p t k -> p (t k)")), reads=["pst4"], writes=["destu"])
                        for i_, tg in enumerate(tiles):
                            hq = i_ % 3
                            P.dma("sp", lambda e, hq=hq, tg=tg: e.dma_start(out=hl[hq][:], in_=htm[tg * 128:(tg + 1) * 128, :]), writes=[f"hl{hq}"])
                            for k4 in range(4):
                                P.dma("pool", lambda e, hq=hq, tg=tg, k4=k4: ind_dma(e, out=xs_d, out_offset=bass.IndirectOffsetOnAxis(ap=destu[:, tg * 4 + k4:tg * 4 + k4 + 1], axis=0), in_=hl[hq][:, :], in_offset=None), reads=[f"hl{hq}", "destu"])
                        P.barrier(); P.emit()

                    with contextlib.ExitStack() as ph:
                        wg = [SB(ph, f"wg{i}", [128, 9, 2048], BF16) for i in range(2)]
                        wd = [SB(ph, f"wd{i}", [128, 8, 1024], BF16) for i in range(2)]
                        xsb = [SB(ph, f"xsb{i}", [128, 2, D], BF16) for i in range(2)]
                        xT = [SB(ph, f"xT{i}", [128, 8, BLK], BF16) for i in range(2)]
                        aT = [SB(ph, f"aT{i}", [128, 8, BLK], BF16) for i in range(2)]
                        ysb = [SB(ph, f"ysb{i}", [128, 2, D]) for i in range(2)]
                        gt = [SB(ph, f"gt{i}", [128, BLK]) for i in range(2)]; sg = [SB(ph, f"sg{i}", [128, BLK]) for i in range(2)]; up = [SB(ph, f"up{i}", [128, BLK]) for i in range(2)]
                        onr = SB(ph, "onr", [1, BLK], BF16)
                        P.op("pool", lambda e: e.memset(onr[:], 1.0), writes=["onr"])

                        def wload(b):
                            p_ = b % 2
                            P.dma("pool", lambda e: ind_dma(e, out=wg[p_][:].rearrange("p k f -> p (k f)"), out_offset=None, in_=WGb.rearrange("e p k f -> (e p) (k f)"), in_offset=bass.IndirectOffsetOnAxis(ap=widx[:, b:b + 1], axis=0), bounds_check=vbox["v"], oob_is_err=False), reads=["widx"], writes=[f"wg{p_}"])
                            P.dma("pool", lambda e: ind_dma(e, out=wd[p_][:].rearrange("p k f -> p (k f)"), out_offset=None, in_=WDb.rearrange("e p k f -> (e p) (k f)"), in_offset=bass.IndirectOffsetOnAxis(ap=widx[:, b:b + 1], axis=0), bounds_check=vbox["v"], oob_is_err=False), reads=["widx"], writes=[f"wd{p_}"])
                            P.dma("sp", lambda e: e.dma_start(out=xsb[p_][:], in_=xs_d[b * BLK:(b + 1) * BLK, :].rearrange("(j p) d -> p j d", p=128)), writes=[f"xsb{p_}"])

                        def block(b):
                            p_ = b % 2
                            if b + 1 < nblk:
                                wload(b + 1)
                            for j in range(2):
                                psb = PS[6 + j][:].bitcast(BF16)
                                for k in range(8):
                                    P.op("pe", lambda e, j=j, k=k, psb=psb: e.transpose(out=psb[:, k * 128:(k + 1) * 128], in_=xsb[p_][:, j, k * 128:(k + 1) * 128], identity=IDb), reads=[f"xsb{p_}", "cstb"], writes=[psn[6 + j]])
                                if j == 0:
                                    P.op("act", lambda e, psb=psb: e.activation(out=xT[p_][:, :, 0:128], in_=psb.rearrange("p (k t) -> p k t", t=128), func=AF.Copy), reads=[psn[6]], writes=[(f"xT{p_}", 0)])
                                else:
                                    P.op("dve", lambda e, psb=psb: e.tensor_copy(out=xT[p_][:, :, 128:256], in_=psb.rearrange("p (k t) -> p k t", t=128)), reads=[psn[7]], writes=[(f"xT{p_}", 1)])
                            for fc in range(8):
                                q = fc % 2
                                for hf in range(2):
                                    c0_ = hf * 1024 + fc * 128
                                    for k in range(8):
                                        P.op("pe", lambda e, q=q, hf=hf, c0_=c0_, k=k: e.matmul(PS[q][:, hf * BLK:(hf + 1) * BLK], lhsT=wg[p_][:, k, c0_:c0_ + 128], rhs=xT[p_][:, k, :], start=(k == 0), stop=False), reads=[f"wg{p_}", f"xT{p_}"], writes=[psn[q]])
                                    P.op("pe", lambda e, q=q, hf=hf, c0_=c0_: e.matmul(PS[q][:, hf * BLK:(hf + 1) * BLK], lhsT=wg[p_][0:1, 8, c0_:c0_ + 128], rhs=onr[0:1, :], start=False, stop=True), reads=[f"wg{p_}", "onr"], writes=[psn[q]])
                                P.op("dve", lambda e, q=q: e.tensor_scalar(out=gt[q][:], in0=PS[q][:, 0:BLK], scalar1=7.0, scalar2=None, op0=ALU.min), reads=[psn[q]], writes=[f"gt{q}"])
                                P.op("act", lambda e, q=q: e.activation(out=sg[q][:], in_=gt[q][:], func=AF.Sigmoid, scale=1.702), reads=[f"gt{q}"], writes=[f"sg{q}"])
                                P.op("dve", lambda e, q=q: e.tensor_scalar(out=up[q][:], in0=PS[q][:, BLK:2 * BLK], scalar1=7.0, scalar2=-7.0, op0=ALU.min, op1=ALU.max), reads=[psn[q]], writes=[f"up{q}"])
                                P.op("pool", lambda e, q=q: e.tensor_tensor(out=gt[q][:], in0=gt[q][:], in1=sg[q][:], op=ALU.mult), reads=[f"gt{q}", f"sg{q}"], writes=[f"gt{q}"])
                                P.op("dve", lambda e, q=q, fc=fc: e.scalar_tensor_tensor(out=aT[p_][:, fc, :], in0=up[q][:], scalar=1.0, in1=gt[q][:], op0=ALU.add, op1=ALU.mult), reads=[f"up{q}", f"gt{q}"], writes=[(f"aT{p_}", fc)])
                            for j in range(2):
                                for dh in range(2):
                                    q = 2 + (j * 2 + dh) % 4
                                    for fc in range(8):
                                        P.op("pe", lambda e, q=q, j=j, dh=dh, fc=fc: e.matmul(PS[q][:, :], lhsT=aT[p_][:, fc, j * 128:(j + 1) * 128], rhs=wd[p_][:, fc, dh * 512:(dh + 1) * 512], start=(fc == 0), stop=(fc == 7)), reads=[(f"aT{p_}", fc), f"wd{p_}"], writes=[psn[q]])
                                    if dh == 0:
                                        P.op("act", lambda e, q=q, j=j, dh=dh: e.activation(out=ysb[p_][:, j, dh * 512:(dh + 1) * 512], in_=PS[q][:, :], func=AF.Copy), reads=[psn[q]], writes=[(f"ysb{p_}", j * 2 + dh)])
                                    else:
                                        P.op("pool" if False else "dve", lambda e, q=q, j=j, dh=dh: e.tensor_copy(out=ysb[p_][:, j, dh * 512:(dh + 1) * 512], in_=PS[q][:, :]), reads=[psn[q]], writes=[(f"ysb{p_}", j * 2 + dh)])
                            P.dma("sp", lambda e: e.dma_start(out=ys_d[b * BLK:(b + 1) * BLK, :].rearrange("(j p) d -> p j d", p=128), in_=ysb[p_][:]), reads=[f"ysb{p_}"])

                        if l + 1 < depth and stop_after is None:
                            P.bg = precast_gen(l + 1)
                            P.bg_every = 500
                        wload(0)
                        for b in range(nblk):
                            block(b)
                        P.bg_every = 100
                        P.barrier(); P.emit()

                    with contextlib.ExitStack() as ph:
                        gb = [[SB(ph, f"gb{i}{k}", [128, D]) for k in range(4)] for i in range(2)]
                        acc = [SB(ph, f"acc{i}", [128, D]) for i in range(4)]
                        sqbc = SB(ph, "sqbc", [128, 8, 512], BF16); rstdc = SB(ph, "rstdc", [128, 512])
                        rb = [SB(ph, f"r{i}", [128, 8, 512]) for i in range(2)]
                        it_ = 0
                        for ci, (s0, n) in chs:
                            b = ci % 2; w = 1 if ci == 0 else 0
                            P.dma("sp", lambda e, b=b, s0=s0, n=n: e.dma_start(out=rb[b][:, :, :n], in_=res.rearrange("(k p) s -> p k s", p=128)[:, :, s0:s0 + n]), writes=[f"r{b}"])
                            for ti, t0 in enumerate(range(0, n, 128)):
                                tg = (s0 + t0) // 128
                                gq = it_ % 2; it_ += 1
                                for k4 in range(4):
                                    P.dma("pool", lambda e, gq=gq, k4=k4, tg=tg: ind_dma(e, out=gb[gq][k4][:, :], out_offset=None, in_=ys_d, in_offset=bass.IndirectOffsetOnAxis(ap=destu[:, tg * 4 + k4:tg * 4 + k4 + 1], axis=0)), reads=["destu"], writes=[f"gb{gq}{k4}"])
                                P.op("dve", lambda e, gq=gq, ti=ti, tg=tg: e.tensor_scalar(out=acc[ti][:], in0=gb[gq][0][:], scalar1=g4[:, tg, 0:1], scalar2=None, op0=ALU.mult), reads=[f"gb{gq}0", "g4"], writes=[f"acc{ti}"])
                                for k4 in range(1, 4):
                                    P.op("dve", lambda e, gq=gq, ti=ti, tg=tg, k4=k4: e.scalar_tensor_tensor(out=acc[ti][:], in0=gb[gq][k4][:], scalar=g4[:, tg, k4:k4 + 1], in1=acc[ti][:], op0=ALU.mult, op1=ALU.add), reads=[f"gb{gq}{k4}", "g4", f"acc{ti}"], writes=[f"acc{ti}"])
                            for k in range(8):
                                q = k % 4
                                for ti, t0 in enumerate(range(0, n, 128)):
                                    P.op("pe", lambda e, q=q, k=k, ti=ti, t0=t0: e.transpose(out=PS[q][:, t0:t0 + 128], in_=acc[ti][:, k * 128:(k + 1) * 128], identity=ID32), reads=[f"acc{ti}", "cst"], writes=[psn[q]])
                                P.op("dve", lambda e, q=q, k=k, b=b, n=n, w=w: e.scalar_tensor_tensor(out=rb[b][:, k, :n], in0=PS[q][:, :n], scalar=G2(k, w), in1=rb[b][:, k, :n], op0=ALU.mult, op1=ALU.add), reads=[psn[q], "modT", f"r{b}"], writes=[f"r{b}"])
                            if last and stop_after is None:
                                rms_rstd(rb[b], f"r{b}", sqbc, "sqbc", n, 7, rstdc, "rstdc")
                                for k in range(8):
                                    P.op("dve", lambda e, b=b, k=k, n=n: e.scalar_tensor_tensor(out=rb[b][:, k, :n], in0=rb[b][:, k, :n], scalar=pc("fng", k), in1=rstdc[:, :n], op0=ALU.mult, op1=ALU.mult), reads=[f"r{b}", "rstdc", "pvt"], writes=[f"r{b}"])
                                P.dma("sp", lambda e, b=b, s0=s0, n=n: e.dma_start(out=outT.rearrange("(k p) s -> p k s", p=128)[:, :, s0 - LC:s0 - LC + n], in_=rb[b][:, :, :n]), reads=[f"r{b}"])
                            else:
                                P.dma("sp", lambda e, b=b, s0=s0, n=n: e.dma_start(out=res.rearrange("(k p) s -> p k s", p=128)[:, :, s0:s0 + n], in_=rb[b][:, :, :n]), reads=[f"r{b}"])
                        P.barrier(); P.emit()
                    continue

                with contextlib.ExitStack() as ph:
                    wg = [SB(ph, f"wg{i}", [128, 8, 2048], BF16) for i in range(2)]
                    wd = [SB(ph, f"wd{i}", [128, 8, 1024], BF16) for i in range(2)]
                    stg = [SB(ph, f"stg{i}", [128, 2048]) for i in range(3)]
                    hbuf = [SB(ph, f"hb{i}", [128, 8, 512], BF16) for i in range(2)]
                    act_ = SB(ph, "actT", [128, 8, 512], BF16)
                    gbc = [SB(ph, f"gbc{i}", [128, 512]) for i in range(2)]
                    gt = [SB(ph, f"gt{i}", [128, 512]) for i in range(2)]; sg = [SB(ph, f"sg{i}", [128, 512]) for i in range(2)]; up = [SB(ph, f"up{i}", [128, 512]) for i in range(2)]
                    rtb = [SB(ph, f"rt{i}", [128, 8, 512]) for i in range(2)]; ty = [SB(ph, f"ty{i}", [128, 512]) for i in range(2)]
                    scn = [0]

                    def wstep(ex_, k):
                        wb_ = ex_ % 2
                        s_ = scn[0] % 3; scn[0] += 1
                        if k < 8:
                            P.dma("act", lambda e: e.dma_start(out=stg[s_][:, :], in_=w_gu[l, ex_, k * 128:(k + 1) * 128, :]), writes=[f"stg{s_}"])
                            P.op("pool", lambda e: e.tensor_copy(out=wg[wb_][:, k, :], in_=stg[s_][:, :]), reads=[f"stg{s_}"], writes=[(f"wg{wb_}", k)])
                        else:
                            k2 = k - 8
                            P.dma("act", lambda e: e.dma_start(out=stg[s_][:, 0:1024], in_=w_down[l, ex_, k2 * 128:(k2 + 1) * 128, :]), writes=[f"stg{s_}"])
                            P.op("pool", lambda e: e.tensor_copy(out=wd[wb_][:, k2, :], in_=stg[s_][:, 0:1024]), reads=[f"stg{s_}"], writes=[(f"wd{wb_}", k2)])

                    work = [(ex_, j, ci, s0, n) for ex_ in range(n_exp) for j, (ci, (s0, n)) in enumerate(chs)]

                    def loads(i):
                        ex_, j, ci, s0, n = work[i]
                        b_ = i % 2
                        P.dma("sp", lambda e: e.dma_start(out=hbuf[b_][:, :, :n], in_=hT.rearrange("(k p) s -> p k s", p=128)[:, :, s0:s0 + n]), writes=[f"hb{b_}"])
                        P.dma("sp", lambda e: e.dma_start(out=gbc[b_][:, :n], in_=gTd[ex_:ex_ + 1, s0:s0 + n].partition_broadcast(128)), writes=[f"gbc{b_}"])
                        P.dma("sp", lambda e: e.dma_start(out=rtb[b_][:, :, :n], in_=res.rearrange("(k p) s -> p k s", p=128)[:, :, s0:s0 + n]), reads=[("res", ci)], writes=[f"rt{b_}"])

                    for k in range(16):
                        wstep(0, k)
                    loads(0)
                    nch = len(chs)
                    def compute(i):
                        ex_, j, ci, s0, n = work[i]
                        wb_ = ex_ % 2; b_ = i % 2
                        w = 1 if ci == 0 else 0
                        if ex_ + 1 < n_exp:
                            for k in range(16):
                                if k * nch // 16 == j:
                                    wstep(ex_ + 1, k)
                        if i + 1 < len(work):
                            loads(i + 1)
                        for fc in range(8):
                            q = fc % 2
                            for k in range(8):
                                P.op("pe", lambda e, q=q, fc=fc, k=k: e.matmul(PS[q][:, :n], lhsT=wg[wb_][:, k, fc * 128:(fc + 1) * 128], rhs=hbuf[b_][:, k, :n], start=(k == 0), stop=(k == 7)), reads=[(f"wg{wb_}", k), f"hb{b_}"], writes=[psn[q]])
                            for k in range(8):
                                P.op("pe", lambda e, q=q, fc=fc, k=k: e.matmul(PS[2 + q][:, :n], lhsT=wg[wb_][:, k, 1024 + fc * 128:1024 + (fc + 1) * 128], rhs=hbuf[b_][:, k, :n], start=(k == 0), stop=(k == 7)), reads=[(f"wg{wb_}", k), f"hb{b_}"], writes=[psn[2 + q]])
                            bg = pc("b_gu", ex_ * 16 + fc); bu = pc("b_gu", ex_ * 16 + 8 + fc)
                            P.op("dve", lambda e, q=q, bg=bg: e.tensor_scalar(out=gt[q][:, :n], in0=PS[q][:, :n], scalar1=bg, scalar2=7.0, op0=ALU.add, op1=ALU.min), reads=[psn[q], "pvt"], writes=[f"gt{q}"])
                            P.op("act", lambda e, q=q: e.activation(out=sg[q][:, :n], in_=gt[q][:, :n], func=AF.Sigmoid, scale=1.702), reads=[f"gt{q}"], writes=[f"sg{q}"])
                            P.op("act", lambda e, q=q, bu=bu: e.activation(out=up[q][:, :n], in_=PS[2 + q][:, :n], func=AF.Identity, bias=bu), reads=[psn[2 + q], "pvt"], writes=[f"up{q}"])
                            P.op("pool", lambda e, q=q: e.tensor_scalar(out=up[q][:, :n], in0=up[q][:, :n], scalar1=7.0, scalar2=-7.0, op0=ALU.min, op1=ALU.max), reads=[f"up{q}"], writes=[f"up{q}"])
                            P.op("pool", lambda e, q=q: e.tensor_tensor(out=gt[q][:, :n], in0=gt[q][:, :n], in1=sg[q][:, :n], op=ALU.mult), reads=[f"gt{q}", f"sg{q}"], writes=[f"gt{q}"])
                            P.op("dve", lambda e, q=q, fc=fc: e.scalar_tensor_tensor(out=act_[:, fc, :n], in0=up[q][:, :n], scalar=1.0, in1=gt[q][:, :n], op0=ALU.add, op1=ALU.mult), reads=[f"up{q}", f"gt{q}"], writes=[("actT", fc)])
                        for dc in range(8):
                            q = dc % 2
                            for fc in range(8):
                                P.op("pe", lambda e, q=q, dc=dc, fc=fc: e.matmul(PS[4 + q][:, :n], lhsT=wd[wb_][:, fc, dc * 128:(dc + 1) * 128], rhs=act_[:, fc, :n], start=(fc == 0), stop=(fc == 7)), reads=[(f"wd{wb_}", fc), ("actT", fc)], writes=[psn[4 + q]])
                            P.op("dve", lambda e, q=q, dc=dc: e.scalar_tensor_tensor(out=ty[q][:, :n], in0=PS[4 + q][:, :n], scalar=G2(dc, w), in1=gbc[b_][:, :n], op0=ALU.mult, op1=ALU.mult), reads=[psn[4 + q], "modT", f"gbc{b_}"], writes=[f"ty{q}"])
                            P.op("pool", lambda e, q=q, dc=dc: e.tensor_tensor(out=rtb[b_][:, dc, :n], in0=rtb[b_][:, dc, :n], in1=ty[q][:, :n], op=ALU.add), reads=[f"rt{b_}", f"ty{q}"], writes=[f"rt{b_}"])
                        P.dma("sp", lambda e: e.dma_start(out=res.rearrange("(k p) s -> p k s", p=128)[:, :, s0:s0 + n], in_=rtb[b_][:, :, :n]), reads=[f"rt{b_}"], writes=[("res", ci)])
                    for i in range(len(work)):
                        compute(i)
                    P.barrier(); P.emit()

        if stop_after is None and not (sparse and depth == DEPTH):
            with contextlib.ExitStack() as ph:
                pv = SB(ph, "pvf", [128, NPV])
                P.dma("sp", lambda e: e.dma_start(out=pv[:], in_=pvd[0]), writes=["pvf"])
                rb = [SB(ph, f"r{i}", [128, 8, 512]) for i in range(2)]; sqb = SB(ph, "sqb", [128, 8, 512], BF16); rstd = SB(ph, "rstd", [128, 512])
                ob = [SB(ph, f"of{i}", [128, 8, 512]) for i in range(2)]
                for ci, (s0, n) in list(enumerate(CH))[1:]:
                    b = ci % 2
                    P.dma("sp", lambda e, b=b, s0=s0, n=n: e.dma_start(out=rb[b][:, :, :n], in_=res.rearrange("(k p) s -> p k s", p=128)[:, :, s0:s0 + n]), writes=[f"r{b}"])
                    rms_rstd(rb[b], f"r{b}", sqb, "sqb", n, 7, rstd, "rstd")
                    for k in range(8):
                        P.op("dve", lambda e, b=b, k=k, n=n: e.scalar_tensor_tensor(out=ob[b][:, k, :n], in0=rb[b][:, k, :n], scalar=pv[:, PV["fng"] + k:PV["fng"] + k + 1], in1=rstd[:, :n], op0=ALU.mult, op1=ALU.mult), reads=[f"r{b}", "rstd", "pvf"], writes=[f"of{b}"])
                    P.dma("sp", lambda e, b=b, s0=s0, n=n: e.dma_start(out=outT.rearrange("(k p) s -> p k s", p=128)[:, :, s0 - LC:s0 - LC + n], in_=ob[b][:, :, :n]), reads=[f"of{b}"])
                P.barrier(); P.emit()
    return nc


_CONSTS = None


def make_in_maps(inputs, cores=range(8)):
    global _CONSTS
    if _CONSTS is None:
        _CONSTS = make_consts()
    c = _CONSTS
    f = lambda a: np.ascontiguousarray(np.asarray(a, np.float32))
    shared = dict(pos=c["pos"], w_mod=f(inputs["w_mod"]), pv=np.stack([pack_pv(inputs, l) for l in range(DEPTH)]),
                  bd=np.stack([pack_bd(inputs, l) for l in range(DEPTH)]), w_in=f(inputs["w_in"]), w_pw=f(inputs["w_pw"]),
                  w_out=f(inputs["w_out"]), w_router=f(inputs["w_router"]), b_router=f(inputs["b_router"]), w_gu=f(inputs["w_gu"]),
                  w_down=f(inputs["w_down"]), b_down=f(inputs["b_down"]), b_gu=f(inputs["b_gu"]), cst=c["cst"], dftx=c["dftx"], dftc=c["dftc"],
                  invc=c["invc"], iot=c["iot"], dalt=c["dalt"])
    maps = []
    for b in cores:
        xin = np.ascontiguousarray(np.concatenate([inputs["ctx"][b], inputs["x"][b]], axis=0).T.astype(np.float32))
        cvec = np.stack([_cols(inputs["c"][b]), _cols(inputs["c_ctx"])], axis=-1)
        m = dict(shared)
        m["xin"] = xin
        m["cvec"] = np.ascontiguousarray(cvec.astype(np.float32))
        maps.append(m)
    return maps


def kernel(**inputs):
    inputs = {k: np.asarray(v) for k, v in inputs.items()}
    nc = build_nc()
    maps = make_in_maps(inputs)
    res = run_bass_kernel_spmd(nc, maps, core_ids=list(range(8)))
    out = np.stack([np.ascontiguousarray(r["outT"].T) for r in res.results], axis=0)
    return out.astype(np.float32)
```
